# Optimizing a Trainium2 kernel written in Bass

```python
import math
import jax, jax.numpy as jnp
from jax import lax
import numpy as np

D_MODEL = 1024
BATCH = 4
SEQ = 8192
DEPTH = 1

CHUNK = 64
Q_BLOCK = 128
EPS = 1e-6
D_FF = 2816
A_HEADS = 8
A_HEAD_DIM = 64
A_Q_W = A_HEADS * A_HEAD_DIM
IDX_HEADS = 8
IDX_DIM = 64
TOPK_MAX = 256
B_HEADS = 8
QK_NOPE = 64
QK_ROPE = 32
V_DIM = 64
Q_LORA = 384
KV_LORA = 256
B_V_W = B_HEADS * V_DIM
ROPE_THETA = 10000.0
NUM_BUCKETS = 32
MAX_DISTANCE = 128
IN_SIZES = (A_Q_W, A_HEAD_DIM, A_HEAD_DIM, IDX_HEADS * IDX_DIM, IDX_DIM, IDX_HEADS,
            Q_LORA, KV_LORA, QK_ROPE, 2 * D_MODEL)
IN_COLS = sum(IN_SIZES)
N_ADA = 9

kernel_name = "hybrid_dsa_mla_macaron_block"


def rmsnorm(x, g):
    xf = x.astype(jnp.float32)
    y = xf * lax.rsqrt(jnp.mean(xf * xf, axis=-1, keepdims=True) + EPS)
    return (y * g.astype(jnp.float32)).astype(x.dtype)


def modulate(h, shift, scale):
    return h * (1 + scale[:, None, :]) + shift[:, None, :]


def swiglu(h, w_in, w_down):
    g, u = jnp.split(h @ w_in, 2, axis=-1)
    return (jax.nn.silu(g) * u) @ w_down


def rope_angles(positions):
    half = QK_ROPE // 2
    freqs = ROPE_THETA ** (-2.0 * jnp.arange(half, dtype=jnp.float32) / QK_ROPE)
    ang = positions.astype(jnp.float32)[..., None] * freqs
    return jnp.cos(ang), jnp.sin(ang)


def apply_rope(x, cos, sin):
    cos = cos.astype(x.dtype)
    sin = sin.astype(x.dtype)
    x1, x2 = jnp.split(x, 2, axis=-1)
    return jnp.concatenate([x1 * cos - x2 * sin, x2 * cos + x1 * sin], axis=-1)


def t5_bucket(rel):
    half = NUM_BUCKETS // 2
    max_exact = half // 2
    ret = jnp.where(rel > 0, half, 0)
    n = jnp.abs(rel)
    nf = jnp.maximum(n, 1).astype(jnp.float32)
    large = max_exact + (jnp.log(nf / max_exact) / math.log(MAX_DISTANCE / max_exact)
                         * (half - max_exact)).astype(jnp.int32)
    large = jnp.minimum(large, half - 1)
    return ret + jnp.where(n < max_exact, n, large)


def to_blocks(a, nb):
    return jnp.swapaxes(a.reshape(a.shape[0], nb, Q_BLOCK, *a.shape[2:]), 0, 1)


def from_blocks(o):
    o = jnp.swapaxes(o, 0, 1)
    return o.reshape(o.shape[0], o.shape[1] * o.shape[2], -1)


def block_limit(t0):
    tq = t0 + jnp.arange(Q_BLOCK, dtype=jnp.int32)
    return (tq // CHUNK + 1) * CHUNK


def dsa_attention(q_a, k_a, v_a, q_idx, k_idx, w_idx, positions, rel_bias, topk):
    B, S = q_a.shape[0], q_a.shape[1]
    nb = S // Q_BLOCK
    key_ids = jnp.arange(S, dtype=jnp.int32)
    gather = jax.vmap(lambda a, i: a[i])
    idx_scale = IDX_DIM ** -0.5
    head_w_scale = IDX_HEADS ** -0.5
    attn_scale = A_HEAD_DIM ** -0.5
    k_idx_f = k_idx.astype(jnp.float32)

    def body(args):
        q_b, qi_b, wi_b, qpos_b, t0 = args
        limit = block_limit(t0)
        dots = jnp.einsum('bqhd,bsd->bqhs', qi_b.astype(jnp.float32), k_idx_f) * idx_scale
        score = jnp.einsum('bqh,bqhs->bqs', wi_b.astype(jnp.float32) * head_w_scale,
                           jax.nn.relu(dots))
        admissible = key_ids[None, :] < limit[:, None]
        score = jnp.where(admissible[None], score, -jnp.inf)
        _, sel = lax.top_k(score, topk)
        valid = sel < limit[None, :, None]
        k_sel = gather(k_a, sel)
        v_sel = gather(v_a, sel)
        pos_sel = gather(positions, sel)
        bias = rel_bias[t5_bucket(pos_sel - qpos_b[..., None])]
        s = jnp.einsum('bqhd,bqkd->bqhk', q_b, k_sel).astype(jnp.float32) * attn_scale
        s = s + jnp.moveaxis(bias, -1, 2).astype(jnp.float32)
        s = jnp.where(valid[:, :, None, :], s, -jnp.inf)
        p = jax.nn.softmax(s, axis=-1).astype(v_sel.dtype)
        return jnp.einsum('bqhk,bqkd->bqhd', p, v_sel)

    t0s = jnp.arange(nb, dtype=jnp.int32) * Q_BLOCK
    o = lax.map(body, (to_blocks(q_a, nb), to_blocks(q_idx, nb), to_blocks(w_idx, nb),
                       to_blocks(positions, nb), t0s))
    return from_blocks(o)


def mla_attention(q_nope, q_pe, k_nope, k_pe, v):
    S = q_nope.shape[1]
    nb = S // Q_BLOCK
    key_ids = jnp.arange(S, dtype=jnp.int32)
    scale = (QK_NOPE + QK_ROPE) ** -0.5

    def body(args):
        qn, qp, t0 = args
        limit = block_limit(t0)
        s = (jnp.einsum('bqhd,bshd->bhqs', qn, k_nope)
             + jnp.einsum('bqhr,bsr->bhqs', qp, k_pe)).astype(jnp.float32) * scale
        mask = key_ids[None, :] < limit[:, None]
        s = jnp.where(mask[None, None], s, -jnp.inf)
        p = jax.nn.softmax(s, axis=-1).astype(v.dtype)
        return jnp.einsum('bhqs,bshd->bqhd', p, v)

    t0s = jnp.arange(nb, dtype=jnp.int32) * Q_BLOCK
    o = lax.map(body, (to_blocks(q_nope, nb), to_blocks(q_pe, nb), t0s))
    return from_blocks(o)


def setup_inputs(seed: int = 0) -> dict:
    key = jax.random.key(seed)
    ks = jax.random.split(key, 32)
    f32 = jnp.float32

    def nrm(k, shape, fan_in, mult=1.0):
        return jax.random.normal(k, shape, f32) * (mult * fan_in ** -0.5)

    def gain(k, shape):
        return 1.0 + 0.05 * jax.random.normal(k, shape, f32)

    x = jax.random.normal(ks[0], (BATCH, SEQ, D_MODEL), f32)
    c = jax.random.normal(ks[1], (BATCH, D_MODEL), f32)
    start = jax.random.randint(ks[2], (BATCH, 1), 0, 4096, dtype=jnp.int32)
    positions = start + jnp.arange(SEQ, dtype=jnp.int32)[None, :]
    L = DEPTH
    return {
        "x": x,
        "c": c,
        "positions": positions,
        "w_ada": nrm(ks[3], (L, D_MODEL, N_ADA * D_MODEL), D_MODEL, 0.5),
        "b_ada": 0.02 * jax.random.normal(ks[4], (L, N_ADA * D_MODEL), f32),
        "g_ffn1": gain(ks[5], (L, D_MODEL)),
        "w_ffn1_in": nrm(ks[6], (L, D_MODEL, 2 * D_FF), D_MODEL),
        "w_ffn1_down": nrm(ks[7], (L, D_FF, D_MODEL), D_FF),
        "g_mix": gain(ks[8], (L, D_MODEL)),
        "w_in": nrm(ks[9], (L, D_MODEL, IN_COLS), D_MODEL),
        "g_cq": gain(ks[10], (L, Q_LORA)),
        "w_uq": nrm(ks[11], (L, Q_LORA, B_HEADS * (QK_NOPE + QK_ROPE)), Q_LORA),
        "g_ckv": gain(ks[12], (L, KV_LORA)),
        "w_uk": nrm(ks[13], (L, KV_LORA, B_HEADS * QK_NOPE), KV_LORA),
        "w_uv": nrm(ks[14], (L, KV_LORA, B_HEADS * V_DIM), KV_LORA),
        "rel_bias": 0.5 * jax.random.normal(ks[15], (NUM_BUCKETS, A_HEADS), f32),
        "w_o_a": nrm(ks[16], (L, A_Q_W, D_MODEL), A_Q_W),
        "w_o_b": nrm(ks[17], (L, B_V_W, D_MODEL), B_V_W),
        "w_out": nrm(ks[18], (L, D_MODEL, D_MODEL), D_MODEL),
        "g_ffn2": gain(ks[19], (L, D_MODEL)),
        "w_ffn2_in": nrm(ks[20], (L, D_MODEL, 2 * D_FF), D_MODEL),
        "w_ffn2_down": nrm(ks[21], (L, D_FF, D_MODEL), D_FF),
        "g_final": gain(ks[22], (D_MODEL,)),
    }


def reference(x, c, positions, w_ada, b_ada, g_ffn1, w_ffn1_in, w_ffn1_down, g_mix, w_in,
              g_cq, w_uq, g_ckv, w_uk, w_uv, rel_bias, w_o_a, w_o_b, w_out,
              g_ffn2, w_ffn2_in, w_ffn2_down, g_final):
    B, S, _ = x.shape
    topk = min(TOPK_MAX, S // 4)
    offsets = []
    acc = 0
    for n in IN_SIZES[:-1]:
        acc += n
        offsets.append(acc)
    cos, sin = rope_angles(positions)

    for l in range(DEPTH):
        mod = jax.nn.silu(c) @ w_ada[l] + b_ada[l]
        sh1, sc1, gt1, sh2, sc2, gt2, sh3, sc3, gt3 = jnp.split(mod, N_ADA, axis=-1)

        h = modulate(rmsnorm(x, g_ffn1[l]), sh1, sc1)
        x = x + 0.5 * gt1[:, None, :] * swiglu(h, w_ffn1_in[l], w_ffn1_down[l])

        h = modulate(rmsnorm(x, g_mix[l]), sh2, sc2)
        proj = h @ w_in[l]
        (q_a, k_a, v_a, q_idx, k_idx, w_idx, c_q, c_kv, k_rope,
         gate_logits) = jnp.split(proj, offsets, axis=-1)

        o_a = dsa_attention(q_a.reshape(B, S, A_HEADS, A_HEAD_DIM), k_a, v_a,
                            q_idx.reshape(B, S, IDX_HEADS, IDX_DIM), k_idx, w_idx,
                            positions, rel_bias, topk)

        q_b = (rmsnorm(c_q, g_cq[l]) @ w_uq[l]).reshape(B, S, B_HEADS, QK_NOPE + QK_ROPE)
        q_nope, q_pe = jnp.split(q_b, [QK_NOPE], axis=-1)
        c_kv_n = rmsnorm(c_kv, g_ckv[l])
        k_nope = (c_kv_n @ w_uk[l]).reshape(B, S, B_HEADS, QK_NOPE)
        v_b = (c_kv_n @ w_uv[l]).reshape(B, S, B_HEADS, V_DIM)
        q_pe = apply_rope(q_pe, cos[:, :, None, :], sin[:, :, None, :])
        k_pe = apply_rope(k_rope, cos, sin)
        o_b = mla_attention(q_nope, q_pe, k_nope, k_pe, v_b)

        g_a, g_b = jnp.split(jax.nn.sigmoid(gate_logits), 2, axis=-1)
        y = g_a * (o_a @ w_o_a[l]) + g_b * (o_b @ w_o_b[l])
        x = x + gt2[:, None, :] * (y @ w_out[l])

        h = modulate(rmsnorm(x, g_ffn2[l]), sh3, sc3)
        x = x + 0.5 * gt3[:, None, :] * swiglu(h, w_ffn2_in[l], w_ffn2_down[l])

    return rmsnorm(x, g_final)
```

```python
import contextlib
import numpy as np
import concourse.bass as bass
import concourse.mybir as mybir
from concourse.bass_utils import run_bass_kernel_spmd

F32 = mybir.dt.float32
BF16 = mybir.dt.bfloat16
I32 = mybir.dt.int32
ALU = mybir.AluOpType
AF = mybir.ActivationFunctionType
AX = mybir.AxisListType

D = 1024
S = 8192
DFF = 2816
NT = 512
EPS = 1e-6
NCORES = 8
SOWN = 4096
TOPK = 256
NBIS = 14
NEG = -30000.0
NBLK = 32


_SEMREG = {}


def _semreg(nc):
    return _SEMREG.setdefault(id(nc), {"cnt": {}, "gen": {}, "sem": {}})


class Phase:
    def __init__(self, nc, name):
        self.nc = nc
        self.name = name
        self.ops = []
        self.lw = {}
        self.rd = {}

    def add(self, eng, fn, r=(), w=(), lane=None):
        i = len(self.ops)
        deps = set()
        for k in r:
            if k in self.lw:
                deps.add(self.lw[k])
        for k in w:
            if k in self.lw:
                deps.add(self.lw[k])
            deps.update(self.rd.get(k, {}).values())
        for k in w:
            self.lw[k] = i
            self.rd[k] = {}
        tag = lane if lane is not None else eng
        for k in r:
            self.rd.setdefault(k, {})[tag] = i
        self.ops.append(dict(eng=eng, fn=fn, deps=deps, lane=lane, inc=False))
        return i

    def dma(self, eng, out, in_, r=(), w=(), lane=None, **kw):
        assert lane is not None
        return self.add(eng, lambda e: e.dma_start(out=out, in_=in_, **kw), r, w, lane=lane)

    def mm(self, out, lhsT, rhs, start, stop, r=(), w=()):
        return self.add("pe", lambda e: e.matmul(out, lhsT, rhs, start=start, stop=stop), r, w)

    def act(self, out, in_, func, r=(), w=(), eng="act", **kw):
        return self.add(eng, lambda e: e.activation(out=out, in_=in_, func=func, **kw), r, w)

    def ts(self, eng, out, in0, s1, s2, op0, op1=None, r=(), w=(), **kw):
        if op1 is None:
            return self.add(eng, lambda e: e.tensor_scalar(out=out, in0=in0, scalar1=s1, scalar2=None, op0=op0, **kw), r, w)
        return self.add(eng, lambda e: e.tensor_scalar(out=out, in0=in0, scalar1=s1, scalar2=s2, op0=op0, op1=op1, **kw), r, w)

    def stt(self, eng, out, in0, scalar, in1, op0, op1, r=(), w=()):
        return self.add(eng, lambda e: e.scalar_tensor_tensor(out=out, in0=in0, scalar=scalar, in1=in1, op0=op0, op1=op1), r, w)

    def tt(self, eng, out, in0, in1, op, r=(), w=()):
        return self.add(eng, lambda e: e.tensor_tensor(out=out, in0=in0, in1=in1, op=op), r, w)

    def copy(self, eng, out, in_, r=(), w=()):
        if eng == "act":
            return self.add(eng, lambda e: e.activation(out=out, in_=in_, func=AF.Copy), r, w)
        return self.add(eng, lambda e: e.tensor_copy(out=out, in_=in_), r, w)

    def memset(self, eng, ap, val, w=()):
        return self.add(eng, lambda e: e.memset(ap, val), (), w)

    def emit(self):
        nc = self.nc
        ops = self.ops

        def skip(dop, op):
            return dop["lane"] is None and op["lane"] is None and dop["eng"] == "pe" and op["eng"] == "pe"

        for op in ops:
            for d in op["deps"]:
                if not skip(ops[d], op):
                    ops[d]["inc"] = True
        last_dma = {}
        last_eng = {}
        for i, op in enumerate(ops):
            if op["lane"] is not None:
                op["inc"] = True
                last_dma[op["lane"]] = i
            else:
                last_eng[op["eng"]] = i
        for i in last_eng.values():
            ops[i]["inc"] = True
        reg = _semreg(nc)
        cnt, gen, semh = reg["cnt"], reg["gen"], reg["sem"]
        for op in ops:
            if not op["inc"]:
                continue
            base = ("L", op["lane"]) if op["lane"] is not None else ("E", op["eng"])
            gen[base] = gen.get(base, 0)
            key = base + (gen[base],)
            cnt[key] = cnt.get(key, 0) + (16 if op["lane"] is not None else 1)
            op["sem"] = key
            op["val"] = cnt[key]
            pk = reg.setdefault("prevkey", {})
            if op["lane"] is not None and base in pk and pk[base] != key:
                op["drain"] = (pk[base], cnt[pk[base]])
            pk[base] = key
            if key not in semh:
                semh[key] = nc.alloc_semaphore(name=f"s_{key[0]}_{key[1]}_{key[2]}")
            if cnt[key] >= (512 if op["lane"] is not None else 4000):
                gen[base] += 1
        sems = semh
        if "bar" not in reg:
            reg["bar"] = nc.alloc_semaphore(name="s_phase_barrier")
            reg["barcnt"] = 0
        reg["barcnt"] += 5
        bar, bartarget = reg["bar"], reg["barcnt"]
        with contextlib.ExitStack() as es:
            block = es.enter_context(nc.Block())

            def run(engname):
                def body(e):
                    waited = {}
                    for op in ops:
                        if op["eng"] != engname:
                            continue
                        for d in sorted(op["deps"]):
                            dop = ops[d]
                            if not dop["inc"] or skip(dop, op):
                                continue
                            sk, sv = dop["sem"], dop["val"]
                            if waited.get(sk, 0) < sv:
                                e.wait_ge(sems[sk], sv)
                                waited[sk] = sv
                        if "drain" in op:
                            e.wait_ge(sems[op["drain"][0]], op["drain"][1])
                        ins = op["fn"](e)
                        if op["inc"]:
                            ins.then_inc(sems[op["sem"]], 16 if op["lane"] is not None else 1)
                    if engname == "sp":
                        for lane, i in last_dma.items():
                            op = ops[i]
                            if waited.get(op["sem"], 0) < op["val"]:
                                e.wait_ge(sems[op["sem"]], op["val"])
                                waited[op["sem"]] = op["val"]
                    if engname in last_eng:
                        op = ops[last_eng[engname]]
                        e.wait_ge(sems[op["sem"]], op["val"])
                    e.sem_inc(bar, 1)
                    e.wait_ge(bar, bartarget)
                return body

            block.tensor(run("pe"))
            block.scalar(run("act"))
            block.vector(run("dve"))
            block.gpsimd(run("pool"))
            block.sync(run("sp"))


def rms_rstd(p, ps_bank, sq_ap, nchunk, ones, epsc, rs, width, inv_n, rkeys, tagw):
    for c in range(nchunk):
        p.mm(ps_bank[:, :width], ones, sq_ap(c), c == 0, c == nchunk - 1, r=rkeys + ["ones"], w=[tagw])
    p.act(rs[:, :width], ps_bank[:, :width], AF.Sqrt, r=[tagw, "epsc"], w=["rs"], bias=epsc, scale=inv_n)
    p.add("dve", lambda e: e.reciprocal(out=rs[:, :width], in_=rs[:, :width]), r=["rs"], w=["rs"])


def ffn_phase(nc, name, xsrc, xdst, ntiles, w_in_d, w_dn_d, Ac, Sc, Gc, ps, ones, epsc, gfin=None):
    NJ = DFF // 128
    with (nc.sbuf_tensor(name + "wi", [128, 8, 2 * DFF], BF16) as wi,
          nc.sbuf_tensor(name + "wd", [128, NJ, D], BF16) as wd,
          nc.sbuf_tensor(name + "xs0", [128, 8, NT], F32) as xs0,
          nc.sbuf_tensor(name + "xs1", [128, 8, NT], F32) as xs1,
          nc.sbuf_tensor(name + "hb", [128, 8, NT], BF16) as hb,
          nc.sbuf_tensor(name + "hf", [128, NJ, NT], BF16) as hf,
          nc.sbuf_tensor(name + "sg0", [128, NT], F32) as sg0,
          nc.sbuf_tensor(name + "sg1", [128, NT], F32) as sg1,
          nc.sbuf_tensor(name + "tf", [128, NT], F32) as tf,
          nc.sbuf_tensor(name + "rs", [128, NT], F32) as rs):
        p = Phase(nc, name)
        xs = [xs0, xs1]
        sg = [sg0, sg1]
        w_in_v = w_in_d.rearrange("(c p) n -> p c n", p=128)
        w_dn_v = w_dn_d.rearrange("(c p) n -> p c n", p=128)
        xsrc_v = xsrc.rearrange("(c p) t -> p c t", p=128)
        xdst_v = xdst.rearrange("(c p) t -> p c t", p=128)
        p.dma("sp", xs[0][:], xsrc_v[:, :, 0:NT], w=["xs0"], lane="ld0")
        for c in range(8):
            p.dma("pool", wi[:, c, :], w_in_v[:, c, :], w=["wi"], lane="wi", max_dma_last_dim=4096)
        for c in range(NJ):
            p.dma("pool", wd[:, c, :], w_dn_v[:, c, :], w=["wd"], lane="wd", max_dma_last_dim=4096)
        wik = ["wi" for c in range(8)]
        wdk = ["wd" for c in range(NJ)]
        for t in range(ntiles):
            s = t % 2
            X = xs[s]
            xk = f"xs{s}"
            if t + 1 < ntiles:
                p.dma("sp", xs[1 - s][:], xsrc_v[:, :, (t + 1) * NT:(t + 2) * NT], w=[f"xs{1-s}"], lane=f"ld{1-s}")
            p.act(hb[:], X[:], AF.Square, r=[xk], w=["hb"])
            rms_rstd(p, ps[0], lambda c: hb[:, c, :], 8, ones, epsc, rs, NT, 1.0 / D, ["hb"], "ps0")
            for c in range(8):
                p.stt("dve", tf[:], X[:, c, :], Ac[:, c:c + 1], rs[:], ALU.mult, ALU.mult, r=[xk, "rs"], w=["tf"])
                p.ts("dve", hb[:, c, :], tf[:], Sc[:, c:c + 1], None, ALU.add, r=["tf"], w=["hb"])
            for j in range(NJ):
                pg, pu = ps[1 + 2 * (j % 2)], ps[2 + 2 * (j % 2)]
                kg, ku = f"ps{1 + 2 * (j % 2)}", f"ps{2 + 2 * (j % 2)}"
                for c in range(8):
                    p.mm(pg[:], wi[:, c, j * 128:(j + 1) * 128], hb[:, c, :], c == 0, c == 7, r=["hb", wik[c]], w=[kg])
                for c in range(8):
                    p.mm(pu[:], wi[:, c, DFF + j * 128:DFF + (j + 1) * 128], hb[:, c, :], c == 0, c == 7, r=["hb", wik[c]], w=[ku])
                p.act(sg[j % 2][:], pg[:], AF.Silu, r=[kg], w=[f"sg{j%2}"])
                p.tt("dve", hf[:, j, :], pu[:], sg[j % 2][:], ALU.mult, r=[ku, f"sg{j%2}"], w=[f"hf{j}"])
            for m in range(8):
                po, ko = ps[5 + m % 2], f"ps{5 + m % 2}"
                for j in range(NJ):
                    p.mm(po[:], wd[:, j, m * 128:(m + 1) * 128], hf[:, j, :], j == 0, j == NJ - 1, r=[f"hf{j}", wdk[j]], w=[ko])
                p.stt("dve", X[:, m, :], po[:], Gc[:, m:m + 1], X[:, m, :], ALU.mult, ALU.add, r=[ko, xk], w=[xk])
            if gfin is not None:
                p.act(hb[:], X[:], AF.Square, r=[xk], w=["hb"])
                rms_rstd(p, ps[0], lambda c: hb[:, c, :], 8, ones, epsc, rs, NT, 1.0 / D, ["hb"], "ps0")
                for c in range(8):
                    p.stt("dve", X[:, c, :], X[:, c, :], gfin[:, c:c + 1], rs[:], ALU.mult, ALU.mult, r=[xk, "rs"], w=[xk])
            p.dma("sp", xdst_v[:, :, t * NT:(t + 1) * NT], X[:], r=[xk], lane=f"st{s}")
        p.emit()


def setup_phase(nc, cvec, w_ada, b_ada, gvecs, modT, sc8, ones, epsc, ident_d, identb, ps, derived):
    with (nc.sbuf_tensor("wada0", [128, 8, 1024], BF16) as wa0,
          nc.sbuf_tensor("wada1", [128, 8, 1024], BF16) as wa1,
          nc.sbuf_tensor("cTs", [128, 8], F32) as cT,
          nc.sbuf_tensor("cTb", [128, 8], BF16) as cTb,
          nc.sbuf_tensor("bT", [128, 72], F32) as bT,
          nc.sbuf_tensor("gTs", [128, 3, 8], F32) as gT):
        p = Phase(nc, "setup")
        wa = [wa0, wa1]
        p.memset("dve", ones, 1.0, w=["ones"])
        p.memset("dve", epsc, EPS, w=["epsc"])
        p.dma("pool", identb, ident_d, w=["ident"], lane="ident")
        p.dma("sp", cT[:], cvec, w=["cT"], lane="c")
        p.dma("sp", bT[:], b_ada, w=["bT"], lane="b")
        for i, g in enumerate(gvecs):
            p.dma("sp", gT[:, i, :], g, w=[f"g{i}"], lane=f"g{i}")
        p.act(cTb[:], cT[:], AF.Silu, r=["cT"], w=["cTb"])
        wv = w_ada.rearrange("(c p) n -> p c n", p=128)
        for g in range(9):
            s = g % 2
            for c in range(8):
                p.dma("pool", wa[s][:, c, :], wv[:, c, g * 1024:(g + 1) * 1024], w=[f"wa{s}"], lane=f"wa{s}",
                      max_dma_last_dim=4096)
            for m in range(8):
                col = g * 8 + m
                for c in range(8):
                    p.mm(ps[0][:, col:col + 1], wa[s][:, c, m * 128:(m + 1) * 128], cTb[:, c:c + 1], c == 0, c == 7,
                         r=[f"wa{s}", "cTb"], w=["ps0"])
        p.tt("dve", modT, ps[0][:, 0:72], bT[:], ALU.add, r=["ps0", "bT"], w=["modT"])
        A1, S1, G1, A2, S2, G2, A3, S3, G3 = derived
        for (A, Sh, G, base, gi, gm) in ((A1, S1, G1, 0, 0, 0.5), (A2, S2, G2, 24, 1, 1.0), (A3, S3, G3, 48, 2, 0.5)):
            p.stt("dve", A, modT[:, base + 8:base + 16], 1.0, gT[:, gi, :], ALU.add, ALU.mult, r=["modT", f"g{gi}"], w=["drv"])
            p.copy("dve", Sh, modT[:, base:base + 8], r=["modT"], w=["drv"])
            p.ts("dve", G, modT[:, base + 16:base + 24], gm, None, ALU.mult, r=["modT"], w=["drv"])
        p.emit()


TWO_PI = 6.283185307179586
CW1 = 6.28125
CW2 = TWO_PI - CW1
MAGIC = 12582912.0
KW = 640
QA0, QI0, CQ0, GT0, WI0, WTOT = 640, 1152, 1664, 2048, 4096, 4104
TH = [(-90, 14), (-63, 13), (-45, 12), (-31, 11), (-22, 10), (-15, 9), (-11, 8), (-7, 7), (-6, 6), (-5, 5), (-4, 4),
      (-3, 3), (-2, 2), (-1, 1), (0, 0), (1, 17), (2, 18), (3, 19), (4, 20), (5, 21), (6, 22), (7, 23), (8, 24),
      (12, 25), (16, 26), (23, 27), (32, 28), (46, 29), (64, 30), (91, 31)]


class Banks:
    def __init__(self, ps, ids):
        self.ps, self.ids, self.i = ps, ids, 0

    def get(self):
        b = self.ids[self.i % len(self.ids)]
        self.i += 1
        return self.ps[b], f"ps{b}"


def proj_phase(nc, x1T, g, ps, ones, epsc, A2, S2):
    import os
    NTL = int(os.environ.get("P2TILES", str(S // NT)))
    QSIDE = os.environ.get("P2Q", "1") == "1"
    with contextlib.ExitStack() as es:
        sb = lambda n, s, d=F32: es.enter_context(nc.sbuf_tensor("p2" + n, list(s), d))
        win = sb("win", [128, 8, WTOT], BF16)
        wuk = sb("wuk", [128, 2, 512], BF16); wuv = sb("wuv", [128, 2, 512], BF16)
        wuq = sb("wuq", [128, 3, 768], BF16); wuqs = sb("wuqs", [128, 3, 768], BF16)
        gck = sb("gck", [128, 2]); gcq = sb("gcq", [128, 3])
        frq = sb("frq", [128, 1]); sgn = sb("sgn", [128, 1])
        xs = [sb("xs0", [128, 8, NT]), sb("xs1", [128, 8, NT])]
        hb = sb("hb", [128, 8, NT], BF16)
        tf = sb("tf", [128, NT]); rs = sb("rs", [128, NT])
        sq = sb("sq", [128, 3, NT], BF16)
        cn = sb("cn", [128, 3, NT], BF16)
        ev = [sb(f"ev{i}", [128, NT], BF16) for i in range(4)]
        gs = [sb(f"gs{i}", [128, NT]) for i in range(2)]
        vbs = [sb(f"vbs{i}", [128, 8, 65], BF16) for i in range(2)]
        vas = sb("vas", [128, 4, 65], BF16)
        wis = sb("wis", [128, 4, 8])
        posi = sb("posi", [128, NT], I32)
        rp = [sb(f"rp{i}", [128, NT]) for i in range(6)]
        Ct = sb("Ct", [128, NT]); St = sb("St", [128, NT])
        kpe = sb("kpe", [128, NT], BF16)
        qbs = [sb(f"qbs{i}", [128, NT], BF16) for i in range(2)]
        p = Phase(nc, "p2")
        bk = Banks(ps, [1, 2, 3, 4, 5, 6, 7])
        R = slice(64, 96)
        x1v = x1T.rearrange("(c p) t -> p c t", p=128)
        p.dma("sp", xs[0][:], x1v[:, :, 0:NT], w=["xs0"], lane="ld0")
        wv = g["winP"].rearrange("(c p) n -> p c n", p=128)
        for c in range(8):
            p.dma("pool", win[:, c, :], wv[:, c, :], w=["win"], lane="win", max_dma_last_dim=4096)
        for nm, t_, d_, nch in (("wuk", wuk, g["w_uk"], 2), ("wuv", wuv, g["w_uv"], 2), ("wuq", wuq, g["w_uq"], 3), ("wuqs", wuqs, g["w_uqs"], 3)):
            dv = d_.rearrange("(c p) n -> p c n", p=128)
            for c in range(nch):
                p.dma("pool", t_[:, c, :], dv[:, c, :], w=[nm], lane=nm)
        p.dma("sp", gck[:], g["g_ckv"], w=["gck"], lane="gck")
        p.dma("sp", gcq[:], g["g_cq"], w=["gcq"], lane="gcq")
        p.dma("sp", frq[:], g["freqc"], w=["frq"], lane="frq")
        p.dma("sp", sgn[:], g["sgnc"], w=["sgn"], lane="sgn")
        for i in range(2):
            p.memset("dve", vbs[i][:], 1.0, w=[f"vbs{i}"])
        p.memset("dve", vas[:], 1.0, w=["vas"])
        wk = ["win" for c in range(8)]
        evi = [0]

        def evac(src, rows, kb_, scale=None, eng=None):
            i = evi[0] % 4
            evi[0] += 1
            e = eng or ("act" if i % 2 == 0 else "dve")
            if scale is None and e == "dve":
                p.copy("dve", ev[i][rows, :], src[rows, :], r=[kb_], w=[f"ev{i}"])
            elif e == "dve":
                p.ts("dve", ev[i][rows, :], src[rows, :], scale, None, ALU.mult, r=[kb_], w=[f"ev{i}"])
            else:
                p.act(ev[i][rows, :], src[rows, :], AF.Copy, r=[kb_], w=[f"ev{i}"], scale=(1.0 if scale is None else scale))
            return ev[i], f"ev{i}"

        def colmm(col0, m):
            b, kb_ = bk.get()
            for c in range(8):
                p.mm(b[0:m, :], win[:, c, col0:col0 + m], hb[:, c, :], c == 0, c == 7, r=["hb", wk[c]], w=[kb_])
            return b, kb_

        for t in range(NTL):
            s = t % 2
            X, xk = xs[s], f"xs{s}"
            T0 = t * NT
            if t + 1 < NTL:
                p.dma("sp", xs[1 - s][:], x1v[:, :, (t + 1) * NT:(t + 2) * NT], w=[f"xs{1-s}"], lane=f"ld{1-s}")
            p.dma("sp", posi[R, :], g["pos32"][:, T0:T0 + NT], w=["posi"], lane="pos")
            p.act(hb[:], X[:], AF.Square, r=[xk], w=["hb"])
            rms_rstd(p, ps[0], lambda c: hb[:, c, :], 8, ones, epsc, rs, NT, 1.0 / D, ["hb"], "ps0")
            for c in range(8):
                p.stt("dve", tf[:], X[:, c, :], A2[:, c:c + 1], rs[:], ALU.mult, ALU.mult, r=[xk, "rs"], w=["tf"])
                p.ts("dve", hb[:, c, :], tf[:], S2[:, c:c + 1], None, ALU.add, r=["tf"], w=["hb"])
            p.copy("dve", rp[0][R, :], posi[R, :], r=["posi"], w=["rp0"])
            p.ts("dve", rp[0][R, :], rp[0][R, :], frq[R, 0:1], None, ALU.mult, r=["rp0", "frq"], w=["rp0"])
            for which, dst in ((0, St), (1, Ct)):
                src = rp[0]
                if which == 1:
                    p.ts("dve", rp[1][R, :], rp[0][R, :], 1.5707963267948966, None, ALU.add, r=["rp0"], w=["rp1"])
                    src = rp[1]
                sk = "rp0" if which == 0 else "rp1"
                p.ts("dve", rp[2][R, :], src[R, :], 1.0 / TWO_PI, None, ALU.mult, r=[sk], w=["rp2"])
                p.ts("dve", rp[3][R, :], rp[2][R, :], MAGIC, None, ALU.add, r=["rp2"], w=["rp3"])
                p.ts("dve", rp[3][R, :], rp[3][R, :], -MAGIC, None, ALU.add, r=["rp3"], w=["rp3"])
                p.stt("dve", rp[4][R, :], rp[3][R, :], -CW1, src[R, :], ALU.mult, ALU.add, r=["rp3", sk], w=["rp4"])
                p.stt("dve", rp[4][R, :], rp[3][R, :], -CW2, rp[4][R, :], ALU.mult, ALU.add, r=["rp3", "rp4"], w=["rp4"])
                if which == 0:
                    p.act(dst[R, :], rp[4][R, :], AF.Sin, r=["rp4", "sgn"], w=["St"], scale=sgn[R, 0:1])
                else:
                    p.act(dst[R, :], rp[4][R, :], AF.Sin, r=["rp4"], w=["Ct"])

            def rope(pa, ka, pb, kb2, outap, okey, scale):
                p.stt("dve", rp[5][R, :], pa[R, :], scale, Ct[R, :], ALU.mult, ALU.mult, r=[ka, "Ct"], w=["rp5"])
                p.stt("dve", rp[2][R, :], pb[R, :], scale, St[R, :], ALU.mult, ALU.mult, r=[kb2, "St"], w=["rp2"])
                p.tt("dve", outap[R, :], rp[5][R, :], rp[2][R, :], ALU.add, r=["rp5", "rp2"], w=[okey])

            b, kb_ = colmm(0, 128)
            e_, ek = evac(b, slice(0, 128), kb_)
            p.dma("sp", g["kaT"][:, T0:T0 + NT], e_[0:64, :], r=[ek], lane=ek)
            p.dma("sp", g["kiT"][:, T0:T0 + NT], e_[64:128, :], r=[ek], lane=ek)
            cb = [colmm(128, 128), colmm(256, 128)]
            for c2 in range(2):
                p.act(sq[:, c2, :], cb[c2][0][:], AF.Square, r=[cb[c2][1]], w=["sq"])
            rms_rstd(p, ps[0], lambda c: sq[:, c, :], 2, ones, epsc, rs, NT, 1.0 / 256, ["sq"], "ps0")
            for c2 in range(2):
                p.stt("dve", cn[:, c2, :], cb[c2][0][:], gck[:, c2:c2 + 1], rs[:], ALU.mult, ALU.mult, r=[cb[c2][1], "rs", "gck"], w=["cn"])
            for a in range(4):
                b, kb_ = bk.get()
                for c2 in range(2):
                    p.mm(b[:], wuk[:, c2, a * 128:(a + 1) * 128], cn[:, c2, :], c2 == 0, c2 == 1, r=["cn", "wuk"], w=[kb_])
                e_, ek = evac(b, slice(0, 128), kb_)
                p.dma("sp", g["kbT"][2 * a, 0:64, T0:T0 + NT], e_[0:64, :], r=[ek], lane=ek)
                p.dma("sp", g["kbT"][2 * a + 1, 0:64, T0:T0 + NT], e_[64:128, :], r=[ek], lane=ek)
            for sub in range(4):
                b, kb_ = bk.get()
                for c2 in range(2):
                    p.mm(b[:], cn[:, c2, sub * 128:(sub + 1) * 128], wuv[:, c2, :], c2 == 0, c2 == 1, r=["cn", "wuv"], w=[kb_])
                v = vbs[sub % 2]
                p.copy("dve", v[:, :, 0:64], b[:].rearrange("p (h d) -> p h d", h=8), r=[kb_], w=[f"vbs{sub%2}"])
                r0 = T0 + sub * 128
                p.dma("sp", g["vb"][r0:r0 + 128, :, :], v[:], r=[f"vbs{sub%2}"], lane=f"vb{sub%2}")
            ba, ka = colmm(384, 96)
            bb, kb2 = colmm(480, 96)
            rope(ba, ka, bb, kb2, kpe, "kpe", 1.0)
            for h in range(8):
                p.dma("sp", g["kbT"][h, 64:96, T0:T0 + NT], kpe[R, :], r=["kpe"], lane="kpe")
            b, kb_ = bk.get()
            for sub in range(4):
                for c in range(8):
                    p.mm(b[:, sub * 64:(sub + 1) * 64], hb[:, c, sub * 128:(sub + 1) * 128], win[:, c, 576:640], c == 0, c == 7,
                         r=["hb", wk[c]], w=[kb_])
            p.copy("dve", vas[:, :, 0:64], b[:, 0:256].rearrange("p (s d) -> p s d", s=4), r=[kb_], w=["vas"])
            p.dma("sp", g["va"].rearrange("(n p) d -> p n d", p=128)[:, t * 4:(t + 1) * 4, :], vas[:], r=["vas"], lane="va")
            if t >= SOWN // NT or not QSIDE:
                continue
            for a in range(4):
                b, kb_ = colmm(QA0 + a * 128, 128)
                e_, ek = evac(b, slice(0, 128), kb_, scale=0.125)
                p.dma("sp", g["qaT"][:, 2 * a, T0:T0 + NT], e_[0:64, :], r=[ek], lane=ek)
                p.dma("sp", g["qaT"][:, 2 * a + 1, T0:T0 + NT], e_[64:128, :], r=[ek], lane=ek)
            for a in range(4):
                b, kb_ = colmm(QI0 + a * 128, 128)
                e_, ek = evac(b, slice(0, 128), kb_)
                p.dma("sp", g["qiT"][:, 2 * a, T0:T0 + NT], e_[0:64, :], r=[ek], lane=ek)
                p.dma("sp", g["qiT"][:, 2 * a + 1, T0:T0 + NT], e_[64:128, :], r=[ek], lane=ek)
            b, kb_ = bk.get()
            for sub in range(4):
                for c in range(8):
                    p.mm(b[:, sub * 8:(sub + 1) * 8], hb[:, c, sub * 128:(sub + 1) * 128], win[:, c, WI0:WI0 + 8], c == 0, c == 7,
                         r=["hb", wk[c]], w=[kb_])
            p.ts("dve", wis[:], b[:, 0:32].rearrange("p (s d) -> p s d", s=4), 1.0 / (8.0 * 8.0 ** 0.5), None, ALU.mult, r=[kb_], w=["wis"])
            p.dma("sp", g["widx"].rearrange("(n p) d -> p n d", p=128)[:, t * 4:(t + 1) * 4, :], wis[:], r=["wis"], lane="widx")
            cb = [colmm(CQ0 + c3 * 128, 128) for c3 in range(3)]
            for c3 in range(3):
                p.act(sq[:, c3, :], cb[c3][0][:], AF.Square, r=[cb[c3][1]], w=["sq"])
            rms_rstd(p, ps[0], lambda c: sq[:, c, :], 3, ones, epsc, rs, NT, 1.0 / 384, ["sq"], "ps0")
            for c3 in range(3):
                p.stt("dve", cn[:, c3, :], cb[c3][0][:], gcq[:, c3:c3 + 1], rs[:], ALU.mult, ALU.mult, r=[cb[c3][1], "rs", "gcq"], w=["cn"])
            s96 = 96.0 ** -0.5
            for h in range(8):
                ba, ka = bk.get()
                for c3 in range(3):
                    p.mm(ba[0:96, :], wuq[:, c3, h * 96:(h + 1) * 96], cn[:, c3, :], c3 == 0, c3 == 2, r=["cn", "wuq"], w=[ka])
                bb, kb2 = bk.get()
                for c3 in range(3):
                    p.mm(bb[0:96, :], wuqs[:, c3, h * 96:(h + 1) * 96], cn[:, c3, :], c3 == 0, c3 == 2, r=["cn", "wuqs"], w=[kb2])
                q_, qk = qbs[h % 2], f"qbs{h%2}"
                p.ts("dve", q_[0:64, :], ba[0:64, :], s96, None, ALU.mult, r=[ka], w=[qk])
                rope(ba, ka, bb, kb2, q_, qk, s96)
                p.dma("sp", g["qbT"][:, h, T0:T0 + NT], q_[0:96, :], r=[qk], lane=f"qb{h%2}")
            for m in range(16):
                b, kb_ = colmm(GT0 + m * 128, 128)
                p.act(gs[m % 2][:], b[:], AF.Sigmoid, r=[kb_], w=[f"gs{m%2}"])
                p.dma("sp", g["gT"][m * 128:(m + 1) * 128, T0:T0 + NT], gs[m % 2][:], r=[f"gs{m%2}"], lane=f"gs{m%2}")
        p.emit()


def attn_phase(nc, g, ps, identb, mode):
    NB = 32
    with contextlib.ExitStack() as es:
        sb = lambda n, s, d=F32: es.enter_context(nc.sbuf_tensor("p3" + mode + n, list(s), d))
        dsa = mode == "dsa"
        SD = S if dsa else 2
        SM = 2 if dsa else S
        kiT = sb("kiT", [64, SD], BF16); kaT = sb("kaT", [64, SD], BF16)
        va = sb("va", [128, 64 if dsa else 1, 65], BF16); vb = sb("vb", [128, 1 if dsa else 64, 8 * 65], BF16)
        kb = [sb(f"kb{i}", [96, SM], BF16) for i in range(2)]
        Isc = sb("Isc", [128, SD]); nm = sb("nm", [128, SD], BF16)
        junk = nm
        I4 = sb("I4", [128, 512], BF16); sel = sb("sel", [65, 64], BF16)
        dhi = sb("dhi", [65, 512], BF16); dlo = sb("dlo", [65, 512], BF16)
        qi = sb("qi", [64, 8, 128], BF16); qa = sb("qa", [64, 8, 128], BF16); wq = sb("wq", [128, 8])
        Dh = sb("Dh", [128, 8, 128], BF16)
        rl = [sb(f"rl{i}", [128, 8, 512 if dsa else 2], BF16) for i in range(2)]
        cmq = sb("cmq", [128, 256])
        pw = sb("pw", [128, NBIS + 1]); hk = sb("hk", [128, NBIS + 1])
        sm = sb("sm", [128, 8])
        pt = [sb(f"pt{i}", [128, 1024], BF16) for i in range(2)]
        osb = sb("osb", [65, 1024]); rden = sb("rden", [64, 1024])
        oo = sb("oo", [64, 1024], BF16)
        qb = sb("qb", [96, 8, 2 if dsa else 512], BF16)
        cmk = sb("cmk", [128, 8, 2 if dsa else 512], BF16)
        Bt = sb("Bt", [128, 3, 1024], BF16)
        rb = sb("rb", [128, 32, 8]); dl = sb("dl", [128, len(TH), 8])
        pqi = sb("pqi", [128, 128], I32); pki = sb("pki", [128, 3], I32)
        pqf = sb("pqf", [128, 128]); pkf = sb("pkf", [128, 3])
        rel = sb("rel", [128, 128]); ind = sb("ind", [128, 128]); bacc = sb("bacc", [128, 8, 128])
        p = Phase(nc, "p3" + mode)
        if dsa:
            p.dma("sp", kiT[:], g["kiT"], w=["kiT"], lane="kiT")
            p.dma("sp", kaT[:], g["kaT"], w=["kaT"], lane="kaT")
            vav = g["va"].rearrange("(n p) d -> p n d", p=128)
            for q4 in range(16):
                p.dma("sp", va[:, q4 * 4:(q4 + 1) * 4, :], vav[:, q4 * 4:(q4 + 1) * 4, :], w=["va"], lane="va")
        else:
            vbv = g["vb"].rearrange("(n p) h d -> p n (h d)", p=128)
            for q4 in range(16):
                p.dma("sp", vb[:, q4 * 4:(q4 + 1) * 4, :], vbv[:, q4 * 4:(q4 + 1) * 4, :], w=["vb"], lane="vbl")
        for q4 in range(4):
            p.copy("dve", I4[:, q4 * 128:(q4 + 1) * 128], identb, r=["ident"], w=["I4"])
        p.memset("dve", sel[:], 0.0, w=["sel"])
        p.memset("dve", sel[64:65, :], 1.0, w=["sel"])
        for k in range(NBIS + 1):
            p.memset("dve", pw[:, k:k + 1], 2.0 ** -(k + 1), w=["pw"])
        if dsa:
            p.dma("sp", rb[:], g["rb128"], w=["rb"], lane="rb")
            p.dma("sp", pqi[:], g["posq_bc"], w=["pqi"], lane="pqi")
            p.dma("sp", pki[:], g["posk_col"], w=["pki"], lane="pki")
            p.copy("dve", pqf[:], pqi[:], r=["pqi"], w=["pqf"])
            p.copy("dve", pkf[:], pki[:], r=["pki"], w=["pkf"])
            prev = 15
            for j, (th, nb_) in enumerate(TH):
                p.tt("dve", dl[:, j, :], rb[:, nb_, :], rb[:, prev, :], ALU.subtract, r=["rb"], w=["dl"])
                prev = nb_
            for ty in range(3):
                p.ts("dve", rel[:], pqf[:], pkf[:, ty:ty + 1], -1.0, ALU.subtract, ALU.mult, r=["pqf", "pkf"], w=["rel"])
                p.memset("pool", bacc[:], 0.0, w=["bacc"] + [f"bacc{h}" for h in range(8)])
                for j, (th, nb_) in enumerate(TH):
                    p.ts("dve", ind[:], rel[:], float(th), None, ALU.is_ge, r=["rel"], w=["ind"])
                    for h in range(8):
                        e = "dve"
                        p.stt(e, bacc[:, h, :], ind[:], dl[:, j, h:h + 1], bacc[:, h, :], ALU.mult, ALU.add, r=["ind", "dl", f"bacc{h}"], w=[f"bacc{h}"])
                p.copy("dve", Bt[:, ty, :], bacc[:].rearrange("p h q -> p (h q)"), r=["bacc"] + [f"bacc{h}" for h in range(8)], w=["Bt", "bacc"])

        bkI = Banks(ps, [0, 1, 2, 3])

        def normalize(accs, width, dst, dkey, dma_fn):
            for hf_, (b, kb_) in enumerate(accs):
                p.copy("act", osb[:, hf_ * 512:(hf_ + 1) * 512], b[0:65, :], r=[kb_], w=["osb"])
            for hf_ in range(len(accs)):
                b, kb_ = ps[6 + hf_ % 2], f"ps{6 + hf_ % 2}"
                p.copy("dve", dhi[:], osb[:, hf_ * 512:(hf_ + 1) * 512], r=["osb"], w=["dhi"])
                p.tt("dve", dlo[:], osb[:, hf_ * 512:(hf_ + 1) * 512], dhi[:], ALU.subtract, r=["osb", "dhi"], w=["dlo"])
                p.mm(b[0:64, :], sel[:], dhi[:], True, False, r=["dhi", "sel"], w=[kb_])
                p.mm(b[0:64, :], sel[:], dlo[:], False, True, r=["dlo", "sel"], w=[kb_])
                p.add("dve", lambda e, b=b, hf_=hf_: e.reciprocal(out=rden[:, hf_ * 512:(hf_ + 1) * 512], in_=b[0:64, :]), r=[kb_], w=["rden"])
            p.tt("dve", oo[:, :width], osb[0:64, :width], rden[:, :width], ALU.mult, r=["osb", "rden"], w=["oo"])
            dma_fn()

        kbcount = [0]

        def dsa_block(i):
            Q0 = i * 128
            nk = 2 * (i + 1)
            blocks = list(range(i + 1)) + list(range(32, 32 + i + 1))
            W = nk * 128
            p.dma("sp", qi[:], g["qiT"][:, :, Q0:Q0 + 128], w=["qi"], lane="qi")
            p.dma("sp", qa[:], g["qaT"][:, :, Q0:Q0 + 128], w=["qa"], lane="qa")
            p.dma("sp", wq[:], g["widx"][Q0:Q0 + 128, :], w=["wq"], lane="wq")
            p.dma("sp", cmq[:], g["cmq"][i], w=["cmq"], lane="cmq")
            for h in range(8):
                p.ts("dve", Dh[:, h, :], identb, wq[:, h:h + 1], None, ALU.mult, r=["ident", "wq"], w=["Dh"])
            groups = []
            for (k0, n) in ((0, (i + 1) * 128), (4096, (i + 1) * 128)):
                o = 0
                while o < n:
                    w_ = min(512, n - o)
                    groups.append((k0 + o, w_))
                    o += w_
            col = 0
            for gi, (k0, w_) in enumerate(groups):
                R_ = rl[gi % 2]
                rk = f"rl{gi%2}"
                for h in range(8):
                    b, kb_ = bkI.get()
                    p.mm(b[:, :w_], qi[:, h, :], kiT[:, k0:k0 + w_], True, True, r=["qi", "kiT"], w=[kb_])
                    if h % 2 == 0:
                        p.act(R_[:, h, :w_], b[:, :w_], AF.Relu, r=[kb_], w=[rk])
                    else:
                        p.ts("dve", R_[:, h, :w_], b[:, :w_], 0.0, None, ALU.max, r=[kb_], w=[rk])
                b, kb_ = ps[4 + gi % 2], f"ps{4 + gi % 2}"
                for h in range(8):
                    p.mm(b[:, :w_], Dh[:, h, :], R_[:, h, :w_], h == 0, h == 7, r=["Dh", rk], w=[kb_])
                p.copy("act", Isc[:, col:col + w_], b[:, :w_], r=[kb_], w=["Isc"])
                col += w_
            p.add("dve", lambda e, W=W: e.tensor_reduce(out=sm[:, 0:1], in_=Isc[:, :W], axis=AX.X, op=ALU.max, apply_absolute_value=True),
                  r=["Isc"], w=["sm"])
            c_own = i * 128
            c_oth = (i + 1) * 128 + i * 128
            p.tt("dve", Isc[:, c_own:c_own + 128], Isc[:, c_own:c_own + 128], cmq[:, 0:128], ALU.add, r=["Isc", "cmq"], w=["Isc"])
            p.tt("dve", Isc[:, c_oth:c_oth + 128], Isc[:, c_oth:c_oth + 128], cmq[:, 128:256], ALU.add, r=["Isc", "cmq"], w=["Isc"])
            p.ts("dve", sm[:, 1:2], sm[:, 0:1], 2.02, 2e-6, ALU.mult, ALU.add, r=["sm"], w=["sm"])
            p.ts("dve", hk[:], pw[:], sm[:, 1:2], None, ALU.mult, r=["pw", "sm"], w=["hk"])
            p.ts("dve", sm[:, 2:3], sm[:, 1:2], 0.0, None, ALU.mult, r=["sm"], w=["sm"])
            for k in range(NBIS):
                p.ts("dve", junk[:, :W], Isc[:, :W], sm[:, 2:3], None, ALU.is_ge, ALU.add, r=["Isc", "sm"], w=["junk", "cnt"], accum_out=sm[:, 3:4])
                p.ts("dve", sm[:, 4:5], sm[:, 3:4], float(TOPK), hk[:, k + 1:k + 2], ALU.is_ge, ALU.mult, r=["cnt", "hk"], w=["tmp"])
                kk = k + 1 if k + 1 < NBIS else k + 1
                p.stt("dve", sm[:, 2:3], sm[:, 4:5], 2.0, sm[:, 2:3], ALU.mult, ALU.add, r=["tmp", "sm"], w=["sm"])
                p.tt("dve", sm[:, 2:3], sm[:, 2:3], hk[:, k + 1:k + 2], ALU.subtract, r=["sm", "hk"], w=["sm"])
            p.ts("dve", nm[:, :W], Isc[:, :W], sm[:, 2:3], NEG, ALU.is_lt, ALU.mult, r=["Isc", "sm"], w=["nm"])
            accs = [(ps[4], "ps4"), (ps[5], "ps5")]
            for c, L in enumerate(blocks):
                near = None
                if L == i:
                    near = 0
                elif L == 32 + i - 1:
                    near = 1
                elif L == 32 + i:
                    near = 2
                P_ = pt[c % 2]
                pk = f"pt{c%2}"
                for hf_ in range(2):
                    b, kb_ = ps[(c % 2) * 2 + hf_], f"ps{(c % 2) * 2 + hf_}"
                    p.mm(b[:], kaT[:, L * 128:(L + 1) * 128], qa[:, hf_ * 4:(hf_ + 1) * 4, :].rearrange("d h q -> d (h q)"), True, False,
                         r=["kaT", "qa"], w=[kb_])
                    p.mm(b[:], nm[:, c * 128:(c + 1) * 128], I4[:], False, near is None, r=["nm", "I4"], w=[kb_])
                    if near is not None:
                        p.mm(b[:], identb, Bt[:, near, hf_ * 512:(hf_ + 1) * 512], False, True, r=["ident", "Bt"], w=[kb_])
                    p.act(P_[:, hf_ * 512:(hf_ + 1) * 512], b[:], AF.Exp, r=[kb_], w=[pk])
                for hf_ in range(2):
                    b, kb_ = accs[hf_]
                    p.mm(b[0:65, :], va[:, L, :], P_[:, hf_ * 512:(hf_ + 1) * 512], c == 0, c == nk - 1, r=["va", pk], w=[kb_])

            def dma_oa(Q0=Q0):
                p.dma("sp", g["oaT"][:, :, Q0:Q0 + 128], oo[:, :].rearrange("d (h q) -> d h q", h=8), r=["oo"], lane="oa")
            normalize(accs, 1024, oo, "oo", dma_oa)

        def mla_tile(j):
            T0 = j * NT
            nb_own = 4 * j + 4
            tblocks = list(range(nb_own)) + list(range(32, 32 + nb_own))
            p.dma("sp", qb[:], g["qbT"][:, :, T0:T0 + NT], w=["qb"], lane="qb")
            p.dma("pool", cmk[:], g["cmk"][j], w=["cmk"], lane="cmk")
            for h in range(8):
                K_ = kb[kbcount[0] % 2]
                kk_ = f"kb{kbcount[0] % 2}"
                kbcount[0] += 1
                p.dma("sp", K_[:, 0:nb_own * 128], g["kbT"][h, :, 0:nb_own * 128], w=[kk_], lane=kk_ + "a")
                p.dma("sp", K_[:, nb_own * 128:2 * nb_own * 128], g["kbT"][h, :, 4096:4096 + nb_own * 128], w=[kk_], lane=kk_ + "b")
                acc = (ps[4 + h % 2], f"ps{4 + h % 2}")
                for c, L in enumerate(tblocks):
                    b, kb_ = ps[c % 4], f"ps{c % 4}"
                    mi = None
                    if 4 * j <= L < 4 * j + 4:
                        mi = L - 4 * j
                    elif 32 + 4 * j <= L < 32 + 4 * j + 4:
                        mi = 4 + L - 32 - 4 * j
                    p.mm(b[:], K_[:, c * 128:(c + 1) * 128], qb[:, h, :], True, mi is None, r=[kk_, "qb"], w=[kb_])
                    if mi is not None:
                        p.mm(b[:], identb, cmk[:, mi, :], False, True, r=["ident", "cmk"], w=[kb_])
                    P_ = pt[c % 2]
                    pk = f"pt{c%2}"
                    p.act(P_[:, 0:512], b[:], AF.Exp, r=[kb_], w=[pk])
                    p.mm(acc[0][0:65, :], vb[:, L, h * 65:(h + 1) * 65], P_[:, 0:512], c == 0, c == len(tblocks) - 1, r=["vb", pk], w=[acc[1]])

                def dma_ob(h=h, T0=T0):
                    p.dma("sp", g["obT"][:, h, T0:T0 + NT], oo[:, 0:512], r=["oo"], lane="ob")
                normalize([acc], 512, oo, "oo", dma_ob)

        for i in range(min(NB, NBLK)):
            if dsa:
                dsa_block(i)
            elif i % 4 == 3:
                mla_tile(i // 4)
        p.emit()


def merge_phase(nc, g, x1T, x2T, ps, G2):
    with contextlib.ExitStack() as es:
        sb = lambda n, s, d=F32: es.enter_context(nc.sbuf_tensor("p4" + n, list(s), d))
        woa = sb("woa", [64, 8, D], BF16); wob = sb("wob", [64, 8, D], BF16); wout = sb("wout", [128, 8, D], BF16)
        oa = sb("oa", [64, 8, NT], BF16); ob = sb("ob", [64, 8, NT], BF16)
        gt = sb("gt", [128, 16, NT]); xs = sb("xs", [128, 8, NT])
        y = sb("y", [128, 8, NT], BF16); t1 = sb("t1", [128, NT]); t2 = sb("t2", [128, NT])
        p = Phase(nc, "p4")
        p.dma("pool", woa[:], g["w_o_a"].rearrange("(h d) n -> d h n", d=64), w=["woa"], lane="woa", max_dma_last_dim=4096)
        p.dma("pool", wob[:], g["w_o_b"].rearrange("(h d) n -> d h n", d=64), w=["wob"], lane="wob", max_dma_last_dim=4096)
        p.dma("pool", wout[:], g["w_out"].rearrange("(c p) n -> p c n", p=128), w=["wout"], lane="wout", max_dma_last_dim=4096)
        x1v = x1T.rearrange("(c p) t -> p c t", p=128)
        x2v = x2T.rearrange("(c p) t -> p c t", p=128)
        gv = g["gT"].rearrange("(c p) t -> p c t", p=128)
        for t in range(SOWN // NT):
            T0 = t * NT
            p.dma("sp", oa[:], g["oaT"][:, :, T0:T0 + NT], w=["oa"], lane="oa")
            p.dma("sp", ob[:], g["obT"][:, :, T0:T0 + NT], w=["ob"], lane="ob")
            p.dma("sp", gt[:], gv[:, :, T0:T0 + NT], w=["gt"], lane="gt")
            p.dma("sp", xs[:], x1v[:, :, T0:T0 + NT], w=["xs"], lane="xs")
            for m in range(8):
                ba, ka = ps[m % 2], f"ps{m%2}"
                bb, kb_ = ps[2 + m % 2], f"ps{2 + m%2}"
                for h in range(8):
                    p.mm(ba[:], woa[:, h, m * 128:(m + 1) * 128], oa[:, h, :], h == 0, h == 7, r=["woa", "oa"], w=[ka])
                for h in range(8):
                    p.mm(bb[:], wob[:, h, m * 128:(m + 1) * 128], ob[:, h, :], h == 0, h == 7, r=["wob", "ob"], w=[kb_])
                p.tt("dve", t1[:], ba[:], gt[:, m, :], ALU.mult, r=[ka, "gt"], w=["t1"])
                p.tt("dve", t2[:], bb[:], gt[:, 8 + m, :], ALU.mult, r=[kb_, "gt"], w=["t2"])
                p.tt("dve", y[:, m, :], t1[:], t2[:], ALU.add, r=["t1", "t2"], w=[f"y{m}"])
            for m in range(8):
                b, kb_ = ps[4 + m % 2], f"ps{4 + m%2}"
                for c in range(8):
                    p.mm(b[:], wout[:, c, m * 128:(m + 1) * 128], y[:, c, :], c == 0, c == 7, r=["wout", f"y{c}"], w=[kb_])
                p.stt("dve", xs[:, m, :], b[:], G2[:, m:m + 1], xs[:, m, :], ALU.mult, ALU.add, r=[kb_, "xs"], w=["xs"])
            p.dma("sp", x2v[:, :, T0:T0 + NT], xs[:], r=["xs"], lane="st")
        p.emit()


def build(stage=99, debug=False):
    nc = bass.Bass("TRN2", target_bir_lowering=False)
    dt = lambda n, s, d=F32: nc.dram_tensor(n, list(s), d, kind="ExternalInput").ap()
    xT = dt("xT", [D, S])
    cvec = dt("cvec", [128, 8])
    w_ada = dt("w_ada", [D, 9 * D])
    b_ada = dt("b_ada", [128, 72])
    g_ffn1 = dt("g_ffn1", [128, 8]); g_mix = dt("g_mix", [128, 8]); g_ffn2 = dt("g_ffn2", [128, 8]); g_final = dt("g_final", [128, 8])
    w1i = dt("w_ffn1_in", [D, 2 * DFF]); w1d = dt("w_ffn1_down", [DFF, D])
    w2i = dt("w_ffn2_in", [D, 2 * DFF]); w2d = dt("w_ffn2_down", [DFF, D])
    ident_d = dt("ident", [128, 128])
    g = {}
    g["winP"] = dt("winP", [D, WTOT])
    g["w_uk"] = dt("w_uk", [256, 512]); g["w_uv"] = dt("w_uv", [256, 512])
    g["w_uq"] = dt("w_uq", [384, 768]); g["w_uqs"] = dt("w_uqs", [384, 768])
    g["g_ckv"] = dt("g_ckv", [128, 2]); g["g_cq"] = dt("g_cq", [128, 3])
    g["freqc"] = dt("freqc", [128, 1]); g["sgnc"] = dt("sgnc", [128, 1])
    g["pos32"] = dt("pos32", [32, S], I32)
    g["rb128"] = dt("rb128", [128, 32, 8])
    g["posq_bc"] = dt("posq_bc", [128, 128], I32); g["posk_col"] = dt("posk_col", [128, 3], I32)
    g["cmq"] = dt("cmq", [32, 128, 256]); g["cmk"] = dt("cmk", [8, 128, 8, 512])
    g["w_o_a"] = dt("w_o_a", [512, D]); g["w_o_b"] = dt("w_o_b", [512, D]); g["w_out"] = dt("w_out", [D, D])
    outT = nc.dram_tensor("outT", [D, SOWN], F32, kind="ExternalOutput").ap()
    dbgset = set(debug.split(",")) if debug else set()
    it = lambda n, s, d=F32: nc.dram_tensor(n, list(s), d, kind=("ExternalOutput" if n in dbgset else "Internal")).ap()
    x1T = it("x1T", [D, S]); x2T = it("x2T", [D, SOWN])
    g["kaT"] = it("kaT", [64, S], BF16); g["kiT"] = it("kiT", [64, S], BF16)
    g["kbT"] = it("kbT", [8, 96, S], BF16); g["vb"] = it("vb", [S, 8, 65], BF16); g["va"] = it("va", [S, 65], BF16)
    g["qaT"] = it("qaT", [64, 8, SOWN], BF16); g["qiT"] = it("qiT", [64, 8, SOWN], BF16)
    g["widx"] = it("widx", [SOWN, 8]); g["qbT"] = it("qbT", [96, 8, SOWN], BF16)
    g["gT"] = it("gT", [2048, SOWN])
    g["oaT"] = it("oaT", [64, 8, SOWN], BF16); g["obT"] = it("obT", [64, 8, SOWN], BF16)

    with contextlib.ExitStack() as es:
        ps = [es.enter_context(nc.psum_tensor(f"psb{i}", [128, 512], F32)) for i in range(8)]
        sb = lambda n, s, d=F32: es.enter_context(nc.sbuf_tensor(n, list(s), d))
        ones_t = sb("ones", [128, 128], BF16); ones = ones_t[:]
        epsc_t = sb("epsc", [128, 1]); epsc = epsc_t[:]
        identb_t = sb("identb", [128, 128], BF16); identb = identb_t[:]
        modT_t = sb("modT", [128, 72]); modT = modT_t[:]
        drv_t = sb("drv", [128, 10, 8])
        derived = [drv_t[:, i, :] for i in range(9)]
        gfin = drv_t[:, 9, :]
        A1, S1, G1, A2, S2, G2, A3, S3, G3 = derived

        setup_phase(nc, cvec, w_ada, b_ada, [g_ffn1, g_mix, g_ffn2], modT, None, ones, epsc, ident_d, identb, ps, derived)
        pp = Phase(nc, "gfin")
        pp.dma("sp", gfin, g_final, w=["gf"], lane="gf")
        pp.emit()
        if stage == 0:
            return nc
        if stage == 20:
            proj_phase(nc, x1T, g, ps, ones, epsc, A2, S2)
            return nc
        if stage in (30, 31):
            import os
            global NBLK
            NBLK = int(os.environ.get("NBLK", "32"))
            attn_phase(nc, g, ps, identb, "dsa" if stage == 30 else "mla")
            return nc
        ffn_phase(nc, "f1", xT, x1T, S // NT, w1i, w1d, A1, S1, G1, ps, ones, epsc)
        if stage == 1:
            ffn_phase(nc, "f2", x1T[:, 0:SOWN], outT, SOWN // NT, w2i, w2d, A3, S3, G3, ps, ones, epsc, gfin=gfin)
            return nc
        proj_phase(nc, x1T, g, ps, ones, epsc, A2, S2)
        if stage == 2:
            return nc
        attn_phase(nc, g, ps, identb, "dsa")
        if stage == 3:
            return nc
        attn_phase(nc, g, ps, identb, "mla")
        if stage == 4:
            return nc
        merge_phase(nc, g, x1T, x2T, ps, G2)
        ffn_phase(nc, "f2", x2T, outT, SOWN // NT, w2i, w2d, A3, S3, G3, ps, ones, epsc, gfin=gfin)
    return nc


def local_perm(p):
    own = np.arange(32) * 2 + p
    oth = np.arange(32) * 2 + 1 - p
    blocks = np.concatenate([own, oth])
    return (blocks[:, None] * 128 + np.arange(128)[None, :]).reshape(-1)


def pm(v):
    v = np.asarray(v, np.float32)
    return np.ascontiguousarray(v.reshape(-1, 128).T)


FREQ16 = [1.0, 0.5623413324356079, 0.3162277638912201, 0.17782793939113617, 0.10000000149011612, 0.05623413249850273,
          0.03162277489900589, 0.017782794311642647, 0.009999999776482582, 0.005623413249850273, 0.003162277629598975,
          0.0017782794311642647, 0.0010000000474974513, 0.000562341301701963, 0.0003162277571391314, 0.00017782794020604342]


def host_consts(p):
    perm = local_perm(p)
    lim = (perm // 64 + 1) * 64
    cmq = np.zeros((32, 128, 256), np.float32)
    for i in range(32):
        ql = lim[i * 128:(i + 1) * 128][:, None]
        for half, kbk in ((0, i), (1, 32 + i)):
            kt = perm[kbk * 128:(kbk + 1) * 128][None, :]
            cmq[i, :, half * 128:(half + 1) * 128] = np.where(kt < ql, 0.0, -1e30)
    cmk = np.zeros((8, 128, 8, 512), np.float32)
    for j in range(8):
        ql = lim[j * 512:(j + 1) * 512][None, :]
        for mi in range(8):
            kbk = 4 * j + mi if mi < 4 else 32 + 4 * j + (mi - 4)
            kt = perm[kbk * 128:(kbk + 1) * 128][:, None]
            cmk[j, :, mi, :] = np.where(kt < ql, 0.0, NEG)
    freqc = np.zeros((128, 1), np.float32)
    sgnc = np.zeros((128, 1), np.float32)
    for r in range(32):
        freqc[64 + r, 0] = FREQ16[r % 16]
        sgnc[64 + r, 0] = -1.0 if r < 16 else 1.0
    return perm, cmq, cmk, freqc, sgnc


def kernel(**inputs):
    import os
    stage = int(os.environ.get("KSTAGE", "99"))
    debug = os.environ.get("KDEBUG", "")
    f = lambda a: np.ascontiguousarray(np.asarray(a, np.float32))
    x = np.asarray(inputs["x"], np.float32)
    w_in = np.asarray(inputs["w_in"][0], np.float32)
    q_a, k_a, v_a = w_in[:, 0:512], w_in[:, 512:576], w_in[:, 576:640]
    q_i, k_i, w_i = w_in[:, 640:1152], w_in[:, 1152:1216], w_in[:, 1216:1224]
    c_q, c_kv, k_r, gts = w_in[:, 1224:1608], w_in[:, 1608:1864], w_in[:, 1864:1896], w_in[:, 1896:3944]
    k_rs = np.concatenate([k_r[:, 16:32], k_r[:, 0:16]], axis=1)
    winP = np.ascontiguousarray(np.concatenate([k_a, k_i, c_kv, k_a, k_r, k_a, k_rs, v_a, q_a, q_i, c_q, gts, w_i], axis=1))
    assert winP.shape[1] == WTOT
    w_uq = np.asarray(inputs["w_uq"][0], np.float32)
    w_uqs = w_uq.copy().reshape(384, 8, 96)
    w_uqs[:, :, 64:80], w_uqs[:, :, 80:96] = w_uq.reshape(384, 8, 96)[:, :, 80:96], w_uq.reshape(384, 8, 96)[:, :, 64:80]
    w_uqs = np.ascontiguousarray(w_uqs.reshape(384, 768))
    nc = build(stage, debug)
    in_maps = []
    perms = []
    pos_all = np.asarray(inputs["positions"], np.int32)
    rb128 = np.ascontiguousarray(np.broadcast_to(f(inputs["rel_bias"])[None], (128, 32, 8)))
    consts = [host_consts(0), host_consts(1)]
    for core in range(NCORES):
        b, p = core // 2, core % 2
        perm, cmq, cmk, freqc, sgnc = consts[p]
        perms.append(perm)
        posl = pos_all[b][perm]
        m = {
            "xT": np.ascontiguousarray(x[b][perm].T),
            "cvec": pm(inputs["c"][b]),
            "w_ada": f(inputs["w_ada"][0]),
            "b_ada": pm(inputs["b_ada"][0]),
            "g_ffn1": pm(inputs["g_ffn1"][0]),
            "g_mix": pm(inputs["g_mix"][0]),
            "g_ffn2": pm(inputs["g_ffn2"][0]),
            "g_final": pm(inputs["g_final"]),
            "w_ffn1_in": f(inputs["w_ffn1_in"][0]),
            "w_ffn1_down": f(inputs["w_ffn1_down"][0]),
            "w_ffn2_in": f(inputs["w_ffn2_in"][0]),
            "w_ffn2_down": f(inputs["w_ffn2_down"][0]),
            "ident": np.eye(128, dtype=np.float32),
            "winP": winP, "w_uk": f(inputs["w_uk"][0]), "w_uv": f(inputs["w_uv"][0]), "w_uq": w_uq, "w_uqs": w_uqs,
            "g_ckv": pm(inputs["g_ckv"][0]), "g_cq": pm(inputs["g_cq"][0]),
            "freqc": freqc, "sgnc": sgnc,
            "pos32": np.ascontiguousarray(np.broadcast_to(posl[None], (32, S))),
            "rb128": rb128,
            "posq_bc": np.ascontiguousarray(np.broadcast_to(posl[128:256][None], (128, 128))),
            "posk_col": np.ascontiguousarray(np.stack([posl[128:256], posl[32 * 128:33 * 128], posl[33 * 128:34 * 128]], axis=1)),
            "cmq": cmq, "cmk": cmk,
            "w_o_a": f(inputs["w_o_a"][0]), "w_o_b": f(inputs["w_o_b"][0]), "w_out": f(inputs["w_out"][0]),
        }
        in_maps.append(m)
    res = run_bass_kernel_spmd(nc, in_maps, core_ids=list(range(NCORES)))
    if debug:
        kernel.debug = res.results
        kernel.perms = perms
    out = np.empty((4, S, D), np.float32)
    for core in range(NCORES):
        b = core // 2
        o = res.results[core]["outT"]
        out[b][perms[core][:SOWN]] = o.T
    return out
```

```python
import contextlib
import numpy as np
import concourse.bass as bass
import concourse.mybir as mybir
from concourse.bass_utils import run_bass_kernel_spmd

F32 = mybir.dt.float32
BF16 = mybir.dt.bfloat16
I32 = mybir.dt.int32
ALU = mybir.AluOpType
AF = mybir.ActivationFunctionType
AX = mybir.AxisListType

D = 1024
S = 8192
DFF = 2816
NT = 512
EPS = 1e-6
NCORES = 8
SOWN = 4096
TOPK = 256
NBIS = 12
NEG = -30000.0
NBLK = 32


_SEMREG = {}


def _semreg(nc):
    return _SEMREG.setdefault(id(nc), {"cnt": {}, "gen": {}, "sem": {}})


class Phase:
    def __init__(self, nc, name):
        self.nc = nc
        self.name = name
        self.ops = []
        self.lw = {}
        self.rd = {}

    def add(self, eng, fn, r=(), w=(), lane=None):
        i = len(self.ops)
        deps = set()
        for k in r:
            if k in self.lw:
                deps.add(self.lw[k])
        for k in w:
            if k in self.lw:
                deps.add(self.lw[k])
            deps.update(self.rd.get(k, {}).values())
        for k in w:
            self.lw[k] = i
            self.rd[k] = {}
        tag = lane if lane is not None else eng
        for k in r:
            self.rd.setdefault(k, {})[tag] = i
        self.ops.append(dict(eng=eng, fn=fn, deps=deps, lane=lane, inc=False))
        return i

    def dma(self, eng, out, in_, r=(), w=(), lane=None, **kw):
        assert lane is not None
        return self.add(eng, lambda e: e.dma_start(out=out, in_=in_, **kw), r, w, lane=lane)

    def mm(self, out, lhsT, rhs, start, stop, r=(), w=()):
        return self.add("pe", lambda e: e.matmul(out, lhsT, rhs, start=start, stop=stop), r, w)

    def act(self, out, in_, func, r=(), w=(), eng="act", **kw):
        return self.add(eng, lambda e: e.activation(out=out, in_=in_, func=func, **kw), r, w)

    def ts(self, eng, out, in0, s1, s2, op0, op1=None, r=(), w=(), **kw):
        if op1 is None:
            return self.add(eng, lambda e: e.tensor_scalar(out=out, in0=in0, scalar1=s1, scalar2=None, op0=op0, **kw), r, w)
        return self.add(eng, lambda e: e.tensor_scalar(out=out, in0=in0, scalar1=s1, scalar2=s2, op0=op0, op1=op1, **kw), r, w)

    def stt(self, eng, out, in0, scalar, in1, op0, op1, r=(), w=()):
        return self.add(eng, lambda e: e.scalar_tensor_tensor(out=out, in0=in0, scalar=scalar, in1=in1, op0=op0, op1=op1), r, w)

    def tt(self, eng, out, in0, in1, op, r=(), w=()):
        return self.add(eng, lambda e: e.tensor_tensor(out=out, in0=in0, in1=in1, op=op), r, w)

    def copy(self, eng, out, in_, r=(), w=()):
        if eng == "act":
            return self.add(eng, lambda e: e.activation(out=out, in_=in_, func=AF.Copy), r, w)
        return self.add(eng, lambda e: e.tensor_copy(out=out, in_=in_), r, w)

    def memset(self, eng, ap, val, w=()):
        return self.add(eng, lambda e: e.memset(ap, val), (), w)

    def emit(self):
        nc = self.nc
        ops = self.ops

        def skip(dop, op):
            return dop["lane"] is None and op["lane"] is None and dop["eng"] == "pe" and op["eng"] == "pe"

        for op in ops:
            for d in op["deps"]:
                if not skip(ops[d], op):
                    ops[d]["inc"] = True
        last_dma = {}
        last_eng = {}
        for i, op in enumerate(ops):
            if op["lane"] is not None:
                op["inc"] = True
                last_dma[op["lane"]] = i
            else:
                last_eng[op["eng"]] = i
        for i in last_eng.values():
            ops[i]["inc"] = True
        reg = _semreg(nc)
        cnt, gen, semh = reg["cnt"], reg["gen"], reg["sem"]
        for op in ops:
            if not op["inc"]:
                continue
            base = ("L", op["lane"]) if op["lane"] is not None else ("E", op["eng"])
            gen[base] = gen.get(base, 0)
            key = base + (gen[base],)
            cnt[key] = cnt.get(key, 0) + (16 if op["lane"] is not None else 1)
            op["sem"] = key
            op["val"] = cnt[key]
            pk = reg.setdefault("prevkey", {})
            if op["lane"] is not None and base in pk and pk[base] != key:
                op["drain"] = (pk[base], cnt[pk[base]])
            pk[base] = key
            if key not in semh:
                semh[key] = nc.alloc_semaphore(name=f"s_{key[0]}_{key[1]}_{key[2]}")
            if cnt[key] >= (512 if op["lane"] is not None else 4000):
                gen[base] += 1
        sems = semh
        if "bar" not in reg:
            reg["bar"] = nc.alloc_semaphore(name="s_phase_barrier")
            reg["barcnt"] = 0
        reg["barcnt"] += 5
        bar, bartarget = reg["bar"], reg["barcnt"]
        with contextlib.ExitStack() as es:
            block = es.enter_context(nc.Block())

            def run(engname):
                def body(e):
                    waited = {}
                    for op in ops:
                        if op["eng"] != engname:
                            continue
                        for d in sorted(op["deps"]):
                            dop = ops[d]
                            if not dop["inc"] or skip(dop, op):
                                continue
                            sk, sv = dop["sem"], dop["val"]
                            if waited.get(sk, 0) < sv:
                                e.wait_ge(sems[sk], sv)
                                waited[sk] = sv
                        if "drain" in op:
                            e.wait_ge(sems[op["drain"][0]], op["drain"][1])
                        ins = op["fn"](e)
                        if op["inc"]:
                            ins.then_inc(sems[op["sem"]], 16 if op["lane"] is not None else 1)
                    if engname == "sp":
                        for lane, i in last_dma.items():
                            op = ops[i]
                            if waited.get(op["sem"], 0) < op["val"]:
                                e.wait_ge(sems[op["sem"]], op["val"])
                                waited[op["sem"]] = op["val"]
                    if engname in last_eng:
                        op = ops[last_eng[engname]]
                        e.wait_ge(sems[op["sem"]], op["val"])
                    e.sem_inc(bar, 1)
                    e.wait_ge(bar, bartarget)
                return body

            block.tensor(run("pe"))
            block.scalar(run("act"))
            block.vector(run("dve"))
            block.gpsimd(run("pool"))
            block.sync(run("sp"))


def rms_rstd(p, ps_bank, sq_ap, nchunk, ones, epsc, rs, width, inv_n, rkeys, tagw):
    for c in range(nchunk):
        p.mm(ps_bank[:, :width], ones, sq_ap(c), c == 0, c == nchunk - 1, r=rkeys + ["ones"], w=[tagw])
    p.act(rs[:, :width], ps_bank[:, :width], AF.Sqrt, r=[tagw, "epsc"], w=["rs"], bias=epsc, scale=inv_n)
    p.add("dve", lambda e: e.reciprocal(out=rs[:, :width], in_=rs[:, :width]), r=["rs"], w=["rs"])


def ffn_phase(nc, name, xsrc, xdst, ntiles, w_in_d, w_dn_d, Ac, Sc, Gc, ps, ones, epsc, gfin=None):
    NJ = DFF // 128
    with (nc.sbuf_tensor(name + "wi", [128, 8, 2 * DFF], BF16) as wi,
          nc.sbuf_tensor(name + "wd", [128, NJ, D], BF16) as wd,
          nc.sbuf_tensor(name + "xs0", [128, 8, NT], F32) as xs0,
          nc.sbuf_tensor(name + "xs1", [128, 8, NT], F32) as xs1,
          nc.sbuf_tensor(name + "hb", [128, 8, NT], BF16) as hb,
          nc.sbuf_tensor(name + "hf", [128, NJ, NT], BF16) as hf,
          nc.sbuf_tensor(name + "sg0", [128, NT], F32) as sg0,
          nc.sbuf_tensor(name + "sg1", [128, NT], F32) as sg1,
          nc.sbuf_tensor(name + "tf", [128, NT], F32) as tf,
          nc.sbuf_tensor(name + "rs", [128, NT], F32) as rs):
        p = Phase(nc, name)
        xs = [xs0, xs1]
        sg = [sg0, sg1]
        w_in_v = w_in_d.rearrange("(c p) n -> p c n", p=128)
        w_dn_v = w_dn_d.rearrange("(c p) n -> p c n", p=128)
        xsrc_v = xsrc.rearrange("(c p) t -> p c t", p=128)
        xdst_v = xdst.rearrange("(c p) t -> p c t", p=128)
        p.dma("sp", xs[0][:], xsrc_v[:, :, 0:NT], w=["xs0"], lane="ld0")
        for c in range(8):
            p.dma("pool", wi[:, c, :], w_in_v[:, c, :], w=["wi"], lane="wi", max_dma_last_dim=4096)
        for c in range(NJ):
            p.dma("pool", wd[:, c, :], w_dn_v[:, c, :], w=["wd"], lane="wd", max_dma_last_dim=4096)
        wik = ["wi" for c in range(8)]
        wdk = ["wd" for c in range(NJ)]
        for t in range(ntiles):
            s = t % 2
            X = xs[s]
            xk = f"xs{s}"
            if t + 1 < ntiles:
                p.dma("sp", xs[1 - s][:], xsrc_v[:, :, (t + 1) * NT:(t + 2) * NT], w=[f"xs{1-s}"], lane=f"ld{1-s}")
            p.act(hb[:], X[:], AF.Square, r=[xk], w=["hb"])
            rms_rstd(p, ps[0], lambda c: hb[:, c, :], 8, ones, epsc, rs, NT, 1.0 / D, ["hb"], "ps0")
            for c in range(8):
                p.stt("dve", tf[:], X[:, c, :], Ac[:, c:c + 1], rs[:], ALU.mult, ALU.mult, r=[xk, "rs"], w=["tf"])
                p.ts("dve", hb[:, c, :], tf[:], Sc[:, c:c + 1], None, ALU.add, r=["tf"], w=["hb"])
            for j in range(NJ):
                pg, pu = ps[1 + 2 * (j % 2)], ps[2 + 2 * (j % 2)]
                kg, ku = f"ps{1 + 2 * (j % 2)}", f"ps{2 + 2 * (j % 2)}"
                for c in range(8):
                    p.mm(pg[:], wi[:, c, j * 128:(j + 1) * 128], hb[:, c, :], c == 0, c == 7, r=["hb", wik[c]], w=[kg])
                for c in range(8):
                    p.mm(pu[:], wi[:, c, DFF + j * 128:DFF + (j + 1) * 128], hb[:, c, :], c == 0, c == 7, r=["hb", wik[c]], w=[ku])
                p.act(sg[j % 2][:], pg[:], AF.Silu, r=[kg], w=[f"sg{j%2}"])
                p.tt("dve", hf[:, j, :], pu[:], sg[j % 2][:], ALU.mult, r=[ku, f"sg{j%2}"], w=[f"hf{j}"])
            for m in range(8):
                po, ko = ps[5 + m % 2], f"ps{5 + m % 2}"
                for j in range(NJ):
                    p.mm(po[:], wd[:, j, m * 128:(m + 1) * 128], hf[:, j, :], j == 0, j == NJ - 1, r=[f"hf{j}", wdk[j]], w=[ko])
                p.stt("dve", X[:, m, :], po[:], Gc[:, m:m + 1], X[:, m, :], ALU.mult, ALU.add, r=[ko, xk], w=[xk])
            if gfin is not None:
                p.act(hb[:], X[:], AF.Square, r=[xk], w=["hb"])
                rms_rstd(p, ps[0], lambda c: hb[:, c, :], 8, ones, epsc, rs, NT, 1.0 / D, ["hb"], "ps0")
                for c in range(8):
                    p.stt("dve", X[:, c, :], X[:, c, :], gfin[:, c:c + 1], rs[:], ALU.mult, ALU.mult, r=[xk, "rs"], w=[xk])
            p.dma("sp", xdst_v[:, :, t * NT:(t + 1) * NT], X[:], r=[xk], lane=f"st{s}")
        p.emit()


def setup_phase(nc, cvec, w_ada, b_ada, gvecs, modT, sc8, ones, epsc, ident_d, identb, ps, derived):
    with (nc.sbuf_tensor("wada0", [128, 8, 1024], BF16) as wa0,
          nc.sbuf_tensor("wada1", [128, 8, 1024], BF16) as wa1,
          nc.sbuf_tensor("cTs", [128, 8], F32) as cT,
          nc.sbuf_tensor("cTb", [128, 8], BF16) as cTb,
          nc.sbuf_tensor("bT", [128, 72], F32) as bT,
          nc.sbuf_tensor("gTs", [128, 3, 8], F32) as gT):
        p = Phase(nc, "setup")
        wa = [wa0, wa1]
        p.memset("dve", ones, 1.0, w=["ones"])
        p.memset("dve", epsc, EPS, w=["epsc"])
        p.dma("pool", identb, ident_d, w=["ident"], lane="ident")
        p.dma("sp", cT[:], cvec, w=["cT"], lane="c")
        p.dma("sp", bT[:], b_ada, w=["bT"], lane="b")
        for i, g in enumerate(gvecs):
            p.dma("sp", gT[:, i, :], g, w=[f"g{i}"], lane=f"g{i}")
        p.act(cTb[:], cT[:], AF.Silu, r=["cT"], w=["cTb"])
        wv = w_ada.rearrange("(c p) n -> p c n", p=128)
        for g in range(9):
            s = g % 2
            for c in range(8):
                p.dma("pool", wa[s][:, c, :], wv[:, c, g * 1024:(g + 1) * 1024], w=[f"wa{s}"], lane=f"wa{s}",
                      max_dma_last_dim=4096)
            for m in range(8):
                col = g * 8 + m
                for c in range(8):
                    p.mm(ps[0][:, col:col + 1], wa[s][:, c, m * 128:(m + 1) * 128], cTb[:, c:c + 1], c == 0, c == 7,
                         r=[f"wa{s}", "cTb"], w=["ps0"])
        p.tt("dve", modT, ps[0][:, 0:72], bT[:], ALU.add, r=["ps0", "bT"], w=["modT"])
        A1, S1, G1, A2, S2, G2, A3, S3, G3 = derived
        for (A, Sh, G, base, gi, gm) in ((A1, S1, G1, 0, 0, 0.5), (A2, S2, G2, 24, 1, 1.0), (A3, S3, G3, 48, 2, 0.5)):
            p.stt("dve", A, modT[:, base + 8:base + 16], 1.0, gT[:, gi, :], ALU.add, ALU.mult, r=["modT", f"g{gi}"], w=["drv"])
            p.copy("dve", Sh, modT[:, base:base + 8], r=["modT"], w=["drv"])
            p.ts("dve", G, modT[:, base + 16:base + 24], gm, None, ALU.mult, r=["modT"], w=["drv"])
        p.emit()


TWO_PI = 6.283185307179586
CW1 = 6.28125
CW2 = TWO_PI - CW1
MAGIC = 12582912.0
KW = 640
QA0, QI0, CQ0, GT0, WI0, WTOT = 640, 1152, 1664, 2048, 4096, 4104
TH = [(-90, 14), (-63, 13), (-45, 12), (-31, 11), (-22, 10), (-15, 9), (-11, 8), (-7, 7), (-6, 6), (-5, 5), (-4, 4),
      (-3, 3), (-2, 2), (-1, 1), (0, 0), (1, 17), (2, 18), (3, 19), (4, 20), (5, 21), (6, 22), (7, 23), (8, 24),
      (12, 25), (16, 26), (23, 27), (32, 28), (46, 29), (64, 30), (91, 31)]


class Banks:
    def __init__(self, ps, ids):
        self.ps, self.ids, self.i = ps, ids, 0

    def get(self):
        b = self.ids[self.i % len(self.ids)]
        self.i += 1
        return self.ps[b], f"ps{b}"


def proj_phase(nc, x1T, g, ps, ones, epsc, A2, S2):
    import os
    NTL = int(os.environ.get("P2TILES", str(S // NT)))
    QSIDE = os.environ.get("P2Q", "1") == "1"
    with contextlib.ExitStack() as es:
        sb = lambda n, s, d=F32: es.enter_context(nc.sbuf_tensor("p2" + n, list(s), d))
        win = sb("win", [128, 8, WTOT], BF16)
        wuk = sb("wuk", [128, 2, 512], BF16); wuv = sb("wuv", [128, 2, 512], BF16)
        wuq = sb("wuq", [128, 3, 768], BF16); wuqs = sb("wuqs", [128, 3, 768], BF16)
        gck = sb("gck", [128, 2]); gcq = sb("gcq", [128, 3])
        frq = sb("frq", [128, 1]); sgn = sb("sgn", [128, 1])
        xs = [sb("xs0", [128, 8, NT]), sb("xs1", [128, 8, NT])]
        hb = sb("hb", [128, 8, NT], BF16)
        tf = sb("tf", [128, NT]); rs = sb("rs", [128, NT])
        sq = sb("sq", [128, 3, NT], BF16)
        cn = sb("cn", [128, 3, NT], BF16)
        ev = [sb(f"ev{i}", [128, NT], BF16) for i in range(4)]
        gs = [sb(f"gs{i}", [128, NT]) for i in range(2)]
        vbs = [sb(f"vbs{i}", [128, 8, 65], BF16) for i in range(2)]
        vas = sb("vas", [128, 4, 65], BF16)
        wis = sb("wis", [128, 4, 8])
        posi = sb("posi", [128, NT], I32)
        rp = [sb(f"rp{i}", [128, NT]) for i in range(6)]
        Ct = sb("Ct", [128, NT]); St = sb("St", [128, NT])
        kpe = sb("kpe", [128, NT], BF16)
        qbs = [sb(f"qbs{i}", [128, NT], BF16) for i in range(2)]
        p = Phase(nc, "p2")
        bk = Banks(ps, [1, 2, 3, 4, 5, 6, 7])
        R = slice(64, 96)
        x1v = x1T.rearrange("(c p) t -> p c t", p=128)
        p.dma("sp", xs[0][:], x1v[:, :, 0:NT], w=["xs0"], lane="ld0")
        wv = g["winP"].rearrange("(c p) n -> p c n", p=128)
        for c in range(8):
            p.dma("pool", win[:, c, :], wv[:, c, :], w=["win"], lane="win", max_dma_last_dim=4096)
        for nm, t_, d_, nch in (("wuk", wuk, g["w_uk"], 2), ("wuv", wuv, g["w_uv"], 2), ("wuq", wuq, g["w_uq"], 3), ("wuqs", wuqs, g["w_uqs"], 3)):
            dv = d_.rearrange("(c p) n -> p c n", p=128)
            for c in range(nch):
                p.dma("pool", t_[:, c, :], dv[:, c, :], w=[nm], lane=nm)
        p.dma("sp", gck[:], g["g_ckv"], w=["gck"], lane="gck")
        p.dma("sp", gcq[:], g["g_cq"], w=["gcq"], lane="gcq")
        p.dma("sp", frq[:], g["freqc"], w=["frq"], lane="frq")
        p.dma("sp", sgn[:], g["sgnc"], w=["sgn"], lane="sgn")
        for i in range(2):
            p.memset("dve", vbs[i][:], 1.0, w=[f"vbs{i}"])
        p.memset("dve", vas[:], 1.0, w=["vas"])
        wk = ["win" for c in range(8)]
        evi = [0]

        def evac(src, rows, kb_, scale=None, eng=None):
            i = evi[0] % 4
            evi[0] += 1
            e = eng or ("act" if i % 2 == 0 else "dve")
            if scale is None and e == "dve":
                p.copy("dve", ev[i][rows, :], src[rows, :], r=[kb_], w=[f"ev{i}"])
            elif e == "dve":
                p.ts("dve", ev[i][rows, :], src[rows, :], scale, None, ALU.mult, r=[kb_], w=[f"ev{i}"])
            else:
                p.act(ev[i][rows, :], src[rows, :], AF.Copy, r=[kb_], w=[f"ev{i}"], scale=(1.0 if scale is None else scale))
            return ev[i], f"ev{i}"

        def colmm(col0, m):
            b, kb_ = bk.get()
            for c in range(8):
                p.mm(b[0:m, :], win[:, c, col0:col0 + m], hb[:, c, :], c == 0, c == 7, r=["hb", wk[c]], w=[kb_])
            return b, kb_

        for t in range(NTL):
            s = t % 2
            X, xk = xs[s], f"xs{s}"
            T0 = t * NT
            if t + 1 < NTL:
                p.dma("sp", xs[1 - s][:], x1v[:, :, (t + 1) * NT:(t + 2) * NT], w=[f"xs{1-s}"], lane=f"ld{1-s}")
            p.dma("sp", posi[R, :], g["pos32"][:, T0:T0 + NT], w=["posi"], lane="pos")
            p.act(hb[:], X[:], AF.Square, r=[xk], w=["hb"])
            rms_rstd(p, ps[0], lambda c: hb[:, c, :], 8, ones, epsc, rs, NT, 1.0 / D, ["hb"], "ps0")
            for c in range(8):
                p.stt("dve", tf[:], X[:, c, :], A2[:, c:c + 1], rs[:], ALU.mult, ALU.mult, r=[xk, "rs"], w=["tf"])
                p.ts("dve", hb[:, c, :], tf[:], S2[:, c:c + 1], None, ALU.add, r=["tf"], w=["hb"])
            p.copy("dve", rp[0][R, :], posi[R, :], r=["posi"], w=["rp0"])
            p.ts("dve", rp[0][R, :], rp[0][R, :], frq[R, 0:1], None, ALU.mult, r=["rp0", "frq"], w=["rp0"])
            for which, dst in ((0, St), (1, Ct)):
                src = rp[0]
                if which == 1:
                    p.ts("dve", rp[1][R, :], rp[0][R, :], 1.5707963267948966, None, ALU.add, r=["rp0"], w=["rp1"])
                    src = rp[1]
                sk = "rp0" if which == 0 else "rp1"
                p.ts("dve", rp[2][R, :], src[R, :], 1.0 / TWO_PI, None, ALU.mult, r=[sk], w=["rp2"])
                p.ts("dve", rp[3][R, :], rp[2][R, :], MAGIC, None, ALU.add, r=["rp2"], w=["rp3"])
                p.ts("dve", rp[3][R, :], rp[3][R, :], -MAGIC, None, ALU.add, r=["rp3"], w=["rp3"])
                p.stt("dve", rp[4][R, :], rp[3][R, :], -CW1, src[R, :], ALU.mult, ALU.add, r=["rp3", sk], w=["rp4"])
                p.stt("dve", rp[4][R, :], rp[3][R, :], -CW2, rp[4][R, :], ALU.mult, ALU.add, r=["rp3", "rp4"], w=["rp4"])
                if which == 0:
                    p.act(dst[R, :], rp[4][R, :], AF.Sin, r=["rp4", "sgn"], w=["St"], scale=sgn[R, 0:1])
                else:
                    p.act(dst[R, :], rp[4][R, :], AF.Sin, r=["rp4"], w=["Ct"])

            def rope(pa, ka, pb, kb2, outap, okey, scale):
                p.stt("dve", rp[5][R, :], pa[R, :], scale, Ct[R, :], ALU.mult, ALU.mult, r=[ka, "Ct"], w=["rp5"])
                p.stt("dve", rp[2][R, :], pb[R, :], scale, St[R, :], ALU.mult, ALU.mult, r=[kb2, "St"], w=["rp2"])
                p.tt("dve", outap[R, :], rp[5][R, :], rp[2][R, :], ALU.add, r=["rp5", "rp2"], w=[okey])

            b, kb_ = colmm(0, 128)
            e_, ek = evac(b, slice(0, 128), kb_)
            p.dma("sp", g["kaT"][:, T0:T0 + NT], e_[0:64, :], r=[ek], lane=ek)
            p.dma("sp", g["kiT"][:, T0:T0 + NT], e_[64:128, :], r=[ek], lane=ek)
            cb = [colmm(128, 128), colmm(256, 128)]
            for c2 in range(2):
                p.act(sq[:, c2, :], cb[c2][0][:], AF.Square, r=[cb[c2][1]], w=["sq"])
            rms_rstd(p, ps[0], lambda c: sq[:, c, :], 2, ones, epsc, rs, NT, 1.0 / 256, ["sq"], "ps0")
            for c2 in range(2):
                p.stt("dve", cn[:, c2, :], cb[c2][0][:], gck[:, c2:c2 + 1], rs[:], ALU.mult, ALU.mult, r=[cb[c2][1], "rs", "gck"], w=["cn"])
            for a in range(4):
                b, kb_ = bk.get()
                for c2 in range(2):
                    p.mm(b[:], wuk[:, c2, a * 128:(a + 1) * 128], cn[:, c2, :], c2 == 0, c2 == 1, r=["cn", "wuk"], w=[kb_])
                e_, ek = evac(b, slice(0, 128), kb_)
                p.dma("sp", g["kbT"][2 * a, 0:64, T0:T0 + NT], e_[0:64, :], r=[ek], lane=ek)
                p.dma("sp", g["kbT"][2 * a + 1, 0:64, T0:T0 + NT], e_[64:128, :], r=[ek], lane=ek)
            for sub in range(4):
                b, kb_ = bk.get()
                for c2 in range(2):
                    p.mm(b[:], cn[:, c2, sub * 128:(sub + 1) * 128], wuv[:, c2, :], c2 == 0, c2 == 1, r=["cn", "wuv"], w=[kb_])
                v = vbs[sub % 2]
                p.copy("dve", v[:, :, 0:64], b[:].rearrange("p (h d) -> p h d", h=8), r=[kb_], w=[f"vbs{sub%2}"])
                r0 = T0 + sub * 128
                p.dma("sp", g["vb"][r0:r0 + 128, :, :], v[:], r=[f"vbs{sub%2}"], lane=f"vb{sub%2}")
            ba, ka = colmm(384, 96)
            bb, kb2 = colmm(480, 96)
            rope(ba, ka, bb, kb2, kpe, "kpe", 1.0)
            for h in range(8):
                p.dma("sp", g["kbT"][h, 64:96, T0:T0 + NT], kpe[R, :], r=["kpe"], lane="kpe")
            b, kb_ = bk.get()
            for sub in range(4):
                for c in range(8):
                    p.mm(b[:, sub * 64:(sub + 1) * 64], hb[:, c, sub * 128:(sub + 1) * 128], win[:, c, 576:640], c == 0, c == 7,
                         r=["hb", wk[c]], w=[kb_])
            p.copy("dve", vas[:, :, 0:64], b[:, 0:256].rearrange("p (s d) -> p s d", s=4), r=[kb_], w=["vas"])
            p.dma("sp", g["va"].rearrange("(n p) d -> p n d", p=128)[:, t * 4:(t + 1) * 4, :], vas[:], r=["vas"], lane="va")
            if t >= SOWN // NT or not QSIDE:
                continue
            for a in range(4):
                b, kb_ = colmm(QA0 + a * 128, 128)
                e_, ek = evac(b, slice(0, 128), kb_, scale=0.125)
                p.dma("sp", g["qaT"][:, 2 * a, T0:T0 + NT], e_[0:64, :], r=[ek], lane=ek)
                p.dma("sp", g["qaT"][:, 2 * a + 1, T0:T0 + NT], e_[64:128, :], r=[ek], lane=ek)
            for a in range(4):
                b, kb_ = colmm(QI0 + a * 128, 128)
                e_, ek = evac(b, slice(0, 128), kb_)
                p.dma("sp", g["qiT"][:, 2 * a, T0:T0 + NT], e_[0:64, :], r=[ek], lane=ek)
                p.dma("sp", g["qiT"][:, 2 * a + 1, T0:T0 + NT], e_[64:128, :], r=[ek], lane=ek)
            b, kb_ = bk.get()
            for sub in range(4):
                for c in range(8):
                    p.mm(b[:, sub * 8:(sub + 1) * 8], hb[:, c, sub * 128:(sub + 1) * 128], win[:, c, WI0:WI0 + 8], c == 0, c == 7,
                         r=["hb", wk[c]], w=[kb_])
            p.ts("dve", wis[:], b[:, 0:32].rearrange("p (s d) -> p s d", s=4), 1.0 / (8.0 * 8.0 ** 0.5), None, ALU.mult, r=[kb_], w=["wis"])
            p.dma("sp", g["widx"].rearrange("(n p) d -> p n d", p=128)[:, t * 4:(t + 1) * 4, :], wis[:], r=["wis"], lane="widx")
            cb = [colmm(CQ0 + c3 * 128, 128) for c3 in range(3)]
            for c3 in range(3):
                p.act(sq[:, c3, :], cb[c3][0][:], AF.Square, r=[cb[c3][1]], w=["sq"])
            rms_rstd(p, ps[0], lambda c: sq[:, c, :], 3, ones, epsc, rs, NT, 1.0 / 384, ["sq"], "ps0")
            for c3 in range(3):
                p.stt("dve", cn[:, c3, :], cb[c3][0][:], gcq[:, c3:c3 + 1], rs[:], ALU.mult, ALU.mult, r=[cb[c3][1], "rs", "gcq"], w=["cn"])
            s96 = 96.0 ** -0.5
            for h in range(8):
                ba, ka = bk.get()
                for c3 in range(3):
                    p.mm(ba[0:96, :], wuq[:, c3, h * 96:(h + 1) * 96], cn[:, c3, :], c3 == 0, c3 == 2, r=["cn", "wuq"], w=[ka])
                bb, kb2 = bk.get()
                for c3 in range(3):
                    p.mm(bb[0:96, :], wuqs[:, c3, h * 96:(h + 1) * 96], cn[:, c3, :], c3 == 0, c3 == 2, r=["cn", "wuqs"], w=[kb2])
                q_, qk = qbs[h % 2], f"qbs{h%2}"
                p.ts("dve", q_[0:64, :], ba[0:64, :], s96, None, ALU.mult, r=[ka], w=[qk])
                rope(ba, ka, bb, kb2, q_, qk, s96)
                p.dma("sp", g["qbT"][:, h, T0:T0 + NT], q_[0:96, :], r=[qk], lane=f"qb{h%2}")
            for m in range(16):
                b, kb_ = colmm(GT0 + m * 128, 128)
                p.act(gs[m % 2][:], b[:], AF.Sigmoid, r=[kb_], w=[f"gs{m%2}"])
                p.dma("sp", g["gT"][m * 128:(m + 1) * 128, T0:T0 + NT], gs[m % 2][:], r=[f"gs{m%2}"], lane=f"gs{m%2}")
        p.emit()


def attn_phase(nc, g, ps, identb, mode):
    NB = 32
    with contextlib.ExitStack() as es:
        sb = lambda n, s, d=F32: es.enter_context(nc.sbuf_tensor("p3" + mode + n, list(s), d))
        dsa = mode == "dsa"
        SD = S if dsa else 2
        SM = 2 if dsa else S
        kiT = sb("kiT", [64, SD], BF16); kaT = sb("kaT", [64, SD], BF16)
        va = sb("va", [128, 64 if dsa else 1, 65], BF16); vb = sb("vb", [128, 1 if dsa else 64, 8 * 65], BF16)
        kb = [sb(f"kb{i}", [96, SM], BF16) for i in range(2)]
        Isc = sb("Isc", [128, SD]); nm = sb("nm", [128, SD], BF16)
        junk = nm
        I4 = sb("I4", [128, 512], BF16); sel = sb("sel", [65, 64], BF16)
        dhi = sb("dhi", [65, 512], BF16); dlo = sb("dlo", [65, 512], BF16)
        qi = sb("qi", [64, 8, 128], BF16); qa = sb("qa", [64, 8, 128], BF16); wq = sb("wq", [128, 8])
        Dh = sb("Dh", [128, 8, 128], BF16)
        rl = [sb(f"rl{i}", [128, 8, 512 if dsa else 2], BF16) for i in range(2)]
        cmq = sb("cmq", [128, 256])
        pw = sb("pw", [128, NBIS + 1]); hk = sb("hk", [128, NBIS + 1]); h2 = sb("h2", [128, NBIS + 1])
        sm = sb("sm", [128, 8])
        pt = [sb(f"pt{i}", [128, 1024], BF16) for i in range(4)]
        osb = sb("osb", [65, 1024]); rden = sb("rden", [64, 1024])
        oo = sb("oo", [64, 1024], BF16)
        qb = sb("qb", [96, 8, 2 if dsa else 512], BF16)
        cmk = sb("cmk", [128, 8, 2 if dsa else 512], BF16)
        Bt = sb("Bt", [128, 3, 1024], BF16)
        rb = sb("rb", [128, 32, 8]); dl = sb("dl", [128, len(TH), 8])
        pqi = sb("pqi", [128, 128], I32); pki = sb("pki", [128, 3], I32)
        pqf = sb("pqf", [128, 128]); pkf = sb("pkf", [128, 3])
        rel = sb("rel", [128, 128]); ind = sb("ind", [128, 128]); bacc = sb("bacc", [128, 8, 128])
        p = Phase(nc, "p3" + mode)
        if dsa:
            p.dma("sp", kiT[:], g["kiT"], w=["kiT"], lane="kiT")
            p.dma("sp", kaT[:], g["kaT"], w=["kaT"], lane="kaT")
            vav = g["va"].rearrange("(n p) d -> p n d", p=128)
            for q4 in range(16):
                p.dma("sp", va[:, q4 * 4:(q4 + 1) * 4, :], vav[:, q4 * 4:(q4 + 1) * 4, :], w=["va"], lane="va")
        else:
            vbv = g["vb"].rearrange("(n p) h d -> p n (h d)", p=128)
            for q4 in range(16):
                p.dma("sp", vb[:, q4 * 4:(q4 + 1) * 4, :], vbv[:, q4 * 4:(q4 + 1) * 4, :], w=["vb"], lane="vbl")
        for q4 in range(4):
            p.copy("dve", I4[:, q4 * 128:(q4 + 1) * 128], identb, r=["ident"], w=["I4"])
        p.memset("dve", sel[:], 0.0, w=["sel"])
        p.memset("dve", sel[64:65, :], 1.0, w=["sel"])
        for k in range(NBIS + 1):
            p.memset("dve", pw[:, k:k + 1], 2.0 ** -(k + 1), w=["pw"])
        if dsa:
            p.dma("sp", rb[:], g["rb128"], w=["rb"], lane="rb")
            p.dma("sp", pqi[:], g["posq_bc"], w=["pqi"], lane="pqi")
            p.dma("sp", pki[:], g["posk_col"], w=["pki"], lane="pki")
            p.copy("dve", pqf[:], pqi[:], r=["pqi"], w=["pqf"])
            p.copy("dve", pkf[:], pki[:], r=["pki"], w=["pkf"])
            prev = 15
            for j, (th, nb_) in enumerate(TH):
                p.tt("dve", dl[:, j, :], rb[:, nb_, :], rb[:, prev, :], ALU.subtract, r=["rb"], w=["dl"])
                prev = nb_
            for ty in range(3):
                p.ts("dve", rel[:], pqf[:], pkf[:, ty:ty + 1], -1.0, ALU.subtract, ALU.mult, r=["pqf", "pkf"], w=["rel"])
                p.memset("pool", bacc[:], 0.0, w=["bacc"] + [f"bacc{h}" for h in range(8)])
                for j, (th, nb_) in enumerate(TH):
                    p.ts("dve", ind[:], rel[:], float(th), None, ALU.is_ge, r=["rel"], w=["ind"])
                    for h in range(8):
                        e = "dve"
                        p.stt(e, bacc[:, h, :], ind[:], dl[:, j, h:h + 1], bacc[:, h, :], ALU.mult, ALU.add, r=["ind", "dl", f"bacc{h}"], w=[f"bacc{h}"])
                p.copy("dve", Bt[:, ty, :], bacc[:].rearrange("p h q -> p (h q)"), r=["bacc"] + [f"bacc{h}" for h in range(8)], w=["Bt", "bacc"])

        bkI = Banks(ps, [0, 1, 2, 3])

        def normalize(accs, width, dst, dkey, dma_fn):
            for hf_, (b, kb_) in enumerate(accs):
                p.copy("act", osb[:, hf_ * 512:(hf_ + 1) * 512], b[0:65, :], r=[kb_], w=["osb"])
            for hf_ in range(len(accs)):
                b, kb_ = ps[6 + hf_ % 2], f"ps{6 + hf_ % 2}"
                p.copy("dve", dhi[:], osb[:, hf_ * 512:(hf_ + 1) * 512], r=["osb"], w=["dhi"])
                p.tt("dve", dlo[:], osb[:, hf_ * 512:(hf_ + 1) * 512], dhi[:], ALU.subtract, r=["osb", "dhi"], w=["dlo"])
                p.mm(b[0:64, :], sel[:], dhi[:], True, False, r=["dhi", "sel"], w=[kb_])
                p.mm(b[0:64, :], sel[:], dlo[:], False, True, r=["dlo", "sel"], w=[kb_])
                p.add("dve", lambda e, b=b, hf_=hf_: e.reciprocal(out=rden[:, hf_ * 512:(hf_ + 1) * 512], in_=b[0:64, :]), r=[kb_], w=["rden"])
            p.tt("dve", oo[:, :width], osb[0:64, :width], rden[:, :width], ALU.mult, r=["osb", "rden"], w=["oo"])
            dma_fn()

        kbcount = [0]

        def dsa_block(i):
            Q0 = i * 128
            nk = 2 * (i + 1)
            blocks = list(range(i + 1)) + list(range(32, 32 + i + 1))
            W = nk * 128
            p.dma("sp", qi[:], g["qiT"][:, :, Q0:Q0 + 128], w=["qi"], lane="qi")
            p.dma("sp", qa[:], g["qaT"][:, :, Q0:Q0 + 128], w=["qa"], lane="qa")
            p.dma("sp", wq[:], g["widx"][Q0:Q0 + 128, :], w=["wq"], lane="wq")
            p.dma("sp", cmq[:], g["cmq"][i], w=["cmq"], lane="cmq")
            for h in range(8):
                p.ts("dve", Dh[:, h, :], identb, wq[:, h:h + 1], None, ALU.mult, r=["ident", "wq"], w=["Dh"])
            groups = []
            for (k0, n) in ((0, (i + 1) * 128), (4096, (i + 1) * 128)):
                o = 0
                while o < n:
                    w_ = min(512, n - o)
                    groups.append((k0 + o, w_))
                    o += w_
            col = 0
            for gi, (k0, w_) in enumerate(groups):
                R_ = rl[gi % 2]
                rk = f"rl{gi%2}"
                for h in range(8):
                    b, kb_ = bkI.get()
                    p.mm(b[:, :w_], qi[:, h, :], kiT[:, k0:k0 + w_], True, True, r=["qi", "kiT"], w=[kb_])
                    if h % 2 == 0:
                        p.act(R_[:, h, :w_], b[:, :w_], AF.Relu, r=[kb_], w=[rk])
                    else:
                        p.ts("dve", R_[:, h, :w_], b[:, :w_], 0.0, None, ALU.max, r=[kb_], w=[rk])
                b, kb_ = ps[4 + gi % 2], f"ps{4 + gi % 2}"
                for h in range(8):
                    p.mm(b[:, :w_], Dh[:, h, :], R_[:, h, :w_], h == 0, h == 7, r=["Dh", rk], w=[kb_])
                p.copy("act", Isc[:, col:col + w_], b[:, :w_], r=[kb_], w=["Isc"])
                col += w_
            p.add("dve", lambda e, W=W: e.tensor_reduce(out=sm[:, 0:1], in_=Isc[:, :W], axis=AX.X, op=ALU.max, apply_absolute_value=True),
                  r=["Isc"], w=["sm"])
            c_own = i * 128
            c_oth = (i + 1) * 128 + i * 128
            p.tt("dve", Isc[:, c_own:c_own + 128], Isc[:, c_own:c_own + 128], cmq[:, 0:128], ALU.add, r=["Isc", "cmq"], w=["Isc"])
            p.tt("dve", Isc[:, c_oth:c_oth + 128], Isc[:, c_oth:c_oth + 128], cmq[:, 128:256], ALU.add, r=["Isc", "cmq"], w=["Isc"])
            p.ts("dve", sm[:, 1:2], sm[:, 0:1], 2.02, 2e-6, ALU.mult, ALU.add, r=["sm"], w=["sm"])
            p.ts("dve", hk[:], pw[:], sm[:, 1:2], None, ALU.mult, r=["pw", "sm"], w=["hk"])
            p.ts("dve", h2[:], hk[:], 2.0, None, ALU.mult, r=["hk"], w=["h2"])
            p.ts("dve", sm[:, 2:3], sm[:, 1:2], 0.0, None, ALU.mult, r=["sm"], w=["sm"])
            for k in range(NBIS):
                p.ts("dve", junk[:, :W], Isc[:, :W], sm[:, 2:3], None, ALU.is_ge, ALU.add, r=["Isc", "sm"], w=["junk", "cnt"], accum_out=sm[:, 3:4])
                p.ts("dve", sm[:, 4:5], sm[:, 3:4], float(TOPK), h2[:, k + 1:k + 2], ALU.is_ge, ALU.mult, r=["cnt", "h2"], w=["tmp"])
                p.stt("dve", sm[:, 2:3], sm[:, 4:5], hk[:, k + 1:k + 2], sm[:, 2:3], ALU.subtract, ALU.add, r=["tmp", "sm", "hk"], w=["sm"])
            p.ts("dve", nm[:, :W], Isc[:, :W], sm[:, 2:3], NEG, ALU.is_lt, ALU.mult, r=["Isc", "sm"], w=["nm"])
            accs = [(ps[4], "ps4"), (ps[5], "ps5")]

            def s_stage(c, L):
                near = None
                if L == i:
                    near = 0
                elif L == 32 + i - 1:
                    near = 1
                elif L == 32 + i:
                    near = 2
                P_ = pt[c % 4]
                pk = f"pt{c%4}"
                for hf_ in range(2):
                    b, kb_ = ps[(c % 2) * 2 + hf_], f"ps{(c % 2) * 2 + hf_}"
                    p.mm(b[:], kaT[:, L * 128:(L + 1) * 128], qa[:, hf_ * 4:(hf_ + 1) * 4, :].rearrange("d h q -> d (h q)"), True, False,
                         r=["kaT", "qa"], w=[kb_])
                    p.mm(b[:], nm[:, c * 128:(c + 1) * 128], I4[:], False, near is None, r=["nm", "I4"], w=[kb_])
                    if near is not None:
                        p.mm(b[:], identb, Bt[:, near, hf_ * 512:(hf_ + 1) * 512], False, True, r=["ident", "Bt"], w=[kb_])
                    p.act(P_[:, hf_ * 512:(hf_ + 1) * 512], b[:], AF.Exp, r=[kb_], w=[pk])

            def pv_stage(c, L):
                P_ = pt[c % 4]
                pk = f"pt{c%4}"
                for hf_ in range(2):
                    b, kb_ = accs[hf_]
                    p.mm(b[0:65, :], va[:, L, :], P_[:, hf_ * 512:(hf_ + 1) * 512], c == 0, c == nk - 1, r=["va", pk], w=[kb_])

            s_stage(0, blocks[0])
            for c, L in enumerate(blocks):
                if c + 1 < nk:
                    s_stage(c + 1, blocks[c + 1])
                pv_stage(c, L)

            def dma_oa(Q0=Q0):
                p.dma("sp", g["oaT"][:, :, Q0:Q0 + 128], oo[:, :].rearrange("d (h q) -> d h q", h=8), r=["oo"], lane="oa")
            normalize(accs, 1024, oo, "oo", dma_oa)

        def kb_load(j, h, slot):
            nb_own = 4 * j + 4
            K_, kk_ = kb[slot], f"kb{slot}"
            p.dma("sp", K_[:, 0:nb_own * 128], g["kbT"][h, :, 0:nb_own * 128], w=[kk_], lane=kk_ + "a")
            p.dma("sp", K_[:, nb_own * 128:2 * nb_own * 128], g["kbT"][h, :, 4096:4096 + nb_own * 128], w=[kk_], lane=kk_ + "b")

        def mla_tile(j):
            T0 = j * NT
            nb_own = 4 * j + 4
            tblocks = list(range(nb_own)) + list(range(32, 32 + nb_own))
            n_ = len(tblocks)
            p.dma("sp", qb[:], g["qbT"][:, :, T0:T0 + NT], w=["qb"], lane="qb")
            p.dma("pool", cmk[:], g["cmk"][j], w=["cmk"], lane="cmk")
            if j == 0:
                kb_load(0, 0, 0)
            for h in range(8):
                slot = (j * 8 + h) % 2
                K_, kk_ = kb[slot], f"kb{slot}"
                if h + 1 < 8:
                    kb_load(j, h + 1, 1 - slot)
                elif j + 1 < 8:
                    kb_load(j + 1, 0, 1 - slot)
                acc = (ps[4 + h % 2], f"ps{4 + h % 2}")

                def s_stage(c, L):
                    b, kb_ = ps[c % 4], f"ps{c % 4}"
                    mi = None
                    if 4 * j <= L < 4 * j + 4:
                        mi = L - 4 * j
                    elif 32 + 4 * j <= L < 32 + 4 * j + 4:
                        mi = 4 + L - 32 - 4 * j
                    p.mm(b[:], K_[:, c * 128:(c + 1) * 128], qb[:, h, :], True, mi is None, r=[kk_, "qb"], w=[kb_])
                    if mi is not None:
                        p.mm(b[:], identb, cmk[:, mi, :], False, True, r=["ident", "cmk"], w=[kb_])
                    p.act(pt[c % 4][:, 0:512], b[:], AF.Exp, r=[kb_], w=[f"pt{c%4}"])

                def pv_stage(c, L):
                    p.mm(acc[0][0:65, :], vb[:, L, h * 65:(h + 1) * 65], pt[c % 4][:, 0:512], c == 0, c == n_ - 1, r=["vb", f"pt{c%4}"], w=[acc[1]])

                s_stage(0, tblocks[0])
                s_stage(1, tblocks[1])
                for c, L in enumerate(tblocks):
                    if c + 2 < n_:
                        s_stage(c + 2, tblocks[c + 2])
                    pv_stage(c, L)

                def dma_ob(h=h, T0=T0):
                    p.dma("sp", g["obT"][:, h, T0:T0 + NT], oo[:, 0:512], r=["oo"], lane="ob")
                normalize([acc], 512, oo, "oo", dma_ob)

        for i in range(min(NB, NBLK)):
            if dsa:
                dsa_block(i)
            elif i % 4 == 3:
                mla_tile(i // 4)
        p.emit()


def merge_phase(nc, g, x1T, x2T, ps, G2):
    with contextlib.ExitStack() as es:
        sb = lambda n, s, d=F32: es.enter_context(nc.sbuf_tensor("p4" + n, list(s), d))
        woa = sb("woa", [64, 8, D], BF16); wob = sb("wob", [64, 8, D], BF16); wout = sb("wout", [128, 8, D], BF16)
        oa = sb("oa", [64, 8, NT], BF16); ob = sb("ob", [64, 8, NT], BF16)
        gt = sb("gt", [128, 16, NT]); xs = sb("xs", [128, 8, NT])
        y = sb("y", [128, 8, NT], BF16); t1 = sb("t1", [128, NT]); t2 = sb("t2", [128, NT])
        p = Phase(nc, "p4")
        p.dma("pool", woa[:], g["w_o_a"].rearrange("(h d) n -> d h n", d=64), w=["woa"], lane="woa", max_dma_last_dim=4096)
        p.dma("pool", wob[:], g["w_o_b"].rearrange("(h d) n -> d h n", d=64), w=["wob"], lane="wob", max_dma_last_dim=4096)
        p.dma("pool", wout[:], g["w_out"].rearrange("(c p) n -> p c n", p=128), w=["wout"], lane="wout", max_dma_last_dim=4096)
        x1v = x1T.rearrange("(c p) t -> p c t", p=128)
        x2v = x2T.rearrange("(c p) t -> p c t", p=128)
        gv = g["gT"].rearrange("(c p) t -> p c t", p=128)
        for t in range(SOWN // NT):
            T0 = t * NT
            p.dma("sp", oa[:], g["oaT"][:, :, T0:T0 + NT], w=["oa"], lane="oa")
            p.dma("sp", ob[:], g["obT"][:, :, T0:T0 + NT], w=["ob"], lane="ob")
            p.dma("sp", gt[:], gv[:, :, T0:T0 + NT], w=["gt"], lane="gt")
            p.dma("sp", xs[:], x1v[:, :, T0:T0 + NT], w=["xs"], lane="xs")
            for m in range(8):
                ba, ka = ps[m % 2], f"ps{m%2}"
                bb, kb_ = ps[2 + m % 2], f"ps{2 + m%2}"
                for h in range(8):
                    p.mm(ba[:], woa[:, h, m * 128:(m + 1) * 128], oa[:, h, :], h == 0, h == 7, r=["woa", "oa"], w=[ka])
                for h in range(8):
                    p.mm(bb[:], wob[:, h, m * 128:(m + 1) * 128], ob[:, h, :], h == 0, h == 7, r=["wob", "ob"], w=[kb_])
                p.tt("dve", t1[:], ba[:], gt[:, m, :], ALU.mult, r=[ka, "gt"], w=["t1"])
                p.tt("dve", t2[:], bb[:], gt[:, 8 + m, :], ALU.mult, r=[kb_, "gt"], w=["t2"])
                p.tt("dve", y[:, m, :], t1[:], t2[:], ALU.add, r=["t1", "t2"], w=[f"y{m}"])
            for m in range(8):
                b, kb_ = ps[4 + m % 2], f"ps{4 + m%2}"
                for c in range(8):
                    p.mm(b[:], wout[:, c, m * 128:(m + 1) * 128], y[:, c, :], c == 0, c == 7, r=["wout", f"y{c}"], w=[kb_])
                p.stt("dve", xs[:, m, :], b[:], G2[:, m:m + 1], xs[:, m, :], ALU.mult, ALU.add, r=[kb_, "xs"], w=["xs"])
            p.dma("sp", x2v[:, :, T0:T0 + NT], xs[:], r=["xs"], lane="st")
        p.emit()


def build(stage=99, debug=False):
    nc = bass.Bass("TRN2", target_bir_lowering=False)
    dt = lambda n, s, d=F32: nc.dram_tensor(n, list(s), d, kind="ExternalInput").ap()
    xT = dt("xT", [D, S])
    cvec = dt("cvec", [128, 8])
    w_ada = dt("w_ada", [D, 9 * D])
    b_ada = dt("b_ada", [128, 72])
    g_ffn1 = dt("g_ffn1", [128, 8]); g_mix = dt("g_mix", [128, 8]); g_ffn2 = dt("g_ffn2", [128, 8]); g_final = dt("g_final", [128, 8])
    w1i = dt("w_ffn1_in", [D, 2 * DFF]); w1d = dt("w_ffn1_down", [DFF, D])
    w2i = dt("w_ffn2_in", [D, 2 * DFF]); w2d = dt("w_ffn2_down", [DFF, D])
    ident_d = dt("ident", [128, 128])
    g = {}
    g["winP"] = dt("winP", [D, WTOT])
    g["w_uk"] = dt("w_uk", [256, 512]); g["w_uv"] = dt("w_uv", [256, 512])
    g["w_uq"] = dt("w_uq", [384, 768]); g["w_uqs"] = dt("w_uqs", [384, 768])
    g["g_ckv"] = dt("g_ckv", [128, 2]); g["g_cq"] = dt("g_cq", [128, 3])
    g["freqc"] = dt("freqc", [128, 1]); g["sgnc"] = dt("sgnc", [128, 1])
    g["pos32"] = dt("pos32", [32, S], I32)
    g["rb128"] = dt("rb128", [128, 32, 8])
    g["posq_bc"] = dt("posq_bc", [128, 128], I32); g["posk_col"] = dt("posk_col", [128, 3], I32)
    g["cmq"] = dt("cmq", [32, 128, 256]); g["cmk"] = dt("cmk", [8, 128, 8, 512])
    g["w_o_a"] = dt("w_o_a", [512, D]); g["w_o_b"] = dt("w_o_b", [512, D]); g["w_out"] = dt("w_out", [D, D])
    outT = nc.dram_tensor("outT", [D, SOWN], F32, kind="ExternalOutput").ap()
    dbgset = set(debug.split(",")) if debug else set()
    it = lambda n, s, d=F32: nc.dram_tensor(n, list(s), d, kind=("ExternalOutput" if n in dbgset else "Internal")).ap()
    x1T = it("x1T", [D, S]); x2T = it("x2T", [D, SOWN])
    g["kaT"] = it("kaT", [64, S], BF16); g["kiT"] = it("kiT", [64, S], BF16)
    g["kbT"] = it("kbT", [8, 96, S], BF16); g["vb"] = it("vb", [S, 8, 65], BF16); g["va"] = it("va", [S, 65], BF16)
    g["qaT"] = it("qaT", [64, 8, SOWN], BF16); g["qiT"] = it("qiT", [64, 8, SOWN], BF16)
    g["widx"] = it("widx", [SOWN, 8]); g["qbT"] = it("qbT", [96, 8, SOWN], BF16)
    g["gT"] = it("gT", [2048, SOWN])
    g["oaT"] = it("oaT", [64, 8, SOWN], BF16); g["obT"] = it("obT", [64, 8, SOWN], BF16)

    with contextlib.ExitStack() as es:
        ps = [es.enter_context(nc.psum_tensor(f"psb{i}", [128, 512], F32)) for i in range(8)]
        sb = lambda n, s, d=F32: es.enter_context(nc.sbuf_tensor(n, list(s), d))
        ones_t = sb("ones", [128, 128], BF16); ones = ones_t[:]
        epsc_t = sb("epsc", [128, 1]); epsc = epsc_t[:]
        identb_t = sb("identb", [128, 128], BF16); identb = identb_t[:]
        modT_t = sb("modT", [128, 72]); modT = modT_t[:]
        drv_t = sb("drv", [128, 10, 8])
        derived = [drv_t[:, i, :] for i in range(9)]
        gfin = drv_t[:, 9, :]
        A1, S1, G1, A2, S2, G2, A3, S3, G3 = derived

        setup_phase(nc, cvec, w_ada, b_ada, [g_ffn1, g_mix, g_ffn2], modT, None, ones, epsc, ident_d, identb, ps, derived)
        pp = Phase(nc, "gfin")
        pp.dma("sp", gfin, g_final, w=["gf"], lane="gf")
        pp.emit()
        if stage == 0:
            return nc
        if stage == 20:
            proj_phase(nc, x1T, g, ps, ones, epsc, A2, S2)
            return nc
        if stage in (30, 31):
            import os
            global NBLK
            NBLK = int(os.environ.get("NBLK", "32"))
            attn_phase(nc, g, ps, identb, "dsa" if stage == 30 else "mla")
            return nc
        ffn_phase(nc, "f1", xT, x1T, S // NT, w1i, w1d, A1, S1, G1, ps, ones, epsc)
        if stage == 1:
            ffn_phase(nc, "f2", x1T[:, 0:SOWN], outT, SOWN // NT, w2i, w2d, A3, S3, G3, ps, ones, epsc, gfin=gfin)
            return nc
        proj_phase(nc, x1T, g, ps, ones, epsc, A2, S2)
        if stage == 2:
            return nc
        attn_phase(nc, g, ps, identb, "dsa")
        if stage == 3:
            return nc
        attn_phase(nc, g, ps, identb, "mla")
        if stage == 4:
            return nc
        merge_phase(nc, g, x1T, x2T, ps, G2)
        ffn_phase(nc, "f2", x2T, outT, SOWN // NT, w2i, w2d, A3, S3, G3, ps, ones, epsc, gfin=gfin)
    return nc


def local_perm(p):
    own = np.arange(32) * 2 + p
    oth = np.arange(32) * 2 + 1 - p
    blocks = np.concatenate([own, oth])
    return (blocks[:, None] * 128 + np.arange(128)[None, :]).reshape(-1)


def pm(v):
    v = np.asarray(v, np.float32)
    return np.ascontiguousarray(v.reshape(-1, 128).T)


FREQ16 = [1.0, 0.5623413324356079, 0.3162277638912201, 0.17782793939113617, 0.10000000149011612, 0.05623413249850273,
          0.03162277489900589, 0.017782794311642647, 0.009999999776482582, 0.005623413249850273, 0.003162277629598975,
          0.0017782794311642647, 0.0010000000474974513, 0.000562341301701963, 0.0003162277571391314, 0.00017782794020604342]


def host_consts(p):
    perm = local_perm(p)
    lim = (perm // 64 + 1) * 64
    cmq = np.zeros((32, 128, 256), np.float32)
    for i in range(32):
        ql = lim[i * 128:(i + 1) * 128][:, None]
        for half, kbk in ((0, i), (1, 32 + i)):
            kt = perm[kbk * 128:(kbk + 1) * 128][None, :]
            cmq[i, :, half * 128:(half + 1) * 128] = np.where(kt < ql, 0.0, -1e30)
    cmk = np.zeros((8, 128, 8, 512), np.float32)
    for j in range(8):
        ql = lim[j * 512:(j + 1) * 512][None, :]
        for mi in range(8):
            kbk = 4 * j + mi if mi < 4 else 32 + 4 * j + (mi - 4)
            kt = perm[kbk * 128:(kbk + 1) * 128][:, None]
            cmk[j, :, mi, :] = np.where(kt < ql, 0.0, NEG)
    freqc = np.zeros((128, 1), np.float32)
    sgnc = np.zeros((128, 1), np.float32)
    for r in range(32):
        freqc[64 + r, 0] = FREQ16[r % 16]
        sgnc[64 + r, 0] = -1.0 if r < 16 else 1.0
    return perm, cmq, cmk, freqc, sgnc


def kernel(**inputs):
    import os
    stage = int(os.environ.get("KSTAGE", "99"))
    debug = os.environ.get("KDEBUG", "")
    f = lambda a: np.ascontiguousarray(np.asarray(a, np.float32))
    x = np.asarray(inputs["x"], np.float32)
    w_in = np.asarray(inputs["w_in"][0], np.float32)
    q_a, k_a, v_a = w_in[:, 0:512], w_in[:, 512:576], w_in[:, 576:640]
    q_i, k_i, w_i = w_in[:, 640:1152], w_in[:, 1152:1216], w_in[:, 1216:1224]
    c_q, c_kv, k_r, gts = w_in[:, 1224:1608], w_in[:, 1608:1864], w_in[:, 1864:1896], w_in[:, 1896:3944]
    k_rs = np.concatenate([k_r[:, 16:32], k_r[:, 0:16]], axis=1)
    winP = np.ascontiguousarray(np.concatenate([k_a, k_i, c_kv, k_a, k_r, k_a, k_rs, v_a, q_a, q_i, c_q, gts, w_i], axis=1))
    assert winP.shape[1] == WTOT
    w_uq = np.asarray(inputs["w_uq"][0], np.float32)
    w_uqs = w_uq.copy().reshape(384, 8, 96)
    w_uqs[:, :, 64:80], w_uqs[:, :, 80:96] = w_uq.reshape(384, 8, 96)[:, :, 80:96], w_uq.reshape(384, 8, 96)[:, :, 64:80]
    w_uqs = np.ascontiguousarray(w_uqs.reshape(384, 768))
    nc = build(stage, debug)
    in_maps = []
    perms = []
    pos_all = np.asarray(inputs["positions"], np.int32)
    rb128 = np.ascontiguousarray(np.broadcast_to(f(inputs["rel_bias"])[None], (128, 32, 8)))
    consts = [host_consts(0), host_consts(1)]
    for core in range(NCORES):
        b, p = core // 2, core % 2
        perm, cmq, cmk, freqc, sgnc = consts[p]
        perms.append(perm)
        posl = pos_all[b][perm]
        m = {
            "xT": np.ascontiguousarray(x[b][perm].T),
            "cvec": pm(inputs["c"][b]),
            "w_ada": f(inputs["w_ada"][0]),
            "b_ada": pm(inputs["b_ada"][0]),
            "g_ffn1": pm(inputs["g_ffn1"][0]),
            "g_mix": pm(inputs["g_mix"][0]),
            "g_ffn2": pm(inputs["g_ffn2"][0]),
            "g_final": pm(inputs["g_final"]),
            "w_ffn1_in": f(inputs["w_ffn1_in"][0]),
            "w_ffn1_down": f(inputs["w_ffn1_down"][0]),
            "w_ffn2_in": f(inputs["w_ffn2_in"][0]),
            "w_ffn2_down": f(inputs["w_ffn2_down"][0]),
            "ident": np.eye(128, dtype=np.float32),
            "winP": winP, "w_uk": f(inputs["w_uk"][0]), "w_uv": f(inputs["w_uv"][0]), "w_uq": w_uq, "w_uqs": w_uqs,
            "g_ckv": pm(inputs["g_ckv"][0]), "g_cq": pm(inputs["g_cq"][0]),
            "freqc": freqc, "sgnc": sgnc,
            "pos32": np.ascontiguousarray(np.broadcast_to(posl[None], (32, S))),
            "rb128": rb128,
            "posq_bc": np.ascontiguousarray(np.broadcast_to(posl[128:256][None], (128, 128))),
            "posk_col": np.ascontiguousarray(np.stack([posl[128:256], posl[32 * 128:33 * 128], posl[33 * 128:34 * 128]], axis=1)),
            "cmq": cmq, "cmk": cmk,
            "w_o_a": f(inputs["w_o_a"][0]), "w_o_b": f(inputs["w_o_b"][0]), "w_out": f(inputs["w_out"][0]),
        }
        in_maps.append(m)
    res = run_bass_kernel_spmd(nc, in_maps, core_ids=list(range(NCORES)))
    if debug:
        kernel.debug = res.results
        kernel.perms = perms
    out = np.empty((4, S, D), np.float32)
    for core in range(NCORES):
        b = core // 2
        o = res.results[core]["outT"]
        out[b][perms[core][:SOWN]] = o.T
    return out
```

```python
import contextlib
import numpy as np
import concourse.bass as bass
import concourse.mybir as mybir
from concourse.bass_utils import run_bass_kernel_spmd

F32 = mybir.dt.float32
BF16 = mybir.dt.bfloat16
I32 = mybir.dt.int32
ALU = mybir.AluOpType
AF = mybir.ActivationFunctionType
AX = mybir.AxisListType

D = 1024
S = 8192
DFF = 2816
NT = 512
EPS = 1e-6
NCORES = 8
SOWN = 4096
TOPK = 256
NBIS = 10
NEG = -30000.0
NBLK = 32


_SEMREG = {}


def _semreg(nc):
    return _SEMREG.setdefault(id(nc), {"cnt": {}, "gen": {}, "sem": {}})


class Phase:
    def __init__(self, nc, name):
        self.nc = nc
        self.name = name
        self.ops = []
        self.lw = {}
        self.rd = {}

    def add(self, eng, fn, r=(), w=(), lane=None):
        i = len(self.ops)
        deps = set()
        for k in r:
            if k in self.lw:
                deps.add(self.lw[k])
        for k in w:
            if k in self.lw:
                deps.add(self.lw[k])
            deps.update(self.rd.get(k, {}).values())
        for k in w:
            self.lw[k] = i
            self.rd[k] = {}
        tag = lane if lane is not None else eng
        for k in r:
            self.rd.setdefault(k, {})[tag] = i
        self.ops.append(dict(eng=eng, fn=fn, deps=deps, lane=lane, inc=False))
        return i

    def dma(self, eng, out, in_, r=(), w=(), lane=None, **kw):
        assert lane is not None
        return self.add(eng, lambda e: e.dma_start(out=out, in_=in_, **kw), r, w, lane=lane)

    def mm(self, out, lhsT, rhs, start, stop, r=(), w=()):
        return self.add("pe", lambda e: e.matmul(out, lhsT, rhs, start=start, stop=stop), r, w)

    def act(self, out, in_, func, r=(), w=(), eng="act", **kw):
        return self.add(eng, lambda e: e.activation(out=out, in_=in_, func=func, **kw), r, w)

    def ts(self, eng, out, in0, s1, s2, op0, op1=None, r=(), w=(), **kw):
        if op1 is None:
            return self.add(eng, lambda e: e.tensor_scalar(out=out, in0=in0, scalar1=s1, scalar2=None, op0=op0, **kw), r, w)
        return self.add(eng, lambda e: e.tensor_scalar(out=out, in0=in0, scalar1=s1, scalar2=s2, op0=op0, op1=op1, **kw), r, w)

    def stt(self, eng, out, in0, scalar, in1, op0, op1, r=(), w=()):
        return self.add(eng, lambda e: e.scalar_tensor_tensor(out=out, in0=in0, scalar=scalar, in1=in1, op0=op0, op1=op1), r, w)

    def tt(self, eng, out, in0, in1, op, r=(), w=()):
        return self.add(eng, lambda e: e.tensor_tensor(out=out, in0=in0, in1=in1, op=op), r, w)

    def copy(self, eng, out, in_, r=(), w=()):
        if eng == "act":
            return self.add(eng, lambda e: e.activation(out=out, in_=in_, func=AF.Copy), r, w)
        return self.add(eng, lambda e: e.tensor_copy(out=out, in_=in_), r, w)

    def memset(self, eng, ap, val, w=()):
        return self.add(eng, lambda e: e.memset(ap, val), (), w)

    def emit(self):
        nc = self.nc
        ops = self.ops

        def skip(dop, op):
            return dop["lane"] is None and op["lane"] is None and dop["eng"] == "pe" and op["eng"] == "pe"

        for op in ops:
            for d in op["deps"]:
                if not skip(ops[d], op):
                    ops[d]["inc"] = True
        last_dma = {}
        last_eng = {}
        for i, op in enumerate(ops):
            if op["lane"] is not None:
                op["inc"] = True
                last_dma[op["lane"]] = i
            else:
                last_eng[op["eng"]] = i
        for i in last_eng.values():
            ops[i]["inc"] = True
        reg = _semreg(nc)
        cnt, gen, semh = reg["cnt"], reg["gen"], reg["sem"]
        for op in ops:
            if not op["inc"]:
                continue
            base = ("L", op["lane"]) if op["lane"] is not None else ("E", op["eng"])
            gen[base] = gen.get(base, 0)
            key = base + (gen[base],)
            cnt[key] = cnt.get(key, 0) + (16 if op["lane"] is not None else 1)
            op["sem"] = key
            op["val"] = cnt[key]
            pk = reg.setdefault("prevkey", {})
            if op["lane"] is not None and base in pk and pk[base] != key:
                op["drain"] = (pk[base], cnt[pk[base]])
            pk[base] = key
            if key not in semh:
                semh[key] = nc.alloc_semaphore(name=f"s_{key[0]}_{key[1]}_{key[2]}")
            if cnt[key] >= (512 if op["lane"] is not None else 4000):
                gen[base] += 1
        sems = semh
        if "bar" not in reg:
            reg["bar"] = nc.alloc_semaphore(name="s_phase_barrier")
            reg["barcnt"] = 0
        reg["barcnt"] += 5
        bar, bartarget = reg["bar"], reg["barcnt"]
        with contextlib.ExitStack() as es:
            block = es.enter_context(nc.Block())

            def run(engname):
                def body(e):
                    waited = {}
                    for op in ops:
                        if op["eng"] != engname:
                            continue
                        for d in sorted(op["deps"]):
                            dop = ops[d]
                            if not dop["inc"] or skip(dop, op):
                                continue
                            sk, sv = dop["sem"], dop["val"]
                            if waited.get(sk, 0) < sv:
                                e.wait_ge(sems[sk], sv)
                                waited[sk] = sv
                        if "drain" in op:
                            e.wait_ge(sems[op["drain"][0]], op["drain"][1])
                        ins = op["fn"](e)
                        if op["inc"]:
                            ins.then_inc(sems[op["sem"]], 16 if op["lane"] is not None else 1)
                    if engname == "sp":
                        for lane, i in last_dma.items():
                            op = ops[i]
                            if waited.get(op["sem"], 0) < op["val"]:
                                e.wait_ge(sems[op["sem"]], op["val"])
                                waited[op["sem"]] = op["val"]
                    if engname in last_eng:
                        op = ops[last_eng[engname]]
                        e.wait_ge(sems[op["sem"]], op["val"])
                    e.sem_inc(bar, 1)
                    e.wait_ge(bar, bartarget)
                return body

            block.tensor(run("pe"))
            block.scalar(run("act"))
            block.vector(run("dve"))
            block.gpsimd(run("pool"))
            block.sync(run("sp"))


def rms_rstd(p, ps_bank, sq_ap, nchunk, ones, epsc, rs, width, inv_n, rkeys, tagw):
    for c in range(nchunk):
        p.mm(ps_bank[:, :width], ones, sq_ap(c), c == 0, c == nchunk - 1, r=rkeys + ["ones"], w=[tagw])
    p.act(rs[:, :width], ps_bank[:, :width], AF.Sqrt, r=[tagw, "epsc"], w=["rs"], bias=epsc, scale=inv_n)
    p.add("dve", lambda e: e.reciprocal(out=rs[:, :width], in_=rs[:, :width]), r=["rs"], w=["rs"])


def ffn_phase(nc, name, xsrc, xdst, ntiles, w_in_d, w_dn_d, Ac, Sc, Gc, ps, ones, epsc, gfin=None):
    NJ = DFF // 128
    with (nc.sbuf_tensor(name + "wi", [128, 8, 2 * DFF], BF16) as wi,
          nc.sbuf_tensor(name + "wd", [128, NJ, D], BF16) as wd,
          nc.sbuf_tensor(name + "xs0", [128, 8, NT], F32) as xs0,
          nc.sbuf_tensor(name + "xs1", [128, 8, NT], F32) as xs1,
          nc.sbuf_tensor(name + "hb", [128, 8, NT], BF16) as hb,
          nc.sbuf_tensor(name + "hf", [128, NJ, NT], BF16) as hf,
          nc.sbuf_tensor(name + "sg0", [128, NT], F32) as sg0,
          nc.sbuf_tensor(name + "sg1", [128, NT], F32) as sg1,
          nc.sbuf_tensor(name + "tf", [128, NT], F32) as tf,
          nc.sbuf_tensor(name + "rs", [128, NT], F32) as rs):
        p = Phase(nc, name)
        xs = [xs0, xs1]
        sg = [sg0, sg1]
        w_in_v = w_in_d.rearrange("(c p) n -> p c n", p=128)
        w_dn_v = w_dn_d.rearrange("(c p) n -> p c n", p=128)
        xsrc_v = xsrc.rearrange("(c p) t -> p c t", p=128)
        xdst_v = xdst.rearrange("(c p) t -> p c t", p=128)
        p.dma("sp", xs[0][:], xsrc_v[:, :, 0:NT], w=["xs0"], lane="ld0")
        for c in range(8):
            p.dma("pool", wi[:, c, :], w_in_v[:, c, :], w=["wi"], lane="wi", max_dma_last_dim=4096)
        for c in range(NJ):
            p.dma("pool", wd[:, c, :], w_dn_v[:, c, :], w=["wd"], lane="wd", max_dma_last_dim=4096)
        wik = ["wi" for c in range(8)]
        wdk = ["wd" for c in range(NJ)]
        for t in range(ntiles):
            s = t % 2
            X = xs[s]
            xk = f"xs{s}"
            if t + 1 < ntiles:
                p.dma("sp", xs[1 - s][:], xsrc_v[:, :, (t + 1) * NT:(t + 2) * NT], w=[f"xs{1-s}"], lane=f"ld{1-s}")
            p.act(hb[:], X[:], AF.Square, r=[xk], w=["hb"])
            rms_rstd(p, ps[0], lambda c: hb[:, c, :], 8, ones, epsc, rs, NT, 1.0 / D, ["hb"], "ps0")
            for c in range(8):
                p.stt("dve", tf[:], X[:, c, :], Ac[:, c:c + 1], rs[:], ALU.mult, ALU.mult, r=[xk, "rs"], w=["tf"])
                p.ts("dve", hb[:, c, :], tf[:], Sc[:, c:c + 1], None, ALU.add, r=["tf"], w=["hb"])
            for j in range(NJ):
                pg, pu = ps[1 + 2 * (j % 2)], ps[2 + 2 * (j % 2)]
                kg, ku = f"ps{1 + 2 * (j % 2)}", f"ps{2 + 2 * (j % 2)}"
                for c in range(8):
                    p.mm(pg[:], wi[:, c, j * 128:(j + 1) * 128], hb[:, c, :], c == 0, c == 7, r=["hb", wik[c]], w=[kg])
                for c in range(8):
                    p.mm(pu[:], wi[:, c, DFF + j * 128:DFF + (j + 1) * 128], hb[:, c, :], c == 0, c == 7, r=["hb", wik[c]], w=[ku])
                p.act(sg[j % 2][:], pg[:], AF.Silu, r=[kg], w=[f"sg{j%2}"])
                p.tt("dve", hf[:, j, :], pu[:], sg[j % 2][:], ALU.mult, r=[ku, f"sg{j%2}"], w=[f"hf{j}"])
            for m in range(8):
                po, ko = ps[5 + m % 2], f"ps{5 + m % 2}"
                for j in range(NJ):
                    p.mm(po[:], wd[:, j, m * 128:(m + 1) * 128], hf[:, j, :], j == 0, j == NJ - 1, r=[f"hf{j}", wdk[j]], w=[ko])
                p.stt("dve", X[:, m, :], po[:], Gc[:, m:m + 1], X[:, m, :], ALU.mult, ALU.add, r=[ko, xk], w=[xk])
            if gfin is not None:
                p.act(hb[:], X[:], AF.Square, r=[xk], w=["hb"])
                rms_rstd(p, ps[0], lambda c: hb[:, c, :], 8, ones, epsc, rs, NT, 1.0 / D, ["hb"], "ps0")
                for c in range(8):
                    p.stt("dve", X[:, c, :], X[:, c, :], gfin[:, c:c + 1], rs[:], ALU.mult, ALU.mult, r=[xk, "rs"], w=[xk])
            p.dma("sp", xdst_v[:, :, t * NT:(t + 1) * NT], X[:], r=[xk], lane=f"st{s}")
        p.emit()


def setup_phase(nc, cvec, w_ada, b_ada, gvecs, modT, sc8, ones, epsc, ident_d, identb, ps, derived):
    with (nc.sbuf_tensor("wada0", [128, 8, 1024], BF16) as wa0,
          nc.sbuf_tensor("wada1", [128, 8, 1024], BF16) as wa1,
          nc.sbuf_tensor("cTs", [128, 8], F32) as cT,
          nc.sbuf_tensor("cTb", [128, 8], BF16) as cTb,
          nc.sbuf_tensor("bT", [128, 72], F32) as bT,
          nc.sbuf_tensor("gTs", [128, 3, 8], F32) as gT):
        p = Phase(nc, "setup")
        wa = [wa0, wa1]
        p.memset("dve", ones, 1.0, w=["ones"])
        p.memset("dve", epsc, EPS, w=["epsc"])
        p.dma("pool", identb, ident_d, w=["ident"], lane="ident")
        p.dma("sp", cT[:], cvec, w=["cT"], lane="c")
        p.dma("sp", bT[:], b_ada, w=["bT"], lane="b")
        for i, g in enumerate(gvecs):
            p.dma("sp", gT[:, i, :], g, w=[f"g{i}"], lane=f"g{i}")
        p.act(cTb[:], cT[:], AF.Silu, r=["cT"], w=["cTb"])
        wv = w_ada.rearrange("(c p) n -> p c n", p=128)
        for g in range(9):
            s = g % 2
            for c in range(8):
                p.dma("pool", wa[s][:, c, :], wv[:, c, g * 1024:(g + 1) * 1024], w=[f"wa{s}"], lane=f"wa{s}",
                      max_dma_last_dim=4096)
            for m in range(8):
                col = g * 8 + m
                for c in range(8):
                    p.mm(ps[0][:, col:col + 1], wa[s][:, c, m * 128:(m + 1) * 128], cTb[:, c:c + 1], c == 0, c == 7,
                         r=[f"wa{s}", "cTb"], w=["ps0"])
        p.tt("dve", modT, ps[0][:, 0:72], bT[:], ALU.add, r=["ps0", "bT"], w=["modT"])
        A1, S1, G1, A2, S2, G2, A3, S3, G3 = derived
        for (A, Sh, G, base, gi, gm) in ((A1, S1, G1, 0, 0, 0.5), (A2, S2, G2, 24, 1, 1.0), (A3, S3, G3, 48, 2, 0.5)):
            p.stt("dve", A, modT[:, base + 8:base + 16], 1.0, gT[:, gi, :], ALU.add, ALU.mult, r=["modT", f"g{gi}"], w=["drv"])
            p.copy("dve", Sh, modT[:, base:base + 8], r=["modT"], w=["drv"])
            p.ts("dve", G, modT[:, base + 16:base + 24], gm, None, ALU.mult, r=["modT"], w=["drv"])
        p.emit()


TWO_PI = 6.283185307179586
CW1 = 6.28125
CW2 = TWO_PI - CW1
MAGIC = 12582912.0
KW = 640
QA0, QI0, CQ0, GT0, WI0, WTOT = 640, 1152, 1664, 2048, 4096, 4104
TH = [(-90, 14), (-63, 13), (-45, 12), (-31, 11), (-22, 10), (-15, 9), (-11, 8), (-7, 7), (-6, 6), (-5, 5), (-4, 4),
      (-3, 3), (-2, 2), (-1, 1), (0, 0), (1, 17), (2, 18), (3, 19), (4, 20), (5, 21), (6, 22), (7, 23), (8, 24),
      (12, 25), (16, 26), (23, 27), (32, 28), (46, 29), (64, 30), (91, 31)]


class Banks:
    def __init__(self, ps, ids):
        self.ps, self.ids, self.i = ps, ids, 0

    def get(self):
        b = self.ids[self.i % len(self.ids)]
        self.i += 1
        return self.ps[b], f"ps{b}"


def proj_phase(nc, x1T, g, ps, ones, epsc, A2, S2):
    import os
    NTL = int(os.environ.get("P2TILES", str(S // NT)))
    QSIDE = os.environ.get("P2Q", "1") == "1"
    with contextlib.ExitStack() as es:
        sb = lambda n, s, d=F32: es.enter_context(nc.sbuf_tensor("p2" + n, list(s), d))
        win = sb("win", [128, 8, WTOT], BF16)
        wuk = sb("wuk", [128, 2, 512], BF16); wuv = sb("wuv", [128, 2, 512], BF16)
        wuq = sb("wuq", [128, 3, 768], BF16); wuqs = sb("wuqs", [128, 3, 768], BF16)
        gck = sb("gck", [128, 2]); gcq = sb("gcq", [128, 3])
        frq = sb("frq", [128, 1]); sgn = sb("sgn", [128, 1])
        xs = [sb("xs0", [128, 8, NT]), sb("xs1", [128, 8, NT])]
        hb = sb("hb", [128, 8, NT], BF16)
        tf = sb("tf", [128, NT]); rs = sb("rs", [128, NT])
        sq = sb("sq", [128, 3, NT], BF16)
        cn = sb("cn", [128, 3, NT], BF16)
        ev = [sb(f"ev{i}", [128, NT], BF16) for i in range(4)]
        gs = [sb(f"gs{i}", [128, NT]) for i in range(2)]
        vbs = [sb(f"vbs{i}", [128, 8, 65], BF16) for i in range(2)]
        vas = sb("vas", [128, 4, 65], BF16)
        wis = sb("wis", [128, 4, 8])
        posi = sb("posi", [128, NT], I32)
        rp = [sb(f"rp{i}", [128, NT]) for i in range(6)]
        Ct = sb("Ct", [128, NT]); St = sb("St", [128, NT])
        kpe = sb("kpe", [128, NT], BF16)
        qbs = [sb(f"qbs{i}", [128, NT], BF16) for i in range(2)]
        p = Phase(nc, "p2")
        bk = Banks(ps, [1, 2, 3, 4, 5, 6, 7])
        R = slice(64, 96)
        x1v = x1T.rearrange("(c p) t -> p c t", p=128)
        p.dma("sp", xs[0][:], x1v[:, :, 0:NT], w=["xs0"], lane="ld0")
        wv = g["winP"].rearrange("(c p) n -> p c n", p=128)
        for c in range(8):
            p.dma("pool", win[:, c, :], wv[:, c, :], w=["win"], lane="win", max_dma_last_dim=4096)
        for nm, t_, d_, nch in (("wuk", wuk, g["w_uk"], 2), ("wuv", wuv, g["w_uv"], 2), ("wuq", wuq, g["w_uq"], 3), ("wuqs", wuqs, g["w_uqs"], 3)):
            dv = d_.rearrange("(c p) n -> p c n", p=128)
            for c in range(nch):
                p.dma("pool", t_[:, c, :], dv[:, c, :], w=[nm], lane=nm)
        p.dma("sp", gck[:], g["g_ckv"], w=["gck"], lane="gck")
        p.dma("sp", gcq[:], g["g_cq"], w=["gcq"], lane="gcq")
        p.dma("sp", frq[:], g["freqc"], w=["frq"], lane="frq")
        p.dma("sp", sgn[:], g["sgnc"], w=["sgn"], lane="sgn")
        for i in range(2):
            p.memset("dve", vbs[i][:], 1.0, w=[f"vbs{i}"])
        p.memset("dve", vas[:], 1.0, w=["vas"])
        wk = ["win" for c in range(8)]
        evi = [0]

        def evac(src, rows, kb_, scale=None, eng=None):
            i = evi[0] % 4
            evi[0] += 1
            e = eng or ("act" if i % 2 == 0 else "dve")
            if scale is None and e == "dve":
                p.copy("dve", ev[i][rows, :], src[rows, :], r=[kb_], w=[f"ev{i}"])
            elif e == "dve":
                p.ts("dve", ev[i][rows, :], src[rows, :], scale, None, ALU.mult, r=[kb_], w=[f"ev{i}"])
            else:
                p.act(ev[i][rows, :], src[rows, :], AF.Copy, r=[kb_], w=[f"ev{i}"], scale=(1.0 if scale is None else scale))
            return ev[i], f"ev{i}"

        def colmm(col0, m):
            b, kb_ = bk.get()
            for c in range(8):
                p.mm(b[0:m, :], win[:, c, col0:col0 + m], hb[:, c, :], c == 0, c == 7, r=["hb", wk[c]], w=[kb_])
            return b, kb_

        for t in range(NTL):
            s = t % 2
            X, xk = xs[s], f"xs{s}"
            T0 = t * NT
            if t + 1 < NTL:
                p.dma("sp", xs[1 - s][:], x1v[:, :, (t + 1) * NT:(t + 2) * NT], w=[f"xs{1-s}"], lane=f"ld{1-s}")
            p.dma("sp", posi[R, :], g["pos32"][:, T0:T0 + NT], w=["posi"], lane="pos")
            p.act(hb[:], X[:], AF.Square, r=[xk], w=["hb"])
            rms_rstd(p, ps[0], lambda c: hb[:, c, :], 8, ones, epsc, rs, NT, 1.0 / D, ["hb"], "ps0")
            for c in range(8):
                p.stt("dve", tf[:], X[:, c, :], A2[:, c:c + 1], rs[:], ALU.mult, ALU.mult, r=[xk, "rs"], w=["tf"])
                p.ts("dve", hb[:, c, :], tf[:], S2[:, c:c + 1], None, ALU.add, r=["tf"], w=["hb"])
            p.copy("dve", rp[0][R, :], posi[R, :], r=["posi"], w=["rp0"])
            p.ts("dve", rp[0][R, :], rp[0][R, :], frq[R, 0:1], None, ALU.mult, r=["rp0", "frq"], w=["rp0"])
            for which, dst in ((0, St), (1, Ct)):
                src = rp[0]
                if which == 1:
                    p.ts("dve", rp[1][R, :], rp[0][R, :], 1.5707963267948966, None, ALU.add, r=["rp0"], w=["rp1"])
                    src = rp[1]
                sk = "rp0" if which == 0 else "rp1"
                p.ts("dve", rp[2][R, :], src[R, :], 1.0 / TWO_PI, None, ALU.mult, r=[sk], w=["rp2"])
                p.ts("dve", rp[3][R, :], rp[2][R, :], MAGIC, None, ALU.add, r=["rp2"], w=["rp3"])
                p.ts("dve", rp[3][R, :], rp[3][R, :], -MAGIC, None, ALU.add, r=["rp3"], w=["rp3"])
                p.stt("dve", rp[4][R, :], rp[3][R, :], -CW1, src[R, :], ALU.mult, ALU.add, r=["rp3", sk], w=["rp4"])
                p.stt("dve", rp[4][R, :], rp[3][R, :], -CW2, rp[4][R, :], ALU.mult, ALU.add, r=["rp3", "rp4"], w=["rp4"])
                if which == 0:
                    p.act(dst[R, :], rp[4][R, :], AF.Sin, r=["rp4", "sgn"], w=["St"], scale=sgn[R, 0:1])
                else:
                    p.act(dst[R, :], rp[4][R, :], AF.Sin, r=["rp4"], w=["Ct"])

            def rope(pa, ka, pb, kb2, outap, okey, scale):
                p.stt("dve", rp[5][R, :], pa[R, :], scale, Ct[R, :], ALU.mult, ALU.mult, r=[ka, "Ct"], w=["rp5"])
                p.stt("dve", rp[2][R, :], pb[R, :], scale, St[R, :], ALU.mult, ALU.mult, r=[kb2, "St"], w=["rp2"])
                p.tt("dve", outap[R, :], rp[5][R, :], rp[2][R, :], ALU.add, r=["rp5", "rp2"], w=[okey])

            b, kb_ = colmm(0, 128)
            e_, ek = evac(b, slice(0, 128), kb_)
            p.dma("sp", g["kaT"][:, T0:T0 + NT], e_[0:64, :], r=[ek], lane=ek)
            p.dma("sp", g["kiT"][:, T0:T0 + NT], e_[64:128, :], r=[ek], lane=ek)
            cb = [colmm(128, 128), colmm(256, 128)]
            for c2 in range(2):
                p.act(sq[:, c2, :], cb[c2][0][:], AF.Square, r=[cb[c2][1]], w=["sq"])
            rms_rstd(p, ps[0], lambda c: sq[:, c, :], 2, ones, epsc, rs, NT, 1.0 / 256, ["sq"], "ps0")
            for c2 in range(2):
                p.stt("dve", cn[:, c2, :], cb[c2][0][:], gck[:, c2:c2 + 1], rs[:], ALU.mult, ALU.mult, r=[cb[c2][1], "rs", "gck"], w=["cn"])
            for a in range(4):
                b, kb_ = bk.get()
                for c2 in range(2):
                    p.mm(b[:], wuk[:, c2, a * 128:(a + 1) * 128], cn[:, c2, :], c2 == 0, c2 == 1, r=["cn", "wuk"], w=[kb_])
                e_, ek = evac(b, slice(0, 128), kb_)
                p.dma("sp", g["kbT"][2 * a, 0:64, T0:T0 + NT], e_[0:64, :], r=[ek], lane=ek)
                p.dma("sp", g["kbT"][2 * a + 1, 0:64, T0:T0 + NT], e_[64:128, :], r=[ek], lane=ek)
            for sub in range(4):
                b, kb_ = bk.get()
                for c2 in range(2):
                    p.mm(b[:], cn[:, c2, sub * 128:(sub + 1) * 128], wuv[:, c2, :], c2 == 0, c2 == 1, r=["cn", "wuv"], w=[kb_])
                v = vbs[sub % 2]
                p.copy("dve", v[:, :, 0:64], b[:].rearrange("p (h d) -> p h d", h=8), r=[kb_], w=[f"vbs{sub%2}"])
                r0 = T0 + sub * 128
                p.dma("sp", g["vb"][r0:r0 + 128, :, :], v[:], r=[f"vbs{sub%2}"], lane=f"vb{sub%2}")
            ba, ka = colmm(384, 96)
            bb, kb2 = colmm(480, 96)
            rope(ba, ka, bb, kb2, kpe, "kpe", 1.0)
            for h in range(8):
                p.dma("sp", g["kbT"][h, 64:96, T0:T0 + NT], kpe[R, :], r=["kpe"], lane="kpe")
            b, kb_ = bk.get()
            for sub in range(4):
                for c in range(8):
                    p.mm(b[:, sub * 64:(sub + 1) * 64], hb[:, c, sub * 128:(sub + 1) * 128], win[:, c, 576:640], c == 0, c == 7,
                         r=["hb", wk[c]], w=[kb_])
            p.copy("dve", vas[:, :, 0:64], b[:, 0:256].rearrange("p (s d) -> p s d", s=4), r=[kb_], w=["vas"])
            p.dma("sp", g["va"].rearrange("(n p) d -> p n d", p=128)[:, t * 4:(t + 1) * 4, :], vas[:], r=["vas"], lane="va")
            if t >= SOWN // NT or not QSIDE:
                continue
            for a in range(4):
                b, kb_ = colmm(QA0 + a * 128, 128)
                e_, ek = evac(b, slice(0, 128), kb_, scale=0.125)
                p.dma("sp", g["qaT"][:, 2 * a, T0:T0 + NT], e_[0:64, :], r=[ek], lane=ek)
                p.dma("sp", g["qaT"][:, 2 * a + 1, T0:T0 + NT], e_[64:128, :], r=[ek], lane=ek)
            for a in range(4):
                b, kb_ = colmm(QI0 + a * 128, 128)
                e_, ek = evac(b, slice(0, 128), kb_)
                p.dma("sp", g["qiT"][:, 2 * a, T0:T0 + NT], e_[0:64, :], r=[ek], lane=ek)
                p.dma("sp", g["qiT"][:, 2 * a + 1, T0:T0 + NT], e_[64:128, :], r=[ek], lane=ek)
            b, kb_ = bk.get()
            for sub in range(4):
                for c in range(8):
                    p.mm(b[:, sub * 8:(sub + 1) * 8], hb[:, c, sub * 128:(sub + 1) * 128], win[:, c, WI0:WI0 + 8], c == 0, c == 7,
                         r=["hb", wk[c]], w=[kb_])
            p.ts("dve", wis[:], b[:, 0:32].rearrange("p (s d) -> p s d", s=4), 1.0 / (8.0 * 8.0 ** 0.5), None, ALU.mult, r=[kb_], w=["wis"])
            p.dma("sp", g["widx"].rearrange("(n p) d -> p n d", p=128)[:, t * 4:(t + 1) * 4, :], wis[:], r=["wis"], lane="widx")
            cb = [colmm(CQ0 + c3 * 128, 128) for c3 in range(3)]
            for c3 in range(3):
                p.act(sq[:, c3, :], cb[c3][0][:], AF.Square, r=[cb[c3][1]], w=["sq"])
            rms_rstd(p, ps[0], lambda c: sq[:, c, :], 3, ones, epsc, rs, NT, 1.0 / 384, ["sq"], "ps0")
            for c3 in range(3):
                p.stt("dve", cn[:, c3, :], cb[c3][0][:], gcq[:, c3:c3 + 1], rs[:], ALU.mult, ALU.mult, r=[cb[c3][1], "rs", "gcq"], w=["cn"])
            s96 = 96.0 ** -0.5
            for h in range(8):
                ba, ka = bk.get()
                for c3 in range(3):
                    p.mm(ba[0:96, :], wuq[:, c3, h * 96:(h + 1) * 96], cn[:, c3, :], c3 == 0, c3 == 2, r=["cn", "wuq"], w=[ka])
                bb, kb2 = bk.get()
                for c3 in range(3):
                    p.mm(bb[0:96, :], wuqs[:, c3, h * 96:(h + 1) * 96], cn[:, c3, :], c3 == 0, c3 == 2, r=["cn", "wuqs"], w=[kb2])
                q_, qk = qbs[h % 2], f"qbs{h%2}"
                p.ts("dve", q_[0:64, :], ba[0:64, :], s96, None, ALU.mult, r=[ka], w=[qk])
                rope(ba, ka, bb, kb2, q_, qk, s96)
                p.dma("sp", g["qbT"][:, h, T0:T0 + NT], q_[0:96, :], r=[qk], lane=f"qb{h%2}")
            for m in range(16):
                b, kb_ = colmm(GT0 + m * 128, 128)
                p.act(gs[m % 2][:], b[:], AF.Sigmoid, r=[kb_], w=[f"gs{m%2}"])
                p.dma("sp", g["gT"][m * 128:(m + 1) * 128, T0:T0 + NT], gs[m % 2][:], r=[f"gs{m%2}"], lane=f"gs{m%2}")
        p.emit()


def attn_phase(nc, g, ps, identb, mode):
    NB = 32
    with contextlib.ExitStack() as es:
        sb = lambda n, s, d=F32: es.enter_context(nc.sbuf_tensor("p3" + mode + n, list(s), d))
        dsa = mode == "dsa"
        SD = S if dsa else 2
        SM = 2 if dsa else S
        kiT = sb("kiT", [64, SD], BF16); kaT = sb("kaT", [64, SD], BF16)
        va = sb("va", [128, 64 if dsa else 1, 65], BF16); vb = sb("vb", [128, 1 if dsa else 64, 8 if dsa else 8 * 65], BF16)
        kb = [sb(f"kb{i}", [96, SM], BF16) for i in range(2)]
        Isc2 = [sb(f"Isc{i}", [128, SD]) for i in range(2)]
        nm2 = [sb(f"nm{i}", [128, SD], BF16) for i in range(2)]
        junk = sb("junk", [128, SD], mybir.dt.uint8)
        I4 = sb("I4", [128, 512], BF16); sel = sb("sel", [65, 64], BF16)
        dhi = sb("dhi", [65, 512], BF16); dlo = sb("dlo", [65, 512], BF16)
        qi2 = [sb(f"qi{i}", [64, 8, 128], BF16) for i in range(2)]
        qa2 = [sb(f"qa{i}", [64, 8, 128], BF16) for i in range(2)]
        wq2 = [sb(f"wq{i}", [128, 8]) for i in range(2)]
        Dh2 = [sb(f"Dh{i}", [128, 8, 128], BF16) for i in range(2)]
        rl = [sb(f"rl{i}", [128, 8, 512 if dsa else 2], BF16) for i in range(2)]
        cmq2 = [sb(f"cmq{i}", [128, 256]) for i in range(2)]
        pw = sb("pw", [128, NBIS + 1])
        hk2 = [sb(f"hk{i}", [128, NBIS + 1]) for i in range(2)]
        h22 = [sb(f"h2{i}", [128, NBIS + 1]) for i in range(2)]
        sm2 = [sb(f"sm{i}", [128, 8]) for i in range(2)]
        pt = [sb(f"pt{i}", [128, 1024], BF16) for i in range(3)]
        osb = sb("osb", [65, 1024]); rden = sb("rden", [64, 1024])
        oo = sb("oo", [64, 1024], BF16)
        qb = sb("qb", [96, 8, 2 if dsa else 512], BF16)
        cmk = sb("cmk", [128, 8, 2 if dsa else 512], BF16)
        Bt = sb("Bt", [128, 3, 1024], BF16)
        rb = sb("rb", [128, 32, 8]); dl = sb("dl", [128, len(TH), 8])
        pqi = sb("pqi", [128, 128], I32); pki = sb("pki", [128, 3], I32)
        pqf = sb("pqf", [128, 128]); pkf = sb("pkf", [128, 3])
        rel = sb("rel", [128, 128]); ind = sb("ind", [128, 128]); bacc = sb("bacc", [128, 4, 128])
        p = Phase(nc, "p3" + mode)
        if dsa:
            p.dma("sp", kiT[:], g["kiT"], w=["kiT"], lane="kiT")
            p.dma("sp", kaT[:], g["kaT"], w=["kaT"], lane="kaT")
            vav = g["va"].rearrange("(n p) d -> p n d", p=128)
            for q4 in range(16):
                p.dma("sp", va[:, q4 * 4:(q4 + 1) * 4, :], vav[:, q4 * 4:(q4 + 1) * 4, :], w=["va"], lane="va")
        else:
            vbv = g["vb"].rearrange("(n p) h d -> p n (h d)", p=128)
            for q4 in range(16):
                p.dma("sp", vb[:, q4 * 4:(q4 + 1) * 4, :], vbv[:, q4 * 4:(q4 + 1) * 4, :], w=["vb"], lane="vbl")
        for q4 in range(4):
            p.copy("dve", I4[:, q4 * 128:(q4 + 1) * 128], identb, r=["ident"], w=["I4"])
        p.memset("dve", sel[:], 0.0, w=["sel"])
        p.memset("dve", sel[64:65, :], 1.0, w=["sel"])
        for k in range(NBIS + 1):
            p.memset("dve", pw[:, k:k + 1], 2.0 ** -(k + 1), w=["pw"])
        if dsa:
            p.dma("sp", rb[:], g["rb128"], w=["rb"], lane="rb")
            p.dma("sp", pqi[:], g["posq_bc"], w=["pqi"], lane="pqi")
            p.dma("sp", pki[:], g["posk_col"], w=["pki"], lane="pki")
            p.copy("dve", pqf[:], pqi[:], r=["pqi"], w=["pqf"])
            p.copy("dve", pkf[:], pki[:], r=["pki"], w=["pkf"])
            prev = 15
            for j, (th, nb_) in enumerate(TH):
                p.tt("dve", dl[:, j, :], rb[:, nb_, :], rb[:, prev, :], ALU.subtract, r=["rb"], w=["dl"])
                prev = nb_
            for ty in range(3):
                p.ts("dve", rel[:], pqf[:], pkf[:, ty:ty + 1], -1.0, ALU.subtract, ALU.mult, r=["pqf", "pkf"], w=["rel"])
                for hh in range(2):
                    p.memset("pool", bacc[:], 0.0, w=["bacc"] + [f"bacc{h}" for h in range(4)])
                    for j, (th, nb_) in enumerate(TH):
                        p.ts("dve", ind[:], rel[:], float(th), None, ALU.is_ge, r=["rel"], w=["ind"])
                        for h in range(4):
                            p.stt("dve", bacc[:, h, :], ind[:], dl[:, j, hh * 4 + h:hh * 4 + h + 1], bacc[:, h, :], ALU.mult, ALU.add,
                                  r=["ind", "dl", f"bacc{h}"], w=[f"bacc{h}"])
                    p.copy("dve", Bt[:, ty, hh * 512:(hh + 1) * 512], bacc[:].rearrange("p h q -> p (h q)"),
                           r=["bacc"] + [f"bacc{h}" for h in range(4)], w=["Bt", "bacc"])

        bkI = Banks(ps, [0, 1, 2, 3])

        def normalize(accs, width, dst, dkey, dma_fn):
            for hf_, (b, kb_) in enumerate(accs):
                p.copy("act", osb[:, hf_ * 512:(hf_ + 1) * 512], b[0:65, :], r=[kb_], w=["osb"])
            for hf_ in range(len(accs)):
                b, kb_ = ps[6 + hf_ % 2], f"ps{6 + hf_ % 2}"
                p.copy("dve", dhi[:], osb[:, hf_ * 512:(hf_ + 1) * 512], r=["osb"], w=["dhi"])
                p.tt("dve", dlo[:], osb[:, hf_ * 512:(hf_ + 1) * 512], dhi[:], ALU.subtract, r=["osb", "dhi"], w=["dlo"])
                p.mm(b[0:64, :], sel[:], dhi[:], True, False, r=["dhi", "sel"], w=[kb_])
                p.mm(b[0:64, :], sel[:], dlo[:], False, True, r=["dlo", "sel"], w=[kb_])
                p.add("dve", lambda e, b=b, hf_=hf_: e.reciprocal(out=rden[:, hf_ * 512:(hf_ + 1) * 512], in_=b[0:64, :]), r=[kb_], w=["rden"])
            p.tt("dve", oo[:, :width], osb[0:64, :width], rden[:, :width], ALU.mult, r=["osb", "rden"], w=["oo"])
            dma_fn()

        kbcount = [0]

        def dsa_A(i):
            sl = i % 2
            Q0 = i * 128
            p.dma("sp", qi2[sl][:], g["qiT"][:, :, Q0:Q0 + 128], w=[f"qi{sl}"], lane=f"qi{sl}")
            p.dma("sp", qa2[sl][:], g["qaT"][:, :, Q0:Q0 + 128], w=[f"qa{sl}"], lane=f"qa{sl}")
            p.dma("sp", wq2[sl][:], g["widx"][Q0:Q0 + 128, :], w=[f"wq{sl}"], lane=f"wq{sl}")
            p.dma("sp", cmq2[sl][:], g["cmq"][i], w=[f"cmq{sl}"], lane=f"cmq{sl}")
            qi, wq, Dh, Isc = qi2[sl], wq2[sl], Dh2[sl], Isc2[sl]
            for h in range(8):
                p.ts("dve", Dh[:, h, :], identb, wq[:, h:h + 1], None, ALU.mult, r=["ident", f"wq{sl}"], w=[f"Dh{sl}"])
            groups = []
            for (k0, n) in ((0, (i + 1) * 128), (4096, (i + 1) * 128)):
                o = 0
                while o < n:
                    w_ = min(512, n - o)
                    groups.append((k0 + o, w_))
                    o += w_
            col = 0
            for gi, (k0, w_) in enumerate(groups):
                R_ = rl[gi % 2]
                rk = f"rl{gi%2}"
                for h in range(8):
                    b, kb_ = bkI.get()
                    p.mm(b[:, :w_], qi[:, h, :], kiT[:, k0:k0 + w_], True, True, r=[f"qi{sl}", "kiT"], w=[kb_])
                    p.act(R_[:, h, :w_], b[:, :w_], AF.Relu, r=[kb_], w=[rk])
                b, kb_ = ps[4 + gi % 2], f"ps{4 + gi % 2}"
                for h in range(8):
                    p.mm(b[:, :w_], Dh[:, h, :], R_[:, h, :w_], h == 0, h == 7, r=[f"Dh{sl}", rk], w=[kb_])
                p.copy("act", Isc[:, col:col + w_], b[:, :w_], r=[kb_], w=[f"Isc{sl}"])
                col += w_

        def dsa_B(i, part):
            sl = i % 2
            W = 2 * (i + 1) * 128
            Isc, nm, sm, hk, h2, cmq = Isc2[sl], nm2[sl], sm2[sl], hk2[sl], h22[sl], cmq2[sl]
            ik, sk, hkk = f"Isc{sl}", f"sm{sl}", f"hk{sl}"
            split = NBIS // 3
            if part == 0:
                p.add("dve", lambda e, W=W: e.tensor_reduce(out=sm[:, 0:1], in_=Isc[:, :W], axis=AX.X, op=ALU.max, apply_absolute_value=True),
                      r=[ik], w=[sk])
                c_own = i * 128
                c_oth = (i + 1) * 128 + i * 128
                p.tt("dve", Isc[:, c_own:c_own + 128], Isc[:, c_own:c_own + 128], cmq[:, 0:128], ALU.add, r=[ik, f"cmq{sl}"], w=[ik])
                p.tt("dve", Isc[:, c_oth:c_oth + 128], Isc[:, c_oth:c_oth + 128], cmq[:, 128:256], ALU.add, r=[ik, f"cmq{sl}"], w=[ik])
                p.ts("dve", sm[:, 1:2], sm[:, 0:1], 2.02, 2e-6, ALU.mult, ALU.add, r=[sk], w=[sk])
                p.ts("dve", hk[:], pw[:], sm[:, 1:2], None, ALU.mult, r=["pw", sk], w=[hkk])
                p.ts("dve", h2[:], hk[:], 2.0, None, ALU.mult, r=[hkk], w=[hkk])
                p.ts("dve", sm[:, 2:3], sm[:, 1:2], 0.0, None, ALU.mult, r=[sk], w=[sk])
            for k in (range(0, split) if part == 0 else range(split, NBIS)):
                p.ts("dve", junk[:, :W], Isc[:, :W], sm[:, 2:3], None, ALU.is_ge, ALU.add, r=[ik, sk], w=["junk", f"cnt{sl}"], accum_out=sm[:, 3:4])
                p.ts("dve", sm[:, 4:5], sm[:, 3:4], float(TOPK), h2[:, k + 1:k + 2], ALU.is_ge, ALU.mult, r=[f"cnt{sl}", hkk], w=[f"tmp{sl}"])
                p.stt("dve", sm[:, 2:3], sm[:, 4:5], hk[:, k + 1:k + 2], sm[:, 2:3], ALU.subtract, ALU.add, r=[f"tmp{sl}", sk, hkk], w=[sk])
            if part == 1:
                p.ts("dve", nm[:, :W], Isc[:, :W], sm[:, 2:3], NEG, ALU.is_lt, ALU.mult, r=[ik, sk], w=[f"nm{sl}"])

        def dsa_C(i, part):
            sl = i % 2
            Q0 = i * 128
            nk = 2 * (i + 1)
            blocks = list(range(i + 1)) + list(range(32, 32 + i + 1))
            qa, nm = qa2[sl], nm2[sl]
            accs = [(ps[4], "ps4"), (ps[5], "ps5")]

            def s_stage(c, L):
                near = None
                if L == i:
                    near = 0
                elif L == 32 + i - 1:
                    near = 1
                elif L == 32 + i:
                    near = 2
                P_ = pt[c % 3]
                pk = f"pt{c%3}"
                for hf_ in range(2):
                    b, kb_ = ps[(c % 2) * 2 + hf_], f"ps{(c % 2) * 2 + hf_}"
                    p.mm(b[:], kaT[:, L * 128:(L + 1) * 128], qa[:, hf_ * 4:(hf_ + 1) * 4, :].rearrange("d h q -> d (h q)"), True, False,
                         r=["kaT", f"qa{sl}"], w=[kb_])
                    p.mm(b[:], nm[:, c * 128:(c + 1) * 128], I4[:], False, near is None, r=[f"nm{sl}", "I4"], w=[kb_])
                    if near is not None:
                        p.mm(b[:], identb, Bt[:, near, hf_ * 512:(hf_ + 1) * 512], False, True, r=["ident", "Bt"], w=[kb_])
                    p.act(P_[:, hf_ * 512:(hf_ + 1) * 512], b[:], AF.Exp, r=[kb_], w=[pk])

            def pv_stage(c, L):
                P_ = pt[c % 3]
                pk = f"pt{c%3}"
                for hf_ in range(2):
                    b, kb_ = accs[hf_]
                    p.mm(b[0:65, :], va[:, L, :], P_[:, hf_ * 512:(hf_ + 1) * 512], c == 0, c == nk - 1, r=["va", pk], w=[kb_])

            if part == 0:
                s_stage(0, blocks[0])
                for c, L in enumerate(blocks):
                    if c + 1 < nk:
                        s_stage(c + 1, blocks[c + 1])
                    pv_stage(c, L)
                return

            def dma_oa(Q0=Q0):
                p.dma("sp", g["oaT"][:, :, Q0:Q0 + 128], oo[:, :].rearrange("d (h q) -> d h q", h=8), r=["oo"], lane="oa")
            normalize(accs, 1024, oo, "oo", dma_oa)

        def kb_load(j, h, slot):
            nb_own = 4 * j + 4
            K_, kk_ = kb[slot], f"kb{slot}"
            p.dma("sp", K_[:, 0:nb_own * 128], g["kbT"][h, :, 0:nb_own * 128], w=[kk_], lane=kk_ + "a")
            p.dma("sp", K_[:, nb_own * 128:2 * nb_own * 128], g["kbT"][h, :, 4096:4096 + nb_own * 128], w=[kk_], lane=kk_ + "b")

        def mla_tile(j):
            T0 = j * NT
            nb_own = 4 * j + 4
            tblocks = list(range(nb_own)) + list(range(32, 32 + nb_own))
            n_ = len(tblocks)
            p.dma("sp", qb[:], g["qbT"][:, :, T0:T0 + NT], w=["qb"], lane="qb")
            p.dma("pool", cmk[:], g["cmk"][j], w=["cmk"], lane="cmk")
            if j == 0:
                kb_load(0, 0, 0)
            for h in range(8):
                slot = (j * 8 + h) % 2
                K_, kk_ = kb[slot], f"kb{slot}"
                if h + 1 < 8:
                    kb_load(j, h + 1, 1 - slot)
                elif j + 1 < 8:
                    kb_load(j + 1, 0, 1 - slot)
                acc = (ps[4 + h % 2], f"ps{4 + h % 2}")

                def s_stage(c, L):
                    b, kb_ = ps[c % 4], f"ps{c % 4}"
                    mi = None
                    if 4 * j <= L < 4 * j + 4:
                        mi = L - 4 * j
                    elif 32 + 4 * j <= L < 32 + 4 * j + 4:
                        mi = 4 + L - 32 - 4 * j
                    p.mm(b[:], K_[:, c * 128:(c + 1) * 128], qb[:, h, :], True, mi is None, r=[kk_, "qb"], w=[kb_])
                    if mi is not None:
                        p.mm(b[:], identb, cmk[:, mi, :], False, True, r=["ident", "cmk"], w=[kb_])
                    p.act(pt[c % 3][:, 0:512], b[:], AF.Exp, r=[kb_], w=[f"pt{c%3}"])

                def pv_stage(c, L):
                    p.mm(acc[0][0:65, :], vb[:, L, h * 65:(h + 1) * 65], pt[c % 3][:, 0:512], c == 0, c == n_ - 1, r=["vb", f"pt{c%3}"], w=[acc[1]])

                s_stage(0, tblocks[0])
                s_stage(1, tblocks[1])
                for c, L in enumerate(tblocks):
                    if c + 2 < n_:
                        s_stage(c + 2, tblocks[c + 2])
                    pv_stage(c, L)

                def dma_ob(h=h, T0=T0):
                    p.dma("sp", g["obT"][:, h, T0:T0 + NT], oo[:, 0:512], r=["oo"], lane="ob")
                normalize([acc], 512, oo, "oo", dma_ob)

        nblk = min(NB, NBLK)
        if dsa:
            dsa_A(0)
            dsa_B(0, 0)
            dsa_B(0, 1)
            if nblk > 1:
                dsa_A(1)
            for i in range(nblk):
                dsa_C(i, 0)
                if i + 1 < nblk:
                    dsa_B(i + 1, 0)
                dsa_C(i, 1)
                if i + 1 < nblk:
                    dsa_B(i + 1, 1)
                if i + 2 < nblk:
                    dsa_A(i + 2)
        else:
            for i in range(nblk):
                if i % 4 == 3:
                    mla_tile(i // 4)
        p.emit()


def merge_phase(nc, g, x1T, x2T, ps, G2):
    with contextlib.ExitStack() as es:
        sb = lambda n, s, d=F32: es.enter_context(nc.sbuf_tensor("p4" + n, list(s), d))
        woa = sb("woa", [64, 8, D], BF16); wob = sb("wob", [64, 8, D], BF16); wout = sb("wout", [128, 8, D], BF16)
        oa = sb("oa", [64, 8, NT], BF16); ob = sb("ob", [64, 8, NT], BF16)
        gt = sb("gt", [128, 16, NT]); xs = sb("xs", [128, 8, NT])
        y = sb("y", [128, 8, NT], BF16); t1 = sb("t1", [128, NT]); t2 = sb("t2", [128, NT])
        p = Phase(nc, "p4")
        p.dma("pool", woa[:], g["w_o_a"].rearrange("(h d) n -> d h n", d=64), w=["woa"], lane="woa", max_dma_last_dim=4096)
        p.dma("pool", wob[:], g["w_o_b"].rearrange("(h d) n -> d h n", d=64), w=["wob"], lane="wob", max_dma_last_dim=4096)
        p.dma("pool", wout[:], g["w_out"].rearrange("(c p) n -> p c n", p=128), w=["wout"], lane="wout", max_dma_last_dim=4096)
        x1v = x1T.rearrange("(c p) t -> p c t", p=128)
        x2v = x2T.rearrange("(c p) t -> p c t", p=128)
        gv = g["gT"].rearrange("(c p) t -> p c t", p=128)
        for t in range(SOWN // NT):
            T0 = t * NT
            p.dma("sp", oa[:], g["oaT"][:, :, T0:T0 + NT], w=["oa"], lane="oa")
            p.dma("sp", ob[:], g["obT"][:, :, T0:T0 + NT], w=["ob"], lane="ob")
            p.dma("sp", gt[:], gv[:, :, T0:T0 + NT], w=["gt"], lane="gt")
            p.dma("sp", xs[:], x1v[:, :, T0:T0 + NT], w=["xs"], lane="xs")
            for m in range(8):
                ba, ka = ps[m % 2], f"ps{m%2}"
                bb, kb_ = ps[2 + m % 2], f"ps{2 + m%2}"
                for h in range(8):
                    p.mm(ba[:], woa[:, h, m * 128:(m + 1) * 128], oa[:, h, :], h == 0, h == 7, r=["woa", "oa"], w=[ka])
                for h in range(8):
                    p.mm(bb[:], wob[:, h, m * 128:(m + 1) * 128], ob[:, h, :], h == 0, h == 7, r=["wob", "ob"], w=[kb_])
                p.tt("dve", t1[:], ba[:], gt[:, m, :], ALU.mult, r=[ka, "gt"], w=["t1"])
                p.tt("dve", t2[:], bb[:], gt[:, 8 + m, :], ALU.mult, r=[kb_, "gt"], w=["t2"])
                p.tt("dve", y[:, m, :], t1[:], t2[:], ALU.add, r=["t1", "t2"], w=[f"y{m}"])
            for m in range(8):
                b, kb_ = ps[4 + m % 2], f"ps{4 + m%2}"
                for c in range(8):
                    p.mm(b[:], wout[:, c, m * 128:(m + 1) * 128], y[:, c, :], c == 0, c == 7, r=["wout", f"y{c}"], w=[kb_])
                p.stt("dve", xs[:, m, :], b[:], G2[:, m:m + 1], xs[:, m, :], ALU.mult, ALU.add, r=[kb_, "xs"], w=["xs"])
            p.dma("sp", x2v[:, :, T0:T0 + NT], xs[:], r=["xs"], lane="st")
        p.emit()


def build(stage=99, debug=False):
    nc = bass.Bass("TRN2", target_bir_lowering=False)
    dt = lambda n, s, d=F32: nc.dram_tensor(n, list(s), d, kind="ExternalInput").ap()
    xT = dt("xT", [D, S])
    cvec = dt("cvec", [128, 8])
    w_ada = dt("w_ada", [D, 9 * D])
    b_ada = dt("b_ada", [128, 72])
    g_ffn1 = dt("g_ffn1", [128, 8]); g_mix = dt("g_mix", [128, 8]); g_ffn2 = dt("g_ffn2", [128, 8]); g_final = dt("g_final", [128, 8])
    w1i = dt("w_ffn1_in", [D, 2 * DFF]); w1d = dt("w_ffn1_down", [DFF, D])
    w2i = dt("w_ffn2_in", [D, 2 * DFF]); w2d = dt("w_ffn2_down", [DFF, D])
    ident_d = dt("ident", [128, 128])
    g = {}
    g["winP"] = dt("winP", [D, WTOT])
    g["w_uk"] = dt("w_uk", [256, 512]); g["w_uv"] = dt("w_uv", [256, 512])
    g["w_uq"] = dt("w_uq", [384, 768]); g["w_uqs"] = dt("w_uqs", [384, 768])
    g["g_ckv"] = dt("g_ckv", [128, 2]); g["g_cq"] = dt("g_cq", [128, 3])
    g["freqc"] = dt("freqc", [128, 1]); g["sgnc"] = dt("sgnc", [128, 1])
    g["pos32"] = dt("pos32", [32, S], I32)
    g["rb128"] = dt("rb128", [128, 32, 8])
    g["posq_bc"] = dt("posq_bc", [128, 128], I32); g["posk_col"] = dt("posk_col", [128, 3], I32)
    g["cmq"] = dt("cmq", [32, 128, 256]); g["cmk"] = dt("cmk", [8, 128, 8, 512])
    g["w_o_a"] = dt("w_o_a", [512, D]); g["w_o_b"] = dt("w_o_b", [512, D]); g["w_out"] = dt("w_out", [D, D])
    outT = nc.dram_tensor("outT", [D, SOWN], F32, kind="ExternalOutput").ap()
    dbgset = set(debug.split(",")) if debug else set()
    it = lambda n, s, d=F32: nc.dram_tensor(n, list(s), d, kind=("ExternalOutput" if n in dbgset else "Internal")).ap()
    x1T = it("x1T", [D, S]); x2T = it("x2T", [D, SOWN])
    g["kaT"] = it("kaT", [64, S], BF16); g["kiT"] = it("kiT", [64, S], BF16)
    g["kbT"] = it("kbT", [8, 96, S], BF16); g["vb"] = it("vb", [S, 8, 65], BF16); g["va"] = it("va", [S, 65], BF16)
    g["qaT"] = it("qaT", [64, 8, SOWN], BF16); g["qiT"] = it("qiT", [64, 8, SOWN], BF16)
    g["widx"] = it("widx", [SOWN, 8]); g["qbT"] = it("qbT", [96, 8, SOWN], BF16)
    g["gT"] = it("gT", [2048, SOWN])
    g["oaT"] = it("oaT", [64, 8, SOWN], BF16); g["obT"] = it("obT", [64, 8, SOWN], BF16)

    with contextlib.ExitStack() as es:
        ps = [es.enter_context(nc.psum_tensor(f"psb{i}", [128, 512], F32)) for i in range(8)]
        sb = lambda n, s, d=F32: es.enter_context(nc.sbuf_tensor(n, list(s), d))
        ones_t = sb("ones", [128, 128], BF16); ones = ones_t[:]
        epsc_t = sb("epsc", [128, 1]); epsc = epsc_t[:]
        identb_t = sb("identb", [128, 128], BF16); identb = identb_t[:]
        modT_t = sb("modT", [128, 72]); modT = modT_t[:]
        drv_t = sb("drv", [128, 10, 8])
        derived = [drv_t[:, i, :] for i in range(9)]
        gfin = drv_t[:, 9, :]
        A1, S1, G1, A2, S2, G2, A3, S3, G3 = derived

        setup_phase(nc, cvec, w_ada, b_ada, [g_ffn1, g_mix, g_ffn2], modT, None, ones, epsc, ident_d, identb, ps, derived)
        pp = Phase(nc, "gfin")
        pp.dma("sp", gfin, g_final, w=["gf"], lane="gf")
        pp.emit()
        if stage == 0:
            return nc
        if stage == 20:
            proj_phase(nc, x1T, g, ps, ones, epsc, A2, S2)
            return nc
        if stage in (30, 31):
            import os
            global NBLK
            NBLK = int(os.environ.get("NBLK", "32"))
            attn_phase(nc, g, ps, identb, "dsa" if stage == 30 else "mla")
            return nc
        ffn_phase(nc, "f1", xT, x1T, S // NT, w1i, w1d, A1, S1, G1, ps, ones, epsc)
        if stage == 1:
            ffn_phase(nc, "f2", x1T[:, 0:SOWN], outT, SOWN // NT, w2i, w2d, A3, S3, G3, ps, ones, epsc, gfin=gfin)
            return nc
        proj_phase(nc, x1T, g, ps, ones, epsc, A2, S2)
        if stage == 2:
            return nc
        attn_phase(nc, g, ps, identb, "dsa")
        if stage == 3:
            return nc
        attn_phase(nc, g, ps, identb, "mla")
        if stage == 4:
            return nc
        merge_phase(nc, g, x1T, x2T, ps, G2)
        ffn_phase(nc, "f2", x2T, outT, SOWN // NT, w2i, w2d, A3, S3, G3, ps, ones, epsc, gfin=gfin)
    return nc


def local_perm(p):
    own = np.arange(32) * 2 + p
    oth = np.arange(32) * 2 + 1 - p
    blocks = np.concatenate([own, oth])
    return (blocks[:, None] * 128 + np.arange(128)[None, :]).reshape(-1)


def pm(v):
    v = np.asarray(v, np.float32)
    return np.ascontiguousarray(v.reshape(-1, 128).T)


FREQ16 = [1.0, 0.5623413324356079, 0.3162277638912201, 0.17782793939113617, 0.10000000149011612, 0.05623413249850273,
          0.03162277489900589, 0.017782794311642647, 0.009999999776482582, 0.005623413249850273, 0.003162277629598975,
          0.0017782794311642647, 0.0010000000474974513, 0.000562341301701963, 0.0003162277571391314, 0.00017782794020604342]


def host_consts(p):
    perm = local_perm(p)
    lim = (perm // 64 + 1) * 64
    cmq = np.zeros((32, 128, 256), np.float32)
    for i in range(32):
        ql = lim[i * 128:(i + 1) * 128][:, None]
        for half, kbk in ((0, i), (1, 32 + i)):
            kt = perm[kbk * 128:(kbk + 1) * 128][None, :]
            cmq[i, :, half * 128:(half + 1) * 128] = np.where(kt < ql, 0.0, -1e30)
    cmk = np.zeros((8, 128, 8, 512), np.float32)
    for j in range(8):
        ql = lim[j * 512:(j + 1) * 512][None, :]
        for mi in range(8):
            kbk = 4 * j + mi if mi < 4 else 32 + 4 * j + (mi - 4)
            kt = perm[kbk * 128:(kbk + 1) * 128][:, None]
            cmk[j, :, mi, :] = np.where(kt < ql, 0.0, NEG)
    freqc = np.zeros((128, 1), np.float32)
    sgnc = np.zeros((128, 1), np.float32)
    for r in range(32):
        freqc[64 + r, 0] = FREQ16[r % 16]
        sgnc[64 + r, 0] = -1.0 if r < 16 else 1.0
    return perm, cmq, cmk, freqc, sgnc


def kernel(**inputs):
    import os
    stage = int(os.environ.get("KSTAGE", "99"))
    debug = os.environ.get("KDEBUG", "")
    f = lambda a: np.ascontiguousarray(np.asarray(a, np.float32))
    x = np.asarray(inputs["x"], np.float32)
    w_in = np.asarray(inputs["w_in"][0], np.float32)
    q_a, k_a, v_a = w_in[:, 0:512], w_in[:, 512:576], w_in[:, 576:640]
    q_i, k_i, w_i = w_in[:, 640:1152], w_in[:, 1152:1216], w_in[:, 1216:1224]
    c_q, c_kv, k_r, gts = w_in[:, 1224:1608], w_in[:, 1608:1864], w_in[:, 1864:1896], w_in[:, 1896:3944]
    k_rs = np.concatenate([k_r[:, 16:32], k_r[:, 0:16]], axis=1)
    winP = np.ascontiguousarray(np.concatenate([k_a, k_i, c_kv, k_a, k_r, k_a, k_rs, v_a, q_a, q_i, c_q, gts, w_i], axis=1))
    assert winP.shape[1] == WTOT
    w_uq = np.asarray(inputs["w_uq"][0], np.float32)
    w_uqs = w_uq.copy().reshape(384, 8, 96)
    w_uqs[:, :, 64:80], w_uqs[:, :, 80:96] = w_uq.reshape(384, 8, 96)[:, :, 80:96], w_uq.reshape(384, 8, 96)[:, :, 64:80]
    w_uqs = np.ascontiguousarray(w_uqs.reshape(384, 768))
    nc = build(stage, debug)
    in_maps = []
    perms = []
    pos_all = np.asarray(inputs["positions"], np.int32)
    rb128 = np.ascontiguousarray(np.broadcast_to(f(inputs["rel_bias"])[None], (128, 32, 8)))
    consts = [host_consts(0), host_consts(1)]
    for core in range(NCORES):
        b, p = core // 2, core % 2
        perm, cmq, cmk, freqc, sgnc = consts[p]
        perms.append(perm)
        posl = pos_all[b][perm]
        m = {
            "xT": np.ascontiguousarray(x[b][perm].T),
            "cvec": pm(inputs["c"][b]),
            "w_ada": f(inputs["w_ada"][0]),
            "b_ada": pm(inputs["b_ada"][0]),
            "g_ffn1": pm(inputs["g_ffn1"][0]),
            "g_mix": pm(inputs["g_mix"][0]),
            "g_ffn2": pm(inputs["g_ffn2"][0]),
            "g_final": pm(inputs["g_final"]),
            "w_ffn1_in": f(inputs["w_ffn1_in"][0]),
            "w_ffn1_down": f(inputs["w_ffn1_down"][0]),
            "w_ffn2_in": f(inputs["w_ffn2_in"][0]),
            "w_ffn2_down": f(inputs["w_ffn2_down"][0]),
            "ident": np.eye(128, dtype=np.float32),
            "winP": winP, "w_uk": f(inputs["w_uk"][0]), "w_uv": f(inputs["w_uv"][0]), "w_uq": w_uq, "w_uqs": w_uqs,
            "g_ckv": pm(inputs["g_ckv"][0]), "g_cq": pm(inputs["g_cq"][0]),
            "freqc": freqc, "sgnc": sgnc,
            "pos32": np.ascontiguousarray(np.broadcast_to(posl[None], (32, S))),
            "rb128": rb128,
            "posq_bc": np.ascontiguousarray(np.broadcast_to(posl[128:256][None], (128, 128))),
            "posk_col": np.ascontiguousarray(np.stack([posl[128:256], posl[32 * 128:33 * 128], posl[33 * 128:34 * 128]], axis=1)),
            "cmq": cmq, "cmk": cmk,
            "w_o_a": f(inputs["w_o_a"][0]), "w_o_b": f(inputs["w_o_b"][0]), "w_out": f(inputs["w_out"][0]),
        }
        in_maps.append(m)
    res = run_bass_kernel_spmd(nc, in_maps, core_ids=list(range(NCORES)))
    if debug:
        kernel.debug = res.results
        kernel.perms = perms
    out = np.empty((4, S, D), np.float32)
    for core in range(NCORES):
        b = core // 2
        o = res.results[core]["outT"]
        out[b][perms[core][:SOWN]] = o.T
    return out
```

```python
import contextlib
import numpy as np
import concourse.bass as bass
import concourse.mybir as mybir
from concourse.bass_utils import run_bass_kernel_spmd

F32 = mybir.dt.float32
BF16 = mybir.dt.bfloat16
I32 = mybir.dt.int32
ALU = mybir.AluOpType
AF = mybir.ActivationFunctionType
AX = mybir.AxisListType

D = 1024
S = 8192
DFF = 2816
NT = 512
EPS = 1e-6
NCORES = 8
SOWN = 4096
TOPK = 256
NBIS = 12
NEG = -30000.0
NBLK = 32


_SEMREG = {}


def _semreg(nc):
    return _SEMREG.setdefault(id(nc), {"cnt": {}, "gen": {}, "sem": {}})


class Phase:
    def __init__(self, nc, name):
        self.nc = nc
        self.name = name
        self.ops = []
        self.lw = {}
        self.rd = {}

    def add(self, eng, fn, r=(), w=(), lane=None):
        i = len(self.ops)
        deps = set()
        for k in r:
            if k in self.lw:
                deps.add(self.lw[k])
        for k in w:
            if k in self.lw:
                deps.add(self.lw[k])
            deps.update(self.rd.get(k, {}).values())
        for k in w:
            self.lw[k] = i
            self.rd[k] = {}
        tag = lane if lane is not None else eng
        for k in r:
            self.rd.setdefault(k, {})[tag] = i
        self.ops.append(dict(eng=eng, fn=fn, deps=deps, lane=lane, inc=False))
        return i

    def dma(self, eng, out, in_, r=(), w=(), lane=None, **kw):
        assert lane is not None
        return self.add(eng, lambda e: e.dma_start(out=out, in_=in_, **kw), r, w, lane=lane)

    def mm(self, out, lhsT, rhs, start, stop, r=(), w=()):
        return self.add("pe", lambda e: e.matmul(out, lhsT, rhs, start=start, stop=stop), r, w)

    def act(self, out, in_, func, r=(), w=(), eng="act", **kw):
        return self.add(eng, lambda e: e.activation(out=out, in_=in_, func=func, **kw), r, w)

    def ts(self, eng, out, in0, s1, s2, op0, op1=None, r=(), w=(), **kw):
        if op1 is None:
            return self.add(eng, lambda e: e.tensor_scalar(out=out, in0=in0, scalar1=s1, scalar2=None, op0=op0, **kw), r, w)
        return self.add(eng, lambda e: e.tensor_scalar(out=out, in0=in0, scalar1=s1, scalar2=s2, op0=op0, op1=op1, **kw), r, w)

    def stt(self, eng, out, in0, scalar, in1, op0, op1, r=(), w=()):
        return self.add(eng, lambda e: e.scalar_tensor_tensor(out=out, in0=in0, scalar=scalar, in1=in1, op0=op0, op1=op1), r, w)

    def tt(self, eng, out, in0, in1, op, r=(), w=()):
        return self.add(eng, lambda e: e.tensor_tensor(out=out, in0=in0, in1=in1, op=op), r, w)

    def copy(self, eng, out, in_, r=(), w=()):
        if eng == "act":
            return self.add(eng, lambda e: e.activation(out=out, in_=in_, func=AF.Copy), r, w)
        return self.add(eng, lambda e: e.tensor_copy(out=out, in_=in_), r, w)

    def memset(self, eng, ap, val, w=()):
        return self.add(eng, lambda e: e.memset(ap, val), (), w)

    def emit(self):
        nc = self.nc
        ops = self.ops

        def skip(dop, op):
            return dop["lane"] is None and op["lane"] is None and dop["eng"] == "pe" and op["eng"] == "pe"

        for op in ops:
            for d in op["deps"]:
                if not skip(ops[d], op):
                    ops[d]["inc"] = True
        last_dma = {}
        last_eng = {}
        for i, op in enumerate(ops):
            if op["lane"] is not None:
                op["inc"] = True
                last_dma[op["lane"]] = i
            else:
                last_eng[op["eng"]] = i
        for i in last_eng.values():
            ops[i]["inc"] = True
        reg = _semreg(nc)
        cnt, gen, semh = reg["cnt"], reg["gen"], reg["sem"]
        for op in ops:
            if not op["inc"]:
                continue
            base = ("L", op["lane"]) if op["lane"] is not None else ("E", op["eng"])
            gen[base] = gen.get(base, 0)
            key = base + (gen[base],)
            cnt[key] = cnt.get(key, 0) + (16 if op["lane"] is not None else 1)
            op["sem"] = key
            op["val"] = cnt[key]
            pk = reg.setdefault("prevkey", {})
            if op["lane"] is not None and base in pk and pk[base] != key:
                op["drain"] = (pk[base], cnt[pk[base]])
            pk[base] = key
            if key not in semh:
                semh[key] = nc.alloc_semaphore(name=f"s_{key[0]}_{key[1]}_{key[2]}")
            if cnt[key] >= (512 if op["lane"] is not None else 4000):
                gen[base] += 1
        sems = semh
        if "bar" not in reg:
            reg["bar"] = nc.alloc_semaphore(name="s_phase_barrier")
            reg["barcnt"] = 0
        reg["barcnt"] += 5
        bar, bartarget = reg["bar"], reg["barcnt"]
        with contextlib.ExitStack() as es:
            block = es.enter_context(nc.Block())

            def run(engname):
                def body(e):
                    waited = {}
                    for op in ops:
                        if op["eng"] != engname:
                            continue
                        for d in sorted(op["deps"]):
                            dop = ops[d]
                            if not dop["inc"] or skip(dop, op):
                                continue
                            sk, sv = dop["sem"], dop["val"]
                            if waited.get(sk, 0) < sv:
                                e.wait_ge(sems[sk], sv)
                                waited[sk] = sv
                        if "drain" in op:
                            e.wait_ge(sems[op["drain"][0]], op["drain"][1])
                        ins = op["fn"](e)
                        if op["inc"]:
                            ins.then_inc(sems[op["sem"]], 16 if op["lane"] is not None else 1)
                    if engname == "sp":
                        for lane, i in last_dma.items():
                            op = ops[i]
                            if waited.get(op["sem"], 0) < op["val"]:
                                e.wait_ge(sems[op["sem"]], op["val"])
                                waited[op["sem"]] = op["val"]
                    if engname in last_eng:
                        op = ops[last_eng[engname]]
                        e.wait_ge(sems[op["sem"]], op["val"])
                    e.sem_inc(bar, 1)
                    e.wait_ge(bar, bartarget)
                return body

            block.tensor(run("pe"))
            block.scalar(run("act"))
            block.vector(run("dve"))
            block.gpsimd(run("pool"))
            block.sync(run("sp"))


def rms_rstd(p, ps_bank, sq_ap, nchunk, ones, epsc, rs, width, inv_n, rkeys, tagw):
    for c in range(nchunk):
        p.mm(ps_bank[:, :width], ones, sq_ap(c), c == 0, c == nchunk - 1, r=rkeys + ["ones"], w=[tagw])
    p.act(rs[:, :width], ps_bank[:, :width], AF.Sqrt, r=[tagw, "epsc"], w=["rs"], bias=epsc, scale=inv_n)
    p.add("dve", lambda e: e.reciprocal(out=rs[:, :width], in_=rs[:, :width]), r=["rs"], w=["rs"])


def ffn_phase(nc, name, xsrc, xdst, ntiles, w_in_d, w_dn_d, Ac, Sc, Gc, ps, ones, epsc, gfin=None):
    NJ = DFF // 128
    with (nc.sbuf_tensor(name + "wi", [128, 8, 2 * DFF], BF16) as wi,
          nc.sbuf_tensor(name + "wd", [128, NJ, D], BF16) as wd,
          nc.sbuf_tensor(name + "xs0", [128, 8, NT], F32) as xs0,
          nc.sbuf_tensor(name + "xs1", [128, 8, NT], F32) as xs1,
          nc.sbuf_tensor(name + "hb", [128, 8, NT], BF16) as hb,
          nc.sbuf_tensor(name + "hf", [128, NJ, NT], BF16) as hf,
          nc.sbuf_tensor(name + "sg0", [128, NT], F32) as sg0,
          nc.sbuf_tensor(name + "sg1", [128, NT], F32) as sg1,
          nc.sbuf_tensor(name + "tf", [128, NT], F32) as tf,
          nc.sbuf_tensor(name + "rs", [128, NT], F32) as rs):
        p = Phase(nc, name)
        xs = [xs0, xs1]
        sg = [sg0, sg1]
        w_in_v = w_in_d.rearrange("(c p) n -> p c n", p=128)
        w_dn_v = w_dn_d.rearrange("(c p) n -> p c n", p=128)
        xsrc_v = xsrc.rearrange("(c p) t -> p c t", p=128)
        xdst_v = xdst.rearrange("(c p) t -> p c t", p=128)
        p.dma("sp", xs[0][:], xsrc_v[:, :, 0:NT], w=["xs0"], lane="ld0")
        for c in range(8):
            p.dma("pool", wi[:, c, :], w_in_v[:, c, :], w=["wi"], lane="wi", max_dma_last_dim=4096)
        for c in range(NJ):
            p.dma("pool", wd[:, c, :], w_dn_v[:, c, :], w=["wd"], lane="wd", max_dma_last_dim=4096)
        wik = ["wi" for c in range(8)]
        wdk = ["wd" for c in range(NJ)]
        for t in range(ntiles):
            s = t % 2
            X = xs[s]
            xk = f"xs{s}"
            if t + 1 < ntiles:
                p.dma("sp", xs[1 - s][:], xsrc_v[:, :, (t + 1) * NT:(t + 2) * NT], w=[f"xs{1-s}"], lane=f"ld{1-s}")
            p.act(hb[:], X[:], AF.Square, r=[xk], w=["hb"])
            rms_rstd(p, ps[0], lambda c: hb[:, c, :], 8, ones, epsc, rs, NT, 1.0 / D, ["hb"], "ps0")
            for c in range(8):
                p.stt("dve", tf[:], X[:, c, :], Ac[:, c:c + 1], rs[:], ALU.mult, ALU.mult, r=[xk, "rs"], w=["tf"])
                p.ts("dve", hb[:, c, :], tf[:], Sc[:, c:c + 1], None, ALU.add, r=["tf"], w=["hb"])
            for j in range(NJ):
                pg, pu = ps[1 + 2 * (j % 2)], ps[2 + 2 * (j % 2)]
                kg, ku = f"ps{1 + 2 * (j % 2)}", f"ps{2 + 2 * (j % 2)}"
                for c in range(8):
                    p.mm(pg[:], wi[:, c, j * 128:(j + 1) * 128], hb[:, c, :], c == 0, c == 7, r=["hb", wik[c]], w=[kg])
                for c in range(8):
                    p.mm(pu[:], wi[:, c, DFF + j * 128:DFF + (j + 1) * 128], hb[:, c, :], c == 0, c == 7, r=["hb", wik[c]], w=[ku])
                p.act(sg[j % 2][:], pg[:], AF.Silu, r=[kg], w=[f"sg{j%2}"])
                p.tt("dve", hf[:, j, :], pu[:], sg[j % 2][:], ALU.mult, r=[ku, f"sg{j%2}"], w=[f"hf{j}"])
            for m in range(8):
                po, ko = ps[5 + m % 2], f"ps{5 + m % 2}"
                for j in range(NJ):
                    p.mm(po[:], wd[:, j, m * 128:(m + 1) * 128], hf[:, j, :], j == 0, j == NJ - 1, r=[f"hf{j}", wdk[j]], w=[ko])
                p.stt("dve", X[:, m, :], po[:], Gc[:, m:m + 1], X[:, m, :], ALU.mult, ALU.add, r=[ko, xk], w=[xk])
            if gfin is not None:
                p.act(hb[:], X[:], AF.Square, r=[xk], w=["hb"])
                rms_rstd(p, ps[0], lambda c: hb[:, c, :], 8, ones, epsc, rs, NT, 1.0 / D, ["hb"], "ps0")
                for c in range(8):
                    p.stt("dve", X[:, c, :], X[:, c, :], gfin[:, c:c + 1], rs[:], ALU.mult, ALU.mult, r=[xk, "rs"], w=[xk])
            p.dma("sp", xdst_v[:, :, t * NT:(t + 1) * NT], X[:], r=[xk], lane=f"st{s}")
        p.emit()


def setup_phase(nc, cvec, w_ada, b_ada, gvecs, modT, sc8, ones, epsc, ident_d, identb, ps, derived):
    with (nc.sbuf_tensor("wada0", [128, 8, 1024], BF16) as wa0,
          nc.sbuf_tensor("wada1", [128, 8, 1024], BF16) as wa1,
          nc.sbuf_tensor("cTs", [128, 8], F32) as cT,
          nc.sbuf_tensor("cTb", [128, 8], BF16) as cTb,
          nc.sbuf_tensor("bT", [128, 72], F32) as bT,
          nc.sbuf_tensor("gTs", [128, 3, 8], F32) as gT):
        p = Phase(nc, "setup")
        wa = [wa0, wa1]
        p.memset("dve", ones, 1.0, w=["ones"])
        p.memset("dve", epsc, EPS, w=["epsc"])
        p.dma("pool", identb, ident_d, w=["ident"], lane="ident")
        p.dma("sp", cT[:], cvec, w=["cT"], lane="c")
        p.dma("sp", bT[:], b_ada, w=["bT"], lane="b")
        for i, g in enumerate(gvecs):
            p.dma("sp", gT[:, i, :], g, w=[f"g{i}"], lane=f"g{i}")
        p.act(cTb[:], cT[:], AF.Silu, r=["cT"], w=["cTb"])
        wv = w_ada.rearrange("(c p) n -> p c n", p=128)
        for g in range(9):
            s = g % 2
            for c in range(8):
                p.dma("pool", wa[s][:, c, :], wv[:, c, g * 1024:(g + 1) * 1024], w=[f"wa{s}"], lane=f"wa{s}",
                      max_dma_last_dim=4096)
            for m in range(8):
                col = g * 8 + m
                for c in range(8):
                    p.mm(ps[0][:, col:col + 1], wa[s][:, c, m * 128:(m + 1) * 128], cTb[:, c:c + 1], c == 0, c == 7,
                         r=[f"wa{s}", "cTb"], w=["ps0"])
        p.tt("dve", modT, ps[0][:, 0:72], bT[:], ALU.add, r=["ps0", "bT"], w=["modT"])
        A1, S1, G1, A2, S2, G2, A3, S3, G3 = derived
        for (A, Sh, G, base, gi, gm) in ((A1, S1, G1, 0, 0, 0.5), (A2, S2, G2, 24, 1, 1.0), (A3, S3, G3, 48, 2, 0.5)):
            p.stt("dve", A, modT[:, base + 8:base + 16], 1.0, gT[:, gi, :], ALU.add, ALU.mult, r=["modT", f"g{gi}"], w=["drv"])
            p.copy("dve", Sh, modT[:, base:base + 8], r=["modT"], w=["drv"])
            p.ts("dve", G, modT[:, base + 16:base + 24], gm, None, ALU.mult, r=["modT"], w=["drv"])
        p.emit()


TWO_PI = 6.283185307179586
CW1 = 6.28125
CW2 = TWO_PI - CW1
MAGIC = 12582912.0
KW = 640
QA0, QI0, CQ0, GT0, WI0, WTOT = 640, 1152, 1664, 2048, 4096, 4104
TH = [(-90, 14), (-63, 13), (-45, 12), (-31, 11), (-22, 10), (-15, 9), (-11, 8), (-7, 7), (-6, 6), (-5, 5), (-4, 4),
      (-3, 3), (-2, 2), (-1, 1), (0, 0), (1, 17), (2, 18), (3, 19), (4, 20), (5, 21), (6, 22), (7, 23), (8, 24),
      (12, 25), (16, 26), (23, 27), (32, 28), (46, 29), (64, 30), (91, 31)]


class Banks:
    def __init__(self, ps, ids):
        self.ps, self.ids, self.i = ps, ids, 0

    def get(self):
        b = self.ids[self.i % len(self.ids)]
        self.i += 1
        return self.ps[b], f"ps{b}"


def proj_phase(nc, x1T, g, ps, ones, epsc, A2, S2):
    import os
    NTL = int(os.environ.get("P2TILES", str(S // NT)))
    QSIDE = os.environ.get("P2Q", "1") == "1"
    with contextlib.ExitStack() as es:
        sb = lambda n, s, d=F32: es.enter_context(nc.sbuf_tensor("p2" + n, list(s), d))
        win = sb("win", [128, 8, WTOT], BF16)
        wuk = sb("wuk", [128, 2, 512], BF16); wuv = sb("wuv", [128, 2, 512], BF16)
        wuq = sb("wuq", [128, 3, 768], BF16); wuqs = sb("wuqs", [128, 3, 768], BF16)
        gck = sb("gck", [128, 2]); gcq = sb("gcq", [128, 3])
        frq = sb("frq", [128, 1]); sgn = sb("sgn", [128, 1])
        xs = [sb("xs0", [128, 8, NT]), sb("xs1", [128, 8, NT])]
        hb = sb("hb", [128, 8, NT], BF16)
        tf = sb("tf", [128, NT]); rs = sb("rs", [128, NT])
        sq = sb("sq", [128, 3, NT], BF16)
        cn = sb("cn", [128, 3, NT], BF16)
        ev = [sb(f"ev{i}", [128, NT], BF16) for i in range(4)]
        gs = [sb(f"gs{i}", [128, NT]) for i in range(2)]
        vbs = [sb(f"vbs{i}", [128, 8, 65], BF16) for i in range(2)]
        vas = sb("vas", [128, 4, 65], BF16)
        wis = sb("wis", [128, 4, 8])
        posi = sb("posi", [128, NT], I32)
        rp = [sb(f"rp{i}", [128, NT]) for i in range(6)]
        Ct = sb("Ct", [128, NT]); St = sb("St", [128, NT])
        kpe = sb("kpe", [128, NT], BF16)
        qbs = [sb(f"qbs{i}", [128, NT], BF16) for i in range(2)]
        p = Phase(nc, "p2")
        bk = Banks(ps, [1, 2, 3, 4, 5, 6, 7])
        R = slice(64, 96)
        x1v = x1T.rearrange("(c p) t -> p c t", p=128)
        p.dma("sp", xs[0][:], x1v[:, :, 0:NT], w=["xs0"], lane="ld0")
        wv = g["winP"].rearrange("(c p) n -> p c n", p=128)
        for c in range(8):
            p.dma("pool", win[:, c, :], wv[:, c, :], w=["win"], lane="win", max_dma_last_dim=4096)
        for nm, t_, d_, nch in (("wuk", wuk, g["w_uk"], 2), ("wuv", wuv, g["w_uv"], 2), ("wuq", wuq, g["w_uq"], 3), ("wuqs", wuqs, g["w_uqs"], 3)):
            dv = d_.rearrange("(c p) n -> p c n", p=128)
            for c in range(nch):
                p.dma("pool", t_[:, c, :], dv[:, c, :], w=[nm], lane=nm)
        p.dma("sp", gck[:], g["g_ckv"], w=["gck"], lane="gck")
        p.dma("sp", gcq[:], g["g_cq"], w=["gcq"], lane="gcq")
        p.dma("sp", frq[:], g["freqc"], w=["frq"], lane="frq")
        p.dma("sp", sgn[:], g["sgnc"], w=["sgn"], lane="sgn")
        for i in range(2):
            p.memset("dve", vbs[i][:], 1.0, w=[f"vbs{i}"])
        p.memset("dve", vas[:], 1.0, w=["vas"])
        wk = ["win" for c in range(8)]
        evi = [0]

        def evac(src, rows, kb_, scale=None, eng=None):
            i = evi[0] % 4
            evi[0] += 1
            e = eng or ("act" if i % 2 == 0 else "dve")
            if scale is None and e == "dve":
                p.copy("dve", ev[i][rows, :], src[rows, :], r=[kb_], w=[f"ev{i}"])
            elif e == "dve":
                p.ts("dve", ev[i][rows, :], src[rows, :], scale, None, ALU.mult, r=[kb_], w=[f"ev{i}"])
            else:
                p.act(ev[i][rows, :], src[rows, :], AF.Copy, r=[kb_], w=[f"ev{i}"], scale=(1.0 if scale is None else scale))
            return ev[i], f"ev{i}"

        def colmm(col0, m):
            b, kb_ = bk.get()
            for c in range(8):
                p.mm(b[0:m, :], win[:, c, col0:col0 + m], hb[:, c, :], c == 0, c == 7, r=["hb", wk[c]], w=[kb_])
            return b, kb_

        for t in range(NTL):
            s = t % 2
            X, xk = xs[s], f"xs{s}"
            T0 = t * NT
            if t + 1 < NTL:
                p.dma("sp", xs[1 - s][:], x1v[:, :, (t + 1) * NT:(t + 2) * NT], w=[f"xs{1-s}"], lane=f"ld{1-s}")
            p.dma("sp", posi[R, :], g["pos32"][:, T0:T0 + NT], w=["posi"], lane="pos")
            p.act(hb[:], X[:], AF.Square, r=[xk], w=["hb"])
            rms_rstd(p, ps[0], lambda c: hb[:, c, :], 8, ones, epsc, rs, NT, 1.0 / D, ["hb"], "ps0")
            for c in range(8):
                p.stt("dve", tf[:], X[:, c, :], A2[:, c:c + 1], rs[:], ALU.mult, ALU.mult, r=[xk, "rs"], w=["tf"])
                p.ts("dve", hb[:, c, :], tf[:], S2[:, c:c + 1], None, ALU.add, r=["tf"], w=["hb"])
            p.copy("dve", rp[0][R, :], posi[R, :], r=["posi"], w=["rp0"])
            p.ts("dve", rp[0][R, :], rp[0][R, :], frq[R, 0:1], None, ALU.mult, r=["rp0", "frq"], w=["rp0"])
            for which, dst in ((0, St), (1, Ct)):
                src = rp[0]
                if which == 1:
                    p.ts("dve", rp[1][R, :], rp[0][R, :], 1.5707963267948966, None, ALU.add, r=["rp0"], w=["rp1"])
                    src = rp[1]
                sk = "rp0" if which == 0 else "rp1"
                p.ts("dve", rp[2][R, :], src[R, :], 1.0 / TWO_PI, None, ALU.mult, r=[sk], w=["rp2"])
                p.ts("dve", rp[3][R, :], rp[2][R, :], MAGIC, None, ALU.add, r=["rp2"], w=["rp3"])
                p.ts("dve", rp[3][R, :], rp[3][R, :], -MAGIC, None, ALU.add, r=["rp3"], w=["rp3"])
                p.stt("dve", rp[4][R, :], rp[3][R, :], -CW1, src[R, :], ALU.mult, ALU.add, r=["rp3", sk], w=["rp4"])
                p.stt("dve", rp[4][R, :], rp[3][R, :], -CW2, rp[4][R, :], ALU.mult, ALU.add, r=["rp3", "rp4"], w=["rp4"])
                if which == 0:
                    p.act(dst[R, :], rp[4][R, :], AF.Sin, r=["rp4", "sgn"], w=["St"], scale=sgn[R, 0:1])
                else:
                    p.act(dst[R, :], rp[4][R, :], AF.Sin, r=["rp4"], w=["Ct"])

            def rope(pa, ka, pb, kb2, outap, okey, scale):
                p.stt("dve", rp[5][R, :], pa[R, :], scale, Ct[R, :], ALU.mult, ALU.mult, r=[ka, "Ct"], w=["rp5"])
                p.stt("dve", rp[2][R, :], pb[R, :], scale, St[R, :], ALU.mult, ALU.mult, r=[kb2, "St"], w=["rp2"])
                p.tt("dve", outap[R, :], rp[5][R, :], rp[2][R, :], ALU.add, r=["rp5", "rp2"], w=[okey])

            b, kb_ = colmm(0, 128)
            e_, ek = evac(b, slice(0, 128), kb_)
            p.dma("sp", g["kaT"][:, T0:T0 + NT], e_[0:64, :], r=[ek], lane=ek)
            p.dma("sp", g["kiT"][:, T0:T0 + NT], e_[64:128, :], r=[ek], lane=ek)
            cb = [colmm(128, 128), colmm(256, 128)]
            for c2 in range(2):
                p.act(sq[:, c2, :], cb[c2][0][:], AF.Square, r=[cb[c2][1]], w=["sq"])
            rms_rstd(p, ps[0], lambda c: sq[:, c, :], 2, ones, epsc, rs, NT, 1.0 / 256, ["sq"], "ps0")
            for c2 in range(2):
                p.stt("dve", cn[:, c2, :], cb[c2][0][:], gck[:, c2:c2 + 1], rs[:], ALU.mult, ALU.mult, r=[cb[c2][1], "rs", "gck"], w=["cn"])
            for a in range(4):
                b, kb_ = bk.get()
                for c2 in range(2):
                    p.mm(b[:], wuk[:, c2, a * 128:(a + 1) * 128], cn[:, c2, :], c2 == 0, c2 == 1, r=["cn", "wuk"], w=[kb_])
                e_, ek = evac(b, slice(0, 128), kb_)
                p.dma("sp", g["kbT"][2 * a, 0:64, T0:T0 + NT], e_[0:64, :], r=[ek], lane=ek)
                p.dma("sp", g["kbT"][2 * a + 1, 0:64, T0:T0 + NT], e_[64:128, :], r=[ek], lane=ek)
            for sub in range(4):
                b, kb_ = bk.get()
                for c2 in range(2):
                    p.mm(b[:], cn[:, c2, sub * 128:(sub + 1) * 128], wuv[:, c2, :], c2 == 0, c2 == 1, r=["cn", "wuv"], w=[kb_])
                v = vbs[sub % 2]
                p.copy("dve", v[:, :, 0:64], b[:].rearrange("p (h d) -> p h d", h=8), r=[kb_], w=[f"vbs{sub%2}"])
                r0 = T0 + sub * 128
                p.dma("sp", g["vb"][r0:r0 + 128, :, :], v[:], r=[f"vbs{sub%2}"], lane=f"vb{sub%2}")
            ba, ka = colmm(384, 96)
            bb, kb2 = colmm(480, 96)
            rope(ba, ka, bb, kb2, kpe, "kpe", 1.0)
            for h in range(8):
                p.dma("sp", g["kbT"][h, 64:96, T0:T0 + NT], kpe[R, :], r=["kpe"], lane="kpe")
            b, kb_ = bk.get()
            for sub in range(4):
                for c in range(8):
                    p.mm(b[:, sub * 64:(sub + 1) * 64], hb[:, c, sub * 128:(sub + 1) * 128], win[:, c, 576:640], c == 0, c == 7,
                         r=["hb", wk[c]], w=[kb_])
            p.copy("dve", vas[:, :, 0:64], b[:, 0:256].rearrange("p (s d) -> p s d", s=4), r=[kb_], w=["vas"])
            p.dma("sp", g["va"].rearrange("(n p) d -> p n d", p=128)[:, t * 4:(t + 1) * 4, :], vas[:], r=["vas"], lane="va")
            if t >= SOWN // NT or not QSIDE:
                continue
            for a in range(4):
                b, kb_ = colmm(QA0 + a * 128, 128)
                e_, ek = evac(b, slice(0, 128), kb_, scale=0.125)
                p.dma("sp", g["qaT"][:, 2 * a, T0:T0 + NT], e_[0:64, :], r=[ek], lane=ek)
                p.dma("sp", g["qaT"][:, 2 * a + 1, T0:T0 + NT], e_[64:128, :], r=[ek], lane=ek)
            for a in range(4):
                b, kb_ = colmm(QI0 + a * 128, 128)
                e_, ek = evac(b, slice(0, 128), kb_)
                p.dma("sp", g["qiT"][:, 2 * a, T0:T0 + NT], e_[0:64, :], r=[ek], lane=ek)
                p.dma("sp", g["qiT"][:, 2 * a + 1, T0:T0 + NT], e_[64:128, :], r=[ek], lane=ek)
            b, kb_ = bk.get()
            for sub in range(4):
                for c in range(8):
                    p.mm(b[:, sub * 8:(sub + 1) * 8], hb[:, c, sub * 128:(sub + 1) * 128], win[:, c, WI0:WI0 + 8], c == 0, c == 7,
                         r=["hb", wk[c]], w=[kb_])
            p.ts("dve", wis[:], b[:, 0:32].rearrange("p (s d) -> p s d", s=4), 1.0 / (8.0 * 8.0 ** 0.5), None, ALU.mult, r=[kb_], w=["wis"])
            p.dma("sp", g["widx"].rearrange("(n p) d -> p n d", p=128)[:, t * 4:(t + 1) * 4, :], wis[:], r=["wis"], lane="widx")
            cb = [colmm(CQ0 + c3 * 128, 128) for c3 in range(3)]
            for c3 in range(3):
                p.act(sq[:, c3, :], cb[c3][0][:], AF.Square, r=[cb[c3][1]], w=["sq"])
            rms_rstd(p, ps[0], lambda c: sq[:, c, :], 3, ones, epsc, rs, NT, 1.0 / 384, ["sq"], "ps0")
            for c3 in range(3):
                p.stt("dve", cn[:, c3, :], cb[c3][0][:], gcq[:, c3:c3 + 1], rs[:], ALU.mult, ALU.mult, r=[cb[c3][1], "rs", "gcq"], w=["cn"])
            s96 = 96.0 ** -0.5
            for h in range(8):
                ba, ka = bk.get()
                for c3 in range(3):
                    p.mm(ba[0:96, :], wuq[:, c3, h * 96:(h + 1) * 96], cn[:, c3, :], c3 == 0, c3 == 2, r=["cn", "wuq"], w=[ka])
                bb, kb2 = bk.get()
                for c3 in range(3):
                    p.mm(bb[0:96, :], wuqs[:, c3, h * 96:(h + 1) * 96], cn[:, c3, :], c3 == 0, c3 == 2, r=["cn", "wuqs"], w=[kb2])
                q_, qk = qbs[h % 2], f"qbs{h%2}"
                p.ts("dve", q_[0:64, :], ba[0:64, :], s96, None, ALU.mult, r=[ka], w=[qk])
                rope(ba, ka, bb, kb2, q_, qk, s96)
                p.dma("sp", g["qbT"][:, h, T0:T0 + NT], q_[0:96, :], r=[qk], lane=f"qb{h%2}")
            for m in range(16):
                b, kb_ = colmm(GT0 + m * 128, 128)
                p.act(gs[m % 2][:], b[:], AF.Sigmoid, r=[kb_], w=[f"gs{m%2}"])
                p.dma("sp", g["gT"][m * 128:(m + 1) * 128, T0:T0 + NT], gs[m % 2][:], r=[f"gs{m%2}"], lane=f"gs{m%2}")
        p.emit()


def attn_phase(nc, g, ps, identb, mode):
    NB = 32
    with contextlib.ExitStack() as es:
        sb = lambda n, s, d=F32: es.enter_context(nc.sbuf_tensor("p3" + mode + n, list(s), d))
        dsa = mode == "dsa"
        SD = S if dsa else 2
        SM = 2 if dsa else S
        kiT = sb("kiT", [128, SD], BF16); kaT = sb("kaT", [128, SD], BF16)
        va = sb("va", [128, 64 if dsa else 1, 65], BF16); vb = sb("vb", [128, 1 if dsa else 64, 8 if dsa else 8 * 65], BF16)
        kb = [sb(f"kb{i}", [96, SM], BF16) for i in range(2)]
        Isc2 = [sb(f"Isc{i}", [128, SD]) for i in range(2)]
        nm2 = [sb(f"nm{i}", [128, SD], BF16) for i in range(2)]
        junk = sb("junk", [128, SD], mybir.dt.uint8)
        I4 = sb("I4", [128, 512], BF16); sel = sb("sel", [65, 64], BF16)
        dhi = sb("dhi", [65, 512], BF16); dlo = sb("dlo", [65, 512], BF16)
        qi2 = [sb(f"qi{i}", [128, 8, 128], BF16) for i in range(2)]
        qa2 = [sb(f"qa{i}", [128, 8, 128], BF16) for i in range(2)]
        wq2 = [sb(f"wq{i}", [128, 8]) for i in range(2)]
        Dh2 = [sb(f"Dh{i}", [128, 8, 128], BF16) for i in range(2)]
        rl = [sb(f"rl{i}", [128, 8, 512 if dsa else 2], BF16) for i in range(2)]
        cmq2 = [sb(f"cmq{i}", [128, 256]) for i in range(2)]
        pw = sb("pw", [128, NBIS + 1])
        hk2 = [sb(f"hk{i}", [128, NBIS + 1]) for i in range(2)]
        h22 = [sb(f"h2{i}", [128, NBIS + 1]) for i in range(2)]
        sm2 = [sb(f"sm{i}", [128, 8]) for i in range(2)]
        pt = [sb(f"pt{i}", [128, 1024], BF16) for i in range(3)]
        osb = sb("osb", [65, 1024]); rden = sb("rden", [64, 1024])
        oo = sb("oo", [64, 1024], BF16)
        qb = sb("qb", [96, 8, 2 if dsa else 512], BF16)
        cmk = sb("cmk", [128, 8, 2 if dsa else 512], BF16)
        Bt = sb("Bt", [128, 3, 1024], BF16)
        rb = sb("rb", [128, 32, 8]); dl = sb("dl", [128, len(TH), 8])
        pqi = sb("pqi", [128, 128], I32); pki = sb("pki", [128, 3], I32)
        pqf = sb("pqf", [128, 128]); pkf = sb("pkf", [128, 3])
        rel = sb("rel", [128, 128]); ind = sb("ind", [128, 128]); bacc = sb("bacc", [128, 4, 128])
        p = Phase(nc, "p3" + mode)
        if dsa:
            p.memset("dve", kiT[64:128, :], 0.0, w=["kiT"])
            p.memset("pool", kaT[64:128, :], 0.0, w=["kaT"])
            for sl_ in range(2):
                p.memset("pool", qi2[sl_][64:128, :, :], 0.0, w=[f"qi{sl_}"])
                p.memset("pool", qa2[sl_][64:128, :, :], 0.0, w=[f"qa{sl_}"])
            p.dma("sp", kiT[0:64, :], g["kiT"], w=["kiT"], lane="kiT")
            p.dma("sp", kaT[0:64, :], g["kaT"], w=["kaT"], lane="kaT")
            vav = g["va"].rearrange("(n p) d -> p n d", p=128)
            for q4 in range(16):
                p.dma("sp", va[:, q4 * 4:(q4 + 1) * 4, :], vav[:, q4 * 4:(q4 + 1) * 4, :], w=["va"], lane="va")
        else:
            vbv = g["vb"].rearrange("(n p) h d -> p n (h d)", p=128)
            for q4 in range(16):
                p.dma("sp", vb[:, q4 * 4:(q4 + 1) * 4, :], vbv[:, q4 * 4:(q4 + 1) * 4, :], w=["vb"], lane="vbl")
        for q4 in range(4):
            p.copy("dve", I4[:, q4 * 128:(q4 + 1) * 128], identb, r=["ident"], w=["I4"])
        p.memset("dve", sel[:], 0.0, w=["sel"])
        p.memset("dve", sel[64:65, :], 1.0, w=["sel"])
        for k in range(NBIS + 1):
            p.memset("dve", pw[:, k:k + 1], 2.0 ** -(k + 1), w=["pw"])
        if dsa:
            p.dma("sp", rb[:], g["rb128"], w=["rb"], lane="rb")
            p.dma("sp", pqi[:], g["posq_bc"], w=["pqi"], lane="pqi")
            p.dma("sp", pki[:], g["posk_col"], w=["pki"], lane="pki")
            p.copy("dve", pqf[:], pqi[:], r=["pqi"], w=["pqf"])
            p.copy("dve", pkf[:], pki[:], r=["pki"], w=["pkf"])
            prev = 15
            for j, (th, nb_) in enumerate(TH):
                p.tt("dve", dl[:, j, :], rb[:, nb_, :], rb[:, prev, :], ALU.subtract, r=["rb"], w=["dl"])
                prev = nb_
            for ty in range(3):
                p.ts("dve", rel[:], pqf[:], pkf[:, ty:ty + 1], -1.0, ALU.subtract, ALU.mult, r=["pqf", "pkf"], w=["rel"])
                for hh in range(2):
                    p.memset("pool", bacc[:], 0.0, w=["bacc"] + [f"bacc{h}" for h in range(4)])
                    for j, (th, nb_) in enumerate(TH):
                        p.ts("dve", ind[:], rel[:], float(th), None, ALU.is_ge, r=["rel"], w=["ind"])
                        for h in range(4):
                            p.stt("dve", bacc[:, h, :], ind[:], dl[:, j, hh * 4 + h:hh * 4 + h + 1], bacc[:, h, :], ALU.mult, ALU.add,
                                  r=["ind", "dl", f"bacc{h}"], w=[f"bacc{h}"])
                    p.copy("dve", Bt[:, ty, hh * 512:(hh + 1) * 512], bacc[:].rearrange("p h q -> p (h q)"),
                           r=["bacc"] + [f"bacc{h}" for h in range(4)], w=["Bt", "bacc"])

        bkI = Banks(ps, [0, 1, 2, 3])

        def normalize(accs, width, dst, dkey, dma_fn):
            for hf_, (b, kb_) in enumerate(accs):
                p.copy("act", osb[:, hf_ * 512:(hf_ + 1) * 512], b[0:65, :], r=[kb_], w=["osb"])
            for hf_ in range(len(accs)):
                b, kb_ = ps[6 + hf_ % 2], f"ps{6 + hf_ % 2}"
                p.copy("dve", dhi[:], osb[:, hf_ * 512:(hf_ + 1) * 512], r=["osb"], w=["dhi"])
                p.tt("dve", dlo[:], osb[:, hf_ * 512:(hf_ + 1) * 512], dhi[:], ALU.subtract, r=["osb", "dhi"], w=["dlo"])
                p.mm(b[0:64, :], sel[:], dhi[:], True, False, r=["dhi", "sel"], w=[kb_])
                p.mm(b[0:64, :], sel[:], dlo[:], False, True, r=["dlo", "sel"], w=[kb_])
                p.add("dve", lambda e, b=b, hf_=hf_: e.reciprocal(out=rden[:, hf_ * 512:(hf_ + 1) * 512], in_=b[0:64, :]), r=[kb_], w=["rden"])
            p.tt("dve", oo[:, :width], osb[0:64, :width], rden[:, :width], ALU.mult, r=["osb", "rden"], w=["oo"])
            dma_fn()

        kbcount = [0]

        def dsa_A(i):
            sl = i % 2
            Q0 = i * 128
            p.dma("sp", qi2[sl][0:64, :, :], g["qiT"][:, :, Q0:Q0 + 128], w=[f"qi{sl}"], lane=f"qi{sl}")
            p.dma("sp", qa2[sl][0:64, :, :], g["qaT"][:, :, Q0:Q0 + 128], w=[f"qa{sl}"], lane=f"qa{sl}")
            p.dma("sp", wq2[sl][:], g["widx"][Q0:Q0 + 128, :], w=[f"wq{sl}"], lane=f"wq{sl}")
            p.dma("sp", cmq2[sl][:], g["cmq"][i], w=[f"cmq{sl}"], lane=f"cmq{sl}")
            qi, wq, Dh, Isc = qi2[sl], wq2[sl], Dh2[sl], Isc2[sl]
            for h in range(8):
                p.ts("dve", Dh[:, h, :], identb, wq[:, h:h + 1], None, ALU.mult, r=["ident", f"wq{sl}"], w=[f"Dh{sl}"])
            groups = []
            for (k0, n) in ((0, (i + 1) * 128), (4096, (i + 1) * 128)):
                o = 0
                while o < n:
                    w_ = min(512, n - o)
                    groups.append((k0 + o, w_))
                    o += w_
            col = 0
            for gi, (k0, w_) in enumerate(groups):
                R_ = rl[gi % 2]
                rk = f"rl{gi%2}"
                for h in range(8):
                    b, kb_ = bkI.get()
                    p.mm(b[:, :w_], qi[:, h, :], kiT[:, k0:k0 + w_], True, True, r=[f"qi{sl}", "kiT"], w=[kb_])
                    p.act(R_[:, h, :w_], b[:, :w_], AF.Relu, r=[kb_], w=[rk])
                b, kb_ = ps[4 + gi % 2], f"ps{4 + gi % 2}"
                for h in range(8):
                    p.mm(b[:, :w_], Dh[:, h, :], R_[:, h, :w_], h == 0, h == 7, r=[f"Dh{sl}", rk], w=[kb_])
                p.copy("act", Isc[:, col:col + w_], b[:, :w_], r=[kb_], w=[f"Isc{sl}"])
                col += w_

        def dsa_B(i):
            sl = i % 2
            W = 2 * (i + 1) * 128
            Isc, nm, sm, hk, h2, cmq = Isc2[sl], nm2[sl], sm2[sl], hk2[sl], h22[sl], cmq2[sl]
            ik, sk, hkk = f"Isc{sl}", f"sm{sl}", f"hk{sl}"
            p.add("dve", lambda e, W=W: e.tensor_reduce(out=sm[:, 0:1], in_=Isc[:, :W], axis=AX.X, op=ALU.max, apply_absolute_value=True),
                  r=[ik], w=[sk])
            c_own = i * 128
            c_oth = (i + 1) * 128 + i * 128
            p.tt("dve", Isc[:, c_own:c_own + 128], Isc[:, c_own:c_own + 128], cmq[:, 0:128], ALU.add, r=[ik, f"cmq{sl}"], w=[ik])
            p.tt("dve", Isc[:, c_oth:c_oth + 128], Isc[:, c_oth:c_oth + 128], cmq[:, 128:256], ALU.add, r=[ik, f"cmq{sl}"], w=[ik])
            p.ts("dve", sm[:, 1:2], sm[:, 0:1], 2.02, 2e-6, ALU.mult, ALU.add, r=[sk], w=[sk])
            p.ts("dve", hk[:], pw[:], sm[:, 1:2], None, ALU.mult, r=["pw", sk], w=[hkk])
            p.ts("dve", h2[:], hk[:], 2.0, None, ALU.mult, r=[hkk], w=[hkk])
            p.ts("dve", sm[:, 2:3], sm[:, 1:2], 0.0, None, ALU.mult, r=[sk], w=[sk])
            for k in range(NBIS):
                p.ts("dve", junk[:, :W], Isc[:, :W], sm[:, 2:3], None, ALU.is_ge, ALU.add, r=[ik, sk], w=["junk", f"cnt{sl}"], accum_out=sm[:, 3:4])
                p.ts("dve", sm[:, 4:5], sm[:, 3:4], float(TOPK), h2[:, k + 1:k + 2], ALU.is_ge, ALU.mult, r=[f"cnt{sl}", hkk], w=[f"tmp{sl}"])
                p.stt("dve", sm[:, 2:3], sm[:, 4:5], hk[:, k + 1:k + 2], sm[:, 2:3], ALU.subtract, ALU.add, r=[f"tmp{sl}", sk, hkk], w=[sk])
            p.ts("dve", nm[:, :W], Isc[:, :W], sm[:, 2:3], NEG, ALU.is_lt, ALU.mult, r=[ik, sk], w=[f"nm{sl}"])

        def dsa_C(i):
            sl = i % 2
            Q0 = i * 128
            nk = 2 * (i + 1)
            blocks = list(range(i + 1)) + list(range(32, 32 + i + 1))
            qa, nm = qa2[sl], nm2[sl]
            accs = [(ps[4], "ps4"), (ps[5], "ps5")]

            def s_stage(c, L):
                near = None
                if L == i:
                    near = 0
                elif L == 32 + i - 1:
                    near = 1
                elif L == 32 + i:
                    near = 2
                P_ = pt[c % 3]
                pk = f"pt{c%3}"
                for hf_ in range(2):
                    b, kb_ = ps[(c % 2) * 2 + hf_], f"ps{(c % 2) * 2 + hf_}"
                    p.mm(b[:], kaT[:, L * 128:(L + 1) * 128], qa[:, hf_ * 4:(hf_ + 1) * 4, :].rearrange("d h q -> d (h q)"), True, False,
                         r=["kaT", f"qa{sl}"], w=[kb_])
                    p.mm(b[:], nm[:, c * 128:(c + 1) * 128], I4[:], False, near is None, r=[f"nm{sl}", "I4"], w=[kb_])
                    if near is not None:
                        p.mm(b[:], identb, Bt[:, near, hf_ * 512:(hf_ + 1) * 512], False, True, r=["ident", "Bt"], w=[kb_])
                    p.act(P_[:, hf_ * 512:(hf_ + 1) * 512], b[:], AF.Exp, r=[kb_], w=[pk])

            def pv_stage(c, L):
                P_ = pt[c % 3]
                pk = f"pt{c%3}"
                for hf_ in range(2):
                    b, kb_ = accs[hf_]
                    p.mm(b[0:65, :], va[:, L, :], P_[:, hf_ * 512:(hf_ + 1) * 512], c == 0, c == nk - 1, r=["va", pk], w=[kb_])

            s_stage(0, blocks[0])
            for c, L in enumerate(blocks):
                if c + 1 < nk:
                    s_stage(c + 1, blocks[c + 1])
                pv_stage(c, L)

            def dma_oa(Q0=Q0):
                p.dma("sp", g["oaT"][:, :, Q0:Q0 + 128], oo[:, :].rearrange("d (h q) -> d h q", h=8), r=["oo"], lane="oa")
            normalize(accs, 1024, oo, "oo", dma_oa)

        def kb_load(j, h, slot):
            nb_own = 4 * j + 4
            K_, kk_ = kb[slot], f"kb{slot}"
            p.dma("sp", K_[:, 0:nb_own * 128], g["kbT"][h, :, 0:nb_own * 128], w=[kk_], lane=kk_ + "a")
            p.dma("sp", K_[:, nb_own * 128:2 * nb_own * 128], g["kbT"][h, :, 4096:4096 + nb_own * 128], w=[kk_], lane=kk_ + "b")

        def mla_tile(j):
            T0 = j * NT
            nb_own = 4 * j + 4
            tblocks = list(range(nb_own)) + list(range(32, 32 + nb_own))
            n_ = len(tblocks)
            p.dma("sp", qb[:], g["qbT"][:, :, T0:T0 + NT], w=["qb"], lane="qb")
            p.dma("pool", cmk[:], g["cmk"][j], w=["cmk"], lane="cmk")
            if j == 0:
                kb_load(0, 0, 0)
            for h in range(8):
                slot = (j * 8 + h) % 2
                K_, kk_ = kb[slot], f"kb{slot}"
                if h + 1 < 8:
                    kb_load(j, h + 1, 1 - slot)
                elif j + 1 < 8:
                    kb_load(j + 1, 0, 1 - slot)
                acc = (ps[4 + h % 2], f"ps{4 + h % 2}")

                def s_stage(c, L):
                    b, kb_ = ps[c % 4], f"ps{c % 4}"
                    mi = None
                    if 4 * j <= L < 4 * j + 4:
                        mi = L - 4 * j
                    elif 32 + 4 * j <= L < 32 + 4 * j + 4:
                        mi = 4 + L - 32 - 4 * j
                    p.mm(b[:], K_[:, c * 128:(c + 1) * 128], qb[:, h, :], True, mi is None, r=[kk_, "qb"], w=[kb_])
                    if mi is not None:
                        p.mm(b[:], identb, cmk[:, mi, :], False, True, r=["ident", "cmk"], w=[kb_])
                    p.act(pt[c % 3][:, 0:512], b[:], AF.Exp, r=[kb_], w=[f"pt{c%3}"])

                def pv_stage(c, L):
                    p.mm(acc[0][0:65, :], vb[:, L, h * 65:(h + 1) * 65], pt[c % 3][:, 0:512], c == 0, c == n_ - 1, r=["vb", f"pt{c%3}"], w=[acc[1]])

                s_stage(0, tblocks[0])
                s_stage(1, tblocks[1])
                for c, L in enumerate(tblocks):
                    if c + 2 < n_:
                        s_stage(c + 2, tblocks[c + 2])
                    pv_stage(c, L)

                def dma_ob(h=h, T0=T0):
                    p.dma("sp", g["obT"][:, h, T0:T0 + NT], oo[:, 0:512], r=["oo"], lane="ob")
                normalize([acc], 512, oo, "oo", dma_ob)

        nblk = min(NB, NBLK)
        if dsa:
            for i in range(min(2, nblk)):
                dsa_A(i)
                dsa_B(i)
            for i in range(nblk):
                dsa_C(i)
                if i + 2 < nblk:
                    dsa_A(i + 2)
                    dsa_B(i + 2)
        else:
            for i in range(nblk):
                if i % 4 == 3:
                    mla_tile(i // 4)
        p.emit()


def merge_phase(nc, g, x1T, x2T, ps, G2):
    with contextlib.ExitStack() as es:
        sb = lambda n, s, d=F32: es.enter_context(nc.sbuf_tensor("p4" + n, list(s), d))
        woa = sb("woa", [128, 4, D], BF16); wob = sb("wob", [128, 4, D], BF16); wout = sb("wout", [128, 8, D], BF16)
        oa = sb("oa", [128, 4, NT], BF16); ob = sb("ob", [128, 4, NT], BF16)
        gt = sb("gt", [128, 16, NT]); xs = sb("xs", [128, 8, NT])
        y = sb("y", [128, 8, NT], BF16); t1 = sb("t1", [128, NT]); t2 = sb("t2", [128, NT])
        p = Phase(nc, "p4")
        p.dma("pool", woa[:], g["w_o_a"].rearrange("(c p) n -> p c n", p=128), w=["woa"], lane="woa", max_dma_last_dim=4096)
        p.dma("pool", wob[:], g["w_o_b"].rearrange("(c p) n -> p c n", p=128), w=["wob"], lane="wob", max_dma_last_dim=4096)
        p.dma("pool", wout[:], g["w_out"].rearrange("(c p) n -> p c n", p=128), w=["wout"], lane="wout", max_dma_last_dim=4096)
        x1v = x1T.rearrange("(c p) t -> p c t", p=128)
        x2v = x2T.rearrange("(c p) t -> p c t", p=128)
        gv = g["gT"].rearrange("(c p) t -> p c t", p=128)
        for t in range(SOWN // NT):
            T0 = t * NT
            for par in range(2):
                p.dma("sp", oa[par * 64:(par + 1) * 64, :, :], g["oaT"].rearrange("d (c two) t -> d two c t", two=2)[:, par, :, T0:T0 + NT],
                      w=["oa"], lane="oa")
                p.dma("sp", ob[par * 64:(par + 1) * 64, :, :], g["obT"].rearrange("d (c two) t -> d two c t", two=2)[:, par, :, T0:T0 + NT],
                      w=["ob"], lane="ob")
            p.dma("sp", gt[:], gv[:, :, T0:T0 + NT], w=["gt"], lane="gt")
            p.dma("sp", xs[:], x1v[:, :, T0:T0 + NT], w=["xs"], lane="xs")
            for m in range(8):
                ba, ka = ps[m % 2], f"ps{m%2}"
                bb, kb_ = ps[2 + m % 2], f"ps{2 + m%2}"
                for h in range(4):
                    p.mm(ba[:], woa[:, h, m * 128:(m + 1) * 128], oa[:, h, :], h == 0, h == 3, r=["woa", "oa"], w=[ka])
                for h in range(4):
                    p.mm(bb[:], wob[:, h, m * 128:(m + 1) * 128], ob[:, h, :], h == 0, h == 3, r=["wob", "ob"], w=[kb_])
                p.tt("dve", t1[:], ba[:], gt[:, m, :], ALU.mult, r=[ka, "gt"], w=["t1"])
                p.tt("dve", t2[:], bb[:], gt[:, 8 + m, :], ALU.mult, r=[kb_, "gt"], w=["t2"])
                p.tt("dve", y[:, m, :], t1[:], t2[:], ALU.add, r=["t1", "t2"], w=[f"y{m}"])
            for m in range(8):
                b, kb_ = ps[4 + m % 2], f"ps{4 + m%2}"
                for c in range(8):
                    p.mm(b[:], wout[:, c, m * 128:(m + 1) * 128], y[:, c, :], c == 0, c == 7, r=["wout", f"y{c}"], w=[kb_])
                p.stt("dve", xs[:, m, :], b[:], G2[:, m:m + 1], xs[:, m, :], ALU.mult, ALU.add, r=[kb_, "xs"], w=["xs"])
            p.dma("sp", x2v[:, :, T0:T0 + NT], xs[:], r=["xs"], lane="st")
        p.emit()


def build(stage=99, debug=False):
    nc = bass.Bass("TRN2", target_bir_lowering=False)
    dt = lambda n, s, d=F32: nc.dram_tensor(n, list(s), d, kind="ExternalInput").ap()
    xT = dt("xT", [D, S])
    cvec = dt("cvec", [128, 8])
    w_ada = dt("w_ada", [D, 9 * D])
    b_ada = dt("b_ada", [128, 72])
    g_ffn1 = dt("g_ffn1", [128, 8]); g_mix = dt("g_mix", [128, 8]); g_ffn2 = dt("g_ffn2", [128, 8]); g_final = dt("g_final", [128, 8])
    w1i = dt("w_ffn1_in", [D, 2 * DFF]); w1d = dt("w_ffn1_down", [DFF, D])
    w2i = dt("w_ffn2_in", [D, 2 * DFF]); w2d = dt("w_ffn2_down", [DFF, D])
    ident_d = dt("ident", [128, 128])
    g = {}
    g["winP"] = dt("winP", [D, WTOT])
    g["w_uk"] = dt("w_uk", [256, 512]); g["w_uv"] = dt("w_uv", [256, 512])
    g["w_uq"] = dt("w_uq", [384, 768]); g["w_uqs"] = dt("w_uqs", [384, 768])
    g["g_ckv"] = dt("g_ckv", [128, 2]); g["g_cq"] = dt("g_cq", [128, 3])
    g["freqc"] = dt("freqc", [128, 1]); g["sgnc"] = dt("sgnc", [128, 1])
    g["pos32"] = dt("pos32", [32, S], I32)
    g["rb128"] = dt("rb128", [128, 32, 8])
    g["posq_bc"] = dt("posq_bc", [128, 128], I32); g["posk_col"] = dt("posk_col", [128, 3], I32)
    g["cmq"] = dt("cmq", [32, 128, 256]); g["cmk"] = dt("cmk", [8, 128, 8, 512])
    g["w_o_a"] = dt("w_o_a", [512, D]); g["w_o_b"] = dt("w_o_b", [512, D]); g["w_out"] = dt("w_out", [D, D])
    outT = nc.dram_tensor("outT", [D, SOWN], F32, kind="ExternalOutput").ap()
    dbgset = set(debug.split(",")) if debug else set()
    it = lambda n, s, d=F32: nc.dram_tensor(n, list(s), d, kind=("ExternalOutput" if n in dbgset else "Internal")).ap()
    x1T = it("x1T", [D, S]); x2T = it("x2T", [D, SOWN])
    g["kaT"] = it("kaT", [64, S], BF16); g["kiT"] = it("kiT", [64, S], BF16)
    g["kbT"] = it("kbT", [8, 96, S], BF16); g["vb"] = it("vb", [S, 8, 65], BF16); g["va"] = it("va", [S, 65], BF16)
    g["qaT"] = it("qaT", [64, 8, SOWN], BF16); g["qiT"] = it("qiT", [64, 8, SOWN], BF16)
    g["widx"] = it("widx", [SOWN, 8]); g["qbT"] = it("qbT", [96, 8, SOWN], BF16)
    g["gT"] = it("gT", [2048, SOWN])
    g["oaT"] = it("oaT", [64, 8, SOWN], BF16); g["obT"] = it("obT", [64, 8, SOWN], BF16)

    with contextlib.ExitStack() as es:
        ps = [es.enter_context(nc.psum_tensor(f"psb{i}", [128, 512], F32)) for i in range(8)]
        sb = lambda n, s, d=F32: es.enter_context(nc.sbuf_tensor(n, list(s), d))
        ones_t = sb("ones", [128, 128], BF16); ones = ones_t[:]
        epsc_t = sb("epsc", [128, 1]); epsc = epsc_t[:]
        identb_t = sb("identb", [128, 128], BF16); identb = identb_t[:]
        modT_t = sb("modT", [128, 72]); modT = modT_t[:]
        drv_t = sb("drv", [128, 10, 8])
        derived = [drv_t[:, i, :] for i in range(9)]
        gfin = drv_t[:, 9, :]
        A1, S1, G1, A2, S2, G2, A3, S3, G3 = derived

        setup_phase(nc, cvec, w_ada, b_ada, [g_ffn1, g_mix, g_ffn2], modT, None, ones, epsc, ident_d, identb, ps, derived)
        pp = Phase(nc, "gfin")
        pp.dma("sp", gfin, g_final, w=["gf"], lane="gf")
        pp.emit()
        if stage == 0:
            return nc
        if stage == 20:
            proj_phase(nc, x1T, g, ps, ones, epsc, A2, S2)
            return nc
        if stage in (30, 31):
            import os
            global NBLK
            NBLK = int(os.environ.get("NBLK", "32"))
            attn_phase(nc, g, ps, identb, "dsa" if stage == 30 else "mla")
            return nc
        ffn_phase(nc, "f1", xT, x1T, S // NT, w1i, w1d, A1, S1, G1, ps, ones, epsc)
        if stage == 1:
            ffn_phase(nc, "f2", x1T[:, 0:SOWN], outT, SOWN // NT, w2i, w2d, A3, S3, G3, ps, ones, epsc, gfin=gfin)
            return nc
        proj_phase(nc, x1T, g, ps, ones, epsc, A2, S2)
        if stage == 2:
            return nc
        attn_phase(nc, g, ps, identb, "dsa")
        if stage == 3:
            return nc
        attn_phase(nc, g, ps, identb, "mla")
        if stage == 4:
            return nc
        merge_phase(nc, g, x1T, x2T, ps, G2)
        ffn_phase(nc, "f2", x2T, outT, SOWN // NT, w2i, w2d, A3, S3, G3, ps, ones, epsc, gfin=gfin)
    return nc


def local_perm(p):
    own = np.arange(32) * 2 + p
    oth = np.arange(32) * 2 + 1 - p
    blocks = np.concatenate([own, oth])
    return (blocks[:, None] * 128 + np.arange(128)[None, :]).reshape(-1)


def pm(v):
    v = np.asarray(v, np.float32)
    return np.ascontiguousarray(v.reshape(-1, 128).T)


FREQ16 = [1.0, 0.5623413324356079, 0.3162277638912201, 0.17782793939113617, 0.10000000149011612, 0.05623413249850273,
          0.03162277489900589, 0.017782794311642647, 0.009999999776482582, 0.005623413249850273, 0.003162277629598975,
          0.0017782794311642647, 0.0010000000474974513, 0.000562341301701963, 0.0003162277571391314, 0.00017782794020604342]


def host_consts(p):
    perm = local_perm(p)
    lim = (perm // 64 + 1) * 64
    cmq = np.zeros((32, 128, 256), np.float32)
    for i in range(32):
        ql = lim[i * 128:(i + 1) * 128][:, None]
        for half, kbk in ((0, i), (1, 32 + i)):
            kt = perm[kbk * 128:(kbk + 1) * 128][None, :]
            cmq[i, :, half * 128:(half + 1) * 128] = np.where(kt < ql, 0.0, -1e30)
    cmk = np.zeros((8, 128, 8, 512), np.float32)
    for j in range(8):
        ql = lim[j * 512:(j + 1) * 512][None, :]
        for mi in range(8):
            kbk = 4 * j + mi if mi < 4 else 32 + 4 * j + (mi - 4)
            kt = perm[kbk * 128:(kbk + 1) * 128][:, None]
            cmk[j, :, mi, :] = np.where(kt < ql, 0.0, NEG)
    freqc = np.zeros((128, 1), np.float32)
    sgnc = np.zeros((128, 1), np.float32)
    for r in range(32):
        freqc[64 + r, 0] = FREQ16[r % 16]
        sgnc[64 + r, 0] = -1.0 if r < 16 else 1.0
    return perm, cmq, cmk, freqc, sgnc


def kernel(**inputs):
    import os
    stage = int(os.environ.get("KSTAGE", "99"))
    debug = os.environ.get("KDEBUG", "")
    f = lambda a: np.ascontiguousarray(np.asarray(a, np.float32))
    x = np.asarray(inputs["x"], np.float32)
    w_in = np.asarray(inputs["w_in"][0], np.float32)
    q_a, k_a, v_a = w_in[:, 0:512], w_in[:, 512:576], w_in[:, 576:640]
    q_i, k_i, w_i = w_in[:, 640:1152], w_in[:, 1152:1216], w_in[:, 1216:1224]
    c_q, c_kv, k_r, gts = w_in[:, 1224:1608], w_in[:, 1608:1864], w_in[:, 1864:1896], w_in[:, 1896:3944]
    k_rs = np.concatenate([k_r[:, 16:32], k_r[:, 0:16]], axis=1)
    winP = np.ascontiguousarray(np.concatenate([k_a, k_i, c_kv, k_a, k_r, k_a, k_rs, v_a, q_a, q_i, c_q, gts, w_i], axis=1))
    assert winP.shape[1] == WTOT
    w_uq = np.asarray(inputs["w_uq"][0], np.float32)
    w_uqs = w_uq.copy().reshape(384, 8, 96)
    w_uqs[:, :, 64:80], w_uqs[:, :, 80:96] = w_uq.reshape(384, 8, 96)[:, :, 80:96], w_uq.reshape(384, 8, 96)[:, :, 64:80]
    w_uqs = np.ascontiguousarray(w_uqs.reshape(384, 768))
    nc = build(stage, debug)
    in_maps = []
    perms = []
    pos_all = np.asarray(inputs["positions"], np.int32)
    rb128 = np.ascontiguousarray(np.broadcast_to(f(inputs["rel_bias"])[None], (128, 32, 8)))
    consts = [host_consts(0), host_consts(1)]
    for core in range(NCORES):
        b, p = core // 2, core % 2
        perm, cmq, cmk, freqc, sgnc = consts[p]
        perms.append(perm)
        posl = pos_all[b][perm]
        m = {
            "xT": np.ascontiguousarray(x[b][perm].T),
            "cvec": pm(inputs["c"][b]),
            "w_ada": f(inputs["w_ada"][0]),
            "b_ada": pm(inputs["b_ada"][0]),
            "g_ffn1": pm(inputs["g_ffn1"][0]),
            "g_mix": pm(inputs["g_mix"][0]),
            "g_ffn2": pm(inputs["g_ffn2"][0]),
            "g_final": pm(inputs["g_final"]),
            "w_ffn1_in": f(inputs["w_ffn1_in"][0]),
            "w_ffn1_down": f(inputs["w_ffn1_down"][0]),
            "w_ffn2_in": f(inputs["w_ffn2_in"][0]),
            "w_ffn2_down": f(inputs["w_ffn2_down"][0]),
            "ident": np.eye(128, dtype=np.float32),
            "winP": winP, "w_uk": f(inputs["w_uk"][0]), "w_uv": f(inputs["w_uv"][0]), "w_uq": w_uq, "w_uqs": w_uqs,
            "g_ckv": pm(inputs["g_ckv"][0]), "g_cq": pm(inputs["g_cq"][0]),
            "freqc": freqc, "sgnc": sgnc,
            "pos32": np.ascontiguousarray(np.broadcast_to(posl[None], (32, S))),
            "rb128": rb128,
            "posq_bc": np.ascontiguousarray(np.broadcast_to(posl[128:256][None], (128, 128))),
            "posk_col": np.ascontiguousarray(np.stack([posl[128:256], posl[32 * 128:33 * 128], posl[33 * 128:34 * 128]], axis=1)),
            "cmq": cmq, "cmk": cmk,
            "w_o_a": f(inputs["w_o_a"][0]), "w_o_b": f(inputs["w_o_b"][0]), "w_out": f(inputs["w_out"][0]),
        }
        in_maps.append(m)
    res = run_bass_kernel_spmd(nc, in_maps, core_ids=list(range(NCORES)))
    if debug:
        kernel.debug = res.results
        kernel.perms = perms
    out = np.empty((4, S, D), np.float32)
    for core in range(NCORES):
        b = core // 2
        o = res.results[core]["outT"]
        out[b][perms[core][:SOWN]] = o.T
    return out
```

```python
import contextlib
import numpy as np
import concourse.bass as bass
import concourse.mybir as mybir
from concourse.bass_utils import run_bass_kernel_spmd

F32 = mybir.dt.float32
BF16 = mybir.dt.bfloat16
I32 = mybir.dt.int32
ALU = mybir.AluOpType
AF = mybir.ActivationFunctionType
AX = mybir.AxisListType

D = 1024
S = 8192
DFF = 2816
NT = 512
EPS = 1e-6
NCORES = 8
SOWN = 4096
TOPK = 256
NBIS = 10
NEG = -30000.0
NBLK = 32


_SEMREG = {}


def _semreg(nc):
    return _SEMREG.setdefault(id(nc), {"cnt": {}, "gen": {}, "sem": {}})


class Phase:
    def __init__(self, nc, name):
        self.nc = nc
        self.name = name
        self.ops = []
        self.lw = {}
        self.rd = {}

    def add(self, eng, fn, r=(), w=(), lane=None):
        i = len(self.ops)
        deps = set()
        for k in r:
            if k in self.lw:
                deps.add(self.lw[k])
        for k in w:
            if k in self.lw:
                deps.add(self.lw[k])
            deps.update(self.rd.get(k, {}).values())
        for k in w:
            self.lw[k] = i
            self.rd[k] = {}
        tag = lane if lane is not None else eng
        for k in r:
            self.rd.setdefault(k, {})[tag] = i
        self.ops.append(dict(eng=eng, fn=fn, deps=deps, lane=lane, inc=False))
        return i

    def dma(self, eng, out, in_, r=(), w=(), lane=None, **kw):
        assert lane is not None
        return self.add(eng, lambda e: e.dma_start(out=out, in_=in_, **kw), r, w, lane=lane)

    def mm(self, out, lhsT, rhs, start, stop, r=(), w=()):
        return self.add("pe", lambda e: e.matmul(out, lhsT, rhs, start=start, stop=stop), r, w)

    def act(self, out, in_, func, r=(), w=(), eng="act", **kw):
        return self.add(eng, lambda e: e.activation(out=out, in_=in_, func=func, **kw), r, w)

    def ts(self, eng, out, in0, s1, s2, op0, op1=None, r=(), w=(), **kw):
        if op1 is None:
            return self.add(eng, lambda e: e.tensor_scalar(out=out, in0=in0, scalar1=s1, scalar2=None, op0=op0, **kw), r, w)
        return self.add(eng, lambda e: e.tensor_scalar(out=out, in0=in0, scalar1=s1, scalar2=s2, op0=op0, op1=op1, **kw), r, w)

    def stt(self, eng, out, in0, scalar, in1, op0, op1, r=(), w=()):
        return self.add(eng, lambda e: e.scalar_tensor_tensor(out=out, in0=in0, scalar=scalar, in1=in1, op0=op0, op1=op1), r, w)

    def tt(self, eng, out, in0, in1, op, r=(), w=()):
        return self.add(eng, lambda e: e.tensor_tensor(out=out, in0=in0, in1=in1, op=op), r, w)

    def copy(self, eng, out, in_, r=(), w=()):
        if eng == "act":
            return self.add(eng, lambda e: e.activation(out=out, in_=in_, func=AF.Copy), r, w)
        return self.add(eng, lambda e: e.tensor_copy(out=out, in_=in_), r, w)

    def memset(self, eng, ap, val, w=()):
        return self.add(eng, lambda e: e.memset(ap, val), (), w)

    def emit(self):
        nc = self.nc
        ops = self.ops

        def skip(dop, op):
            return dop["lane"] is None and op["lane"] is None and dop["eng"] == "pe" and op["eng"] == "pe"

        for op in ops:
            for d in op["deps"]:
                if not skip(ops[d], op):
                    ops[d]["inc"] = True
        last_dma = {}
        last_eng = {}
        for i, op in enumerate(ops):
            if op["lane"] is not None:
                op["inc"] = True
                last_dma[op["lane"]] = i
            else:
                last_eng[op["eng"]] = i
        for i in last_eng.values():
            ops[i]["inc"] = True
        reg = _semreg(nc)
        cnt, gen, semh = reg["cnt"], reg["gen"], reg["sem"]
        for op in ops:
            if not op["inc"]:
                continue
            base = ("L", op["lane"]) if op["lane"] is not None else ("E", op["eng"])
            gen[base] = gen.get(base, 0)
            key = base + (gen[base],)
            cnt[key] = cnt.get(key, 0) + (16 if op["lane"] is not None else 1)
            op["sem"] = key
            op["val"] = cnt[key]
            pk = reg.setdefault("prevkey", {})
            if op["lane"] is not None and base in pk and pk[base] != key:
                op["drain"] = (pk[base], cnt[pk[base]])
            pk[base] = key
            if key not in semh:
                semh[key] = nc.alloc_semaphore(name=f"s_{key[0]}_{key[1]}_{key[2]}")
            if cnt[key] >= (512 if op["lane"] is not None else 4000):
                gen[base] += 1
        sems = semh
        if "bar" not in reg:
            reg["bar"] = nc.alloc_semaphore(name="s_phase_barrier")
            reg["barcnt"] = 0
        reg["barcnt"] += 5
        bar, bartarget = reg["bar"], reg["barcnt"]
        with contextlib.ExitStack() as es:
            block = es.enter_context(nc.Block())

            def run(engname):
                def body(e):
                    waited = {}
                    for op in ops:
                        if op["eng"] != engname:
                            continue
                        for d in sorted(op["deps"]):
                            dop = ops[d]
                            if not dop["inc"] or skip(dop, op):
                                continue
                            sk, sv = dop["sem"], dop["val"]
                            if waited.get(sk, 0) < sv:
                                e.wait_ge(sems[sk], sv)
                                waited[sk] = sv
                        if "drain" in op:
                            e.wait_ge(sems[op["drain"][0]], op["drain"][1])
                        ins = op["fn"](e)
                        if op["inc"]:
                            ins.then_inc(sems[op["sem"]], 16 if op["lane"] is not None else 1)
                    if engname == "sp":
                        for lane, i in last_dma.items():
                            op = ops[i]
                            if waited.get(op["sem"], 0) < op["val"]:
                                e.wait_ge(sems[op["sem"]], op["val"])
                                waited[op["sem"]] = op["val"]
                    if engname in last_eng:
                        op = ops[last_eng[engname]]
                        e.wait_ge(sems[op["sem"]], op["val"])
                    e.sem_inc(bar, 1)
                    e.wait_ge(bar, bartarget)
                return body

            block.tensor(run("pe"))
            block.scalar(run("act"))
            block.vector(run("dve"))
            block.gpsimd(run("pool"))
            block.sync(run("sp"))


def rms_rstd(p, ps_bank, sq_ap, nchunk, ones, epsc, rs, width, inv_n, rkeys, tagw):
    for c in range(nchunk):
        p.mm(ps_bank[:, :width], ones, sq_ap(c), c == 0, c == nchunk - 1, r=rkeys + ["ones"], w=[tagw])
    p.act(rs[:, :width], ps_bank[:, :width], AF.Sqrt, r=[tagw, "epsc"], w=["rs"], bias=epsc, scale=inv_n)
    p.add("dve", lambda e: e.reciprocal(out=rs[:, :width], in_=rs[:, :width]), r=["rs"], w=["rs"])


def ffn_phase(nc, name, xsrc, xdst, ntiles, w_in_d, w_dn_d, Ac, Sc, Gc, ps, ones, epsc, gfin=None):
    NJ = DFF // 128
    with (nc.sbuf_tensor(name + "wi", [128, 8, 2 * DFF], BF16) as wi,
          nc.sbuf_tensor(name + "wd", [128, NJ, D], BF16) as wd,
          nc.sbuf_tensor(name + "xs0", [128, 8, NT], F32) as xs0,
          nc.sbuf_tensor(name + "xs1", [128, 8, NT], F32) as xs1,
          nc.sbuf_tensor(name + "hb", [128, 8, NT], BF16) as hb,
          nc.sbuf_tensor(name + "hf", [128, NJ, NT], BF16) as hf,
          nc.sbuf_tensor(name + "sg0", [128, NT], F32) as sg0,
          nc.sbuf_tensor(name + "sg1", [128, NT], F32) as sg1,
          nc.sbuf_tensor(name + "tf", [128, NT], F32) as tf,
          nc.sbuf_tensor(name + "rs", [128, NT], F32) as rs):
        p = Phase(nc, name)
        xs = [xs0, xs1]
        sg = [sg0, sg1]
        w_in_v = w_in_d.rearrange("(c p) n -> p c n", p=128)
        w_dn_v = w_dn_d.rearrange("(c p) n -> p c n", p=128)
        xsrc_v = xsrc.rearrange("(c p) t -> p c t", p=128)
        xdst_v = xdst.rearrange("(c p) t -> p c t", p=128)
        p.dma("sp", xs[0][:], xsrc_v[:, :, 0:NT], w=["xs0"], lane="ld0")
        for c in range(8):
            p.dma("pool", wi[:, c, :], w_in_v[:, c, :], w=[f"wi{c}"] + (["wi"] if c == 7 else []), lane="wi", max_dma_last_dim=4096)
        for c in range(NJ):
            p.dma("pool", wd[:, c, :], w_dn_v[:, c, :], w=[f"wd{c}"] + (["wd"] if c == NJ - 1 else []), lane="wd", max_dma_last_dim=4096)
        wik = ["wi" for c in range(8)]
        wdk = ["wd" for c in range(NJ)]
        for t in range(ntiles):
            s = t % 2
            X = xs[s]
            xk = f"xs{s}"
            if t + 1 < ntiles:
                p.dma("sp", xs[1 - s][:], xsrc_v[:, :, (t + 1) * NT:(t + 2) * NT], w=[f"xs{1-s}"], lane=f"ld{1-s}")
            p.act(hb[:], X[:], AF.Square, r=[xk], w=["hb"])
            rms_rstd(p, ps[0], lambda c: hb[:, c, :], 8, ones, epsc, rs, NT, 1.0 / D, ["hb"], "ps0")
            for c in range(8):
                p.stt("dve", tf[:], X[:, c, :], Ac[:, c:c + 1], rs[:], ALU.mult, ALU.mult, r=[xk, "rs"], w=["tf"])
                p.ts("dve", hb[:, c, :], tf[:], Sc[:, c:c + 1], None, ALU.add, r=["tf"], w=["hb"])
            for j in range(NJ):
                pg, pu = ps[1 + 2 * (j % 2)], ps[2 + 2 * (j % 2)]
                kg, ku = f"ps{1 + 2 * (j % 2)}", f"ps{2 + 2 * (j % 2)}"
                for c in range(8):
                    p.mm(pg[:], wi[:, c, j * 128:(j + 1) * 128], hb[:, c, :], c == 0, c == 7, r=["hb", wik[c]], w=[kg])
                for c in range(8):
                    p.mm(pu[:], wi[:, c, DFF + j * 128:DFF + (j + 1) * 128], hb[:, c, :], c == 0, c == 7, r=["hb", wik[c]], w=[ku])
                p.act(sg[j % 2][:], pg[:], AF.Silu, r=[kg], w=[f"sg{j%2}"])
                p.tt("dve", hf[:, j, :], pu[:], sg[j % 2][:], ALU.mult, r=[ku, f"sg{j%2}"], w=[f"hf{j}"])
            for m in range(8):
                po, ko = ps[5 + m % 2], f"ps{5 + m % 2}"
                for j in range(NJ):
                    p.mm(po[:], wd[:, j, m * 128:(m + 1) * 128], hf[:, j, :], j == 0, j == NJ - 1, r=[f"hf{j}", wdk[j]], w=[ko])
                p.stt("dve", X[:, m, :], po[:], Gc[:, m:m + 1], X[:, m, :], ALU.mult, ALU.add, r=[ko, xk], w=[xk])
            if gfin is not None:
                p.act(hb[:], X[:], AF.Square, r=[xk], w=["hb"])
                rms_rstd(p, ps[0], lambda c: hb[:, c, :], 8, ones, epsc, rs, NT, 1.0 / D, ["hb"], "ps0")
                for c in range(8):
                    p.stt("dve", X[:, c, :], X[:, c, :], gfin[:, c:c + 1], rs[:], ALU.mult, ALU.mult, r=[xk, "rs"], w=[xk])
            p.dma("sp", xdst_v[:, :, t * NT:(t + 1) * NT], X[:], r=[xk], lane=f"st{s}")
        p.emit()


def setup_phase(nc, cvec, w_ada, b_ada, gvecs, modT, sc8, ones, epsc, ident_d, identb, ps, derived):
    with (nc.sbuf_tensor("wada0", [128, 8, 1024], BF16) as wa0,
          nc.sbuf_tensor("wada1", [128, 8, 1024], BF16) as wa1,
          nc.sbuf_tensor("cTs", [128, 8], F32) as cT,
          nc.sbuf_tensor("cTb", [128, 8], BF16) as cTb,
          nc.sbuf_tensor("bT", [128, 72], F32) as bT,
          nc.sbuf_tensor("gTs", [128, 3, 8], F32) as gT):
        p = Phase(nc, "setup")
        wa = [wa0, wa1]
        p.memset("dve", ones, 1.0, w=["ones"])
        p.memset("dve", epsc, EPS, w=["epsc"])
        p.dma("pool", identb, ident_d, w=["ident"], lane="ident")
        p.dma("sp", cT[:], cvec, w=["cT"], lane="c")
        p.dma("sp", bT[:], b_ada, w=["bT"], lane="b")
        for i, g in enumerate(gvecs):
            p.dma("sp", gT[:, i, :], g, w=[f"g{i}"], lane=f"g{i}")
        p.act(cTb[:], cT[:], AF.Silu, r=["cT"], w=["cTb"])
        wv = w_ada.rearrange("(c p) n -> p c n", p=128)
        for g in range(9):
            s = g % 2
            for c in range(8):
                p.dma("pool", wa[s][:, c, :], wv[:, c, g * 1024:(g + 1) * 1024], w=[f"wa{s}_{c}"] + ([f"wa{s}"] if c == 7 else []),
                      lane=f"wa{s}", max_dma_last_dim=4096)
            for m in range(8):
                col = g * 8 + m
                for c in range(8):
                    p.mm(ps[0][:, col:col + 1], wa[s][:, c, m * 128:(m + 1) * 128], cTb[:, c:c + 1], c == 0, c == 7,
                         r=[f"wa{s}", f"wa{s}_{c}", "cTb"], w=["ps0"])
        p.tt("dve", modT, ps[0][:, 0:72], bT[:], ALU.add, r=["ps0", "bT"], w=["modT"])
        A1, S1, G1, A2, S2, G2, A3, S3, G3 = derived
        for (A, Sh, G, base, gi, gm) in ((A1, S1, G1, 0, 0, 0.5), (A2, S2, G2, 24, 1, 1.0), (A3, S3, G3, 48, 2, 0.5)):
            p.stt("dve", A, modT[:, base + 8:base + 16], 1.0, gT[:, gi, :], ALU.add, ALU.mult, r=["modT", f"g{gi}"], w=["drv"])
            p.copy("dve", Sh, modT[:, base:base + 8], r=["modT"], w=["drv"])
            p.ts("dve", G, modT[:, base + 16:base + 24], gm, None, ALU.mult, r=["modT"], w=["drv"])
        p.emit()


TWO_PI = 6.283185307179586
CW1 = 6.28125
CW2 = TWO_PI - CW1
MAGIC = 12582912.0
KW = 640
QA0, QI0, CQ0, GT0, WI0, WTOT = 640, 1152, 1664, 2048, 4096, 4104
TH = [(-90, 14), (-63, 13), (-45, 12), (-31, 11), (-22, 10), (-15, 9), (-11, 8), (-7, 7), (-6, 6), (-5, 5), (-4, 4),
      (-3, 3), (-2, 2), (-1, 1), (0, 0), (1, 17), (2, 18), (3, 19), (4, 20), (5, 21), (6, 22), (7, 23), (8, 24),
      (12, 25), (16, 26), (23, 27), (32, 28), (46, 29), (64, 30), (91, 31)]


class Banks:
    def __init__(self, ps, ids):
        self.ps, self.ids, self.i = ps, ids, 0

    def get(self):
        b = self.ids[self.i % len(self.ids)]
        self.i += 1
        return self.ps[b], f"ps{b}"


def proj_phase(nc, x1T, g, ps, ones, epsc, A2, S2):
    import os
    NTL = int(os.environ.get("P2TILES", str(S // NT)))
    QSIDE = os.environ.get("P2Q", "1") == "1"
    with contextlib.ExitStack() as es:
        sb = lambda n, s, d=F32: es.enter_context(nc.sbuf_tensor("p2" + n, list(s), d))
        win = sb("win", [128, 8, WTOT], BF16)
        wuk = sb("wuk", [128, 2, 512], BF16); wuv = sb("wuv", [128, 2, 512], BF16)
        wuq = sb("wuq", [128, 3, 768], BF16); wuqs = sb("wuqs", [128, 3, 768], BF16)
        gck = sb("gck", [128, 2]); gcq = sb("gcq", [128, 3])
        frq = sb("frq", [128, 1]); sgn = sb("sgn", [128, 1])
        xs = [sb("xs0", [128, 8, NT]), sb("xs1", [128, 8, NT])]
        hb = sb("hb", [128, 8, NT], BF16)
        tf = sb("tf", [128, NT]); rs = sb("rs", [128, NT])
        sq = sb("sq", [128, 3, NT], BF16)
        cn = sb("cn", [128, 3, NT], BF16)
        ev = [sb(f"ev{i}", [128, NT], BF16) for i in range(4)]
        gs = [sb(f"gs{i}", [128, NT]) for i in range(2)]
        vbs = [sb(f"vbs{i}", [128, 8, 65], BF16) for i in range(2)]
        vas = sb("vas", [128, 4, 65], BF16)
        wis = sb("wis", [128, 4, 8])
        posi = sb("posi", [128, NT], I32)
        rp = [sb(f"rp{i}", [128, NT]) for i in range(6)]
        Ct = sb("Ct", [128, NT]); St = sb("St", [128, NT])
        kpe = sb("kpe", [128, NT], BF16)
        qbs = [sb(f"qbs{i}", [128, NT], BF16) for i in range(2)]
        p = Phase(nc, "p2")
        bk = Banks(ps, [1, 2, 3, 4, 5, 6, 7])
        R = slice(64, 96)
        x1v = x1T.rearrange("(c p) t -> p c t", p=128)
        p.dma("sp", xs[0][:], x1v[:, :, 0:NT], w=["xs0"], lane="ld0")
        wv = g["winP"].rearrange("(c p) n -> p c n", p=128)
        for c in range(8):
            p.dma("pool", win[:, c, :], wv[:, c, :], w=[f"win{c}"] + (["win"] if c == 7 else []), lane="win", max_dma_last_dim=4096)
        for nm, t_, d_, nch in (("wuk", wuk, g["w_uk"], 2), ("wuv", wuv, g["w_uv"], 2), ("wuq", wuq, g["w_uq"], 3), ("wuqs", wuqs, g["w_uqs"], 3)):
            dv = d_.rearrange("(c p) n -> p c n", p=128)
            for c in range(nch):
                p.dma("pool", t_[:, c, :], dv[:, c, :], w=[nm], lane=nm)
        p.dma("sp", gck[:], g["g_ckv"], w=["gck"], lane="gck")
        p.dma("sp", gcq[:], g["g_cq"], w=["gcq"], lane="gcq")
        p.dma("sp", frq[:], g["freqc"], w=["frq"], lane="frq")
        p.dma("sp", sgn[:], g["sgnc"], w=["sgn"], lane="sgn")
        for i in range(2):
            p.memset("dve", vbs[i][:], 1.0, w=[f"vbs{i}"])
        p.memset("dve", vas[:], 1.0, w=["vas"])
        wk = ["win" for c in range(8)]
        evi = [0]

        def evac(src, rows, kb_, scale=None, eng=None):
            i = evi[0] % 4
            evi[0] += 1
            e = eng or ("act" if i % 2 == 0 else "dve")
            if scale is None and e == "dve":
                p.copy("dve", ev[i][rows, :], src[rows, :], r=[kb_], w=[f"ev{i}"])
            elif e == "dve":
                p.ts("dve", ev[i][rows, :], src[rows, :], scale, None, ALU.mult, r=[kb_], w=[f"ev{i}"])
            else:
                p.act(ev[i][rows, :], src[rows, :], AF.Copy, r=[kb_], w=[f"ev{i}"], scale=(1.0 if scale is None else scale))
            return ev[i], f"ev{i}"

        def colmm(col0, m):
            b, kb_ = bk.get()
            for c in range(8):
                p.mm(b[0:m, :], win[:, c, col0:col0 + m], hb[:, c, :], c == 0, c == 7, r=["hb", wk[c]], w=[kb_])
            return b, kb_

        for t in range(NTL):
            s = t % 2
            X, xk = xs[s], f"xs{s}"
            T0 = t * NT
            if t + 1 < NTL:
                p.dma("sp", xs[1 - s][:], x1v[:, :, (t + 1) * NT:(t + 2) * NT], w=[f"xs{1-s}"], lane=f"ld{1-s}")
            p.dma("sp", posi[R, :], g["pos32"][:, T0:T0 + NT], w=["posi"], lane="pos")
            p.act(hb[:], X[:], AF.Square, r=[xk], w=["hb"])
            rms_rstd(p, ps[0], lambda c: hb[:, c, :], 8, ones, epsc, rs, NT, 1.0 / D, ["hb"], "ps0")
            for c in range(8):
                p.stt("dve", tf[:], X[:, c, :], A2[:, c:c + 1], rs[:], ALU.mult, ALU.mult, r=[xk, "rs"], w=["tf"])
                p.ts("dve", hb[:, c, :], tf[:], S2[:, c:c + 1], None, ALU.add, r=["tf"], w=["hb"])
            p.copy("dve", rp[0][R, :], posi[R, :], r=["posi"], w=["rp0"])
            p.ts("dve", rp[0][R, :], rp[0][R, :], frq[R, 0:1], None, ALU.mult, r=["rp0", "frq"], w=["rp0"])
            for which, dst in ((0, St), (1, Ct)):
                src = rp[0]
                if which == 1:
                    p.ts("dve", rp[1][R, :], rp[0][R, :], 1.5707963267948966, None, ALU.add, r=["rp0"], w=["rp1"])
                    src = rp[1]
                sk = "rp0" if which == 0 else "rp1"
                p.ts("dve", rp[2][R, :], src[R, :], 1.0 / TWO_PI, None, ALU.mult, r=[sk], w=["rp2"])
                p.ts("dve", rp[3][R, :], rp[2][R, :], MAGIC, None, ALU.add, r=["rp2"], w=["rp3"])
                p.ts("dve", rp[3][R, :], rp[3][R, :], -MAGIC, None, ALU.add, r=["rp3"], w=["rp3"])
                p.stt("dve", rp[4][R, :], rp[3][R, :], -CW1, src[R, :], ALU.mult, ALU.add, r=["rp3", sk], w=["rp4"])
                p.stt("dve", rp[4][R, :], rp[3][R, :], -CW2, rp[4][R, :], ALU.mult, ALU.add, r=["rp3", "rp4"], w=["rp4"])
                if which == 0:
                    p.act(dst[R, :], rp[4][R, :], AF.Sin, r=["rp4", "sgn"], w=["St"], scale=sgn[R, 0:1])
                else:
                    p.act(dst[R, :], rp[4][R, :], AF.Sin, r=["rp4"], w=["Ct"])

            def rope(pa, ka, pb, kb2, outap, okey, scale):
                p.stt("dve", rp[5][R, :], pa[R, :], scale, Ct[R, :], ALU.mult, ALU.mult, r=[ka, "Ct"], w=["rp5"])
                p.stt("dve", rp[2][R, :], pb[R, :], scale, St[R, :], ALU.mult, ALU.mult, r=[kb2, "St"], w=["rp2"])
                p.tt("dve", outap[R, :], rp[5][R, :], rp[2][R, :], ALU.add, r=["rp5", "rp2"], w=[okey])

            b, kb_ = colmm(0, 128)
            e_, ek = evac(b, slice(0, 128), kb_)
            p.dma("sp", g["kaT"][:, T0:T0 + NT], e_[0:64, :], r=[ek], lane=ek)
            p.dma("sp", g["kiT"][:, T0:T0 + NT], e_[64:128, :], r=[ek], lane=ek)
            cb = [colmm(128, 128), colmm(256, 128)]
            for c2 in range(2):
                p.act(sq[:, c2, :], cb[c2][0][:], AF.Square, r=[cb[c2][1]], w=["sq"])
            rms_rstd(p, ps[0], lambda c: sq[:, c, :], 2, ones, epsc, rs, NT, 1.0 / 256, ["sq"], "ps0")
            for c2 in range(2):
                p.stt("dve", cn[:, c2, :], cb[c2][0][:], gck[:, c2:c2 + 1], rs[:], ALU.mult, ALU.mult, r=[cb[c2][1], "rs", "gck"], w=["cn"])
            for a in range(4):
                b, kb_ = bk.get()
                for c2 in range(2):
                    p.mm(b[:], wuk[:, c2, a * 128:(a + 1) * 128], cn[:, c2, :], c2 == 0, c2 == 1, r=["cn", "wuk"], w=[kb_])
                e_, ek = evac(b, slice(0, 128), kb_)
                p.dma("sp", g["kbT"][2 * a, 0:64, T0:T0 + NT], e_[0:64, :], r=[ek], lane=ek)
                p.dma("sp", g["kbT"][2 * a + 1, 0:64, T0:T0 + NT], e_[64:128, :], r=[ek], lane=ek)
            for sub in range(4):
                b, kb_ = bk.get()
                for c2 in range(2):
                    p.mm(b[:], cn[:, c2, sub * 128:(sub + 1) * 128], wuv[:, c2, :], c2 == 0, c2 == 1, r=["cn", "wuv"], w=[kb_])
                v = vbs[sub % 2]
                p.copy("dve", v[:, :, 0:64], b[:].rearrange("p (h d) -> p h d", h=8), r=[kb_], w=[f"vbs{sub%2}"])
                r0 = T0 + sub * 128
                p.dma("sp", g["vb"][r0:r0 + 128, :, :], v[:], r=[f"vbs{sub%2}"], lane=f"vb{sub%2}")
            ba, ka = colmm(384, 96)
            bb, kb2 = colmm(480, 96)
            rope(ba, ka, bb, kb2, kpe, "kpe", 1.0)
            for h in range(8):
                p.dma("sp", g["kbT"][h, 64:96, T0:T0 + NT], kpe[R, :], r=["kpe"], lane="kpe")
            b, kb_ = bk.get()
            for sub in range(4):
                for c in range(8):
                    p.mm(b[:, sub * 64:(sub + 1) * 64], hb[:, c, sub * 128:(sub + 1) * 128], win[:, c, 576:640], c == 0, c == 7,
                         r=["hb", wk[c]], w=[kb_])
            p.copy("dve", vas[:, :, 0:64], b[:, 0:256].rearrange("p (s d) -> p s d", s=4), r=[kb_], w=["vas"])
            p.dma("sp", g["va"].rearrange("(n p) d -> p n d", p=128)[:, t * 4:(t + 1) * 4, :], vas[:], r=["vas"], lane="va")
            if t >= SOWN // NT or not QSIDE:
                continue
            for a in range(4):
                b, kb_ = colmm(QA0 + a * 128, 128)
                e_, ek = evac(b, slice(0, 128), kb_, scale=0.125)
                p.dma("sp", g["qaT"][:, 2 * a, T0:T0 + NT], e_[0:64, :], r=[ek], lane=ek)
                p.dma("sp", g["qaT"][:, 2 * a + 1, T0:T0 + NT], e_[64:128, :], r=[ek], lane=ek)
            for a in range(4):
                b, kb_ = colmm(QI0 + a * 128, 128)
                e_, ek = evac(b, slice(0, 128), kb_)
                p.dma("sp", g["qiT"][:, 2 * a, T0:T0 + NT], e_[0:64, :], r=[ek], lane=ek)
                p.dma("sp", g["qiT"][:, 2 * a + 1, T0:T0 + NT], e_[64:128, :], r=[ek], lane=ek)
            b, kb_ = bk.get()
            for sub in range(4):
                for c in range(8):
                    p.mm(b[:, sub * 8:(sub + 1) * 8], hb[:, c, sub * 128:(sub + 1) * 128], win[:, c, WI0:WI0 + 8], c == 0, c == 7,
                         r=["hb", wk[c]], w=[kb_])
            p.ts("dve", wis[:], b[:, 0:32].rearrange("p (s d) -> p s d", s=4), 1.0 / (8.0 * 8.0 ** 0.5), None, ALU.mult, r=[kb_], w=["wis"])
            p.dma("sp", g["widx"].rearrange("(n p) d -> p n d", p=128)[:, t * 4:(t + 1) * 4, :], wis[:], r=["wis"], lane="widx")
            cb = [colmm(CQ0 + c3 * 128, 128) for c3 in range(3)]
            for c3 in range(3):
                p.act(sq[:, c3, :], cb[c3][0][:], AF.Square, r=[cb[c3][1]], w=["sq"])
            rms_rstd(p, ps[0], lambda c: sq[:, c, :], 3, ones, epsc, rs, NT, 1.0 / 384, ["sq"], "ps0")
            for c3 in range(3):
                p.stt("dve", cn[:, c3, :], cb[c3][0][:], gcq[:, c3:c3 + 1], rs[:], ALU.mult, ALU.mult, r=[cb[c3][1], "rs", "gcq"], w=["cn"])
            s96 = 96.0 ** -0.5
            for h in range(8):
                ba, ka = bk.get()
                for c3 in range(3):
                    p.mm(ba[0:96, :], wuq[:, c3, h * 96:(h + 1) * 96], cn[:, c3, :], c3 == 0, c3 == 2, r=["cn", "wuq"], w=[ka])
                bb, kb2 = bk.get()
                for c3 in range(3):
                    p.mm(bb[0:96, :], wuqs[:, c3, h * 96:(h + 1) * 96], cn[:, c3, :], c3 == 0, c3 == 2, r=["cn", "wuqs"], w=[kb2])
                q_, qk = qbs[h % 2], f"qbs{h%2}"
                p.ts("dve", q_[0:64, :], ba[0:64, :], s96, None, ALU.mult, r=[ka], w=[qk])
                rope(ba, ka, bb, kb2, q_, qk, s96)
                p.dma("sp", g["qbT"][:, h, T0:T0 + NT], q_[0:96, :], r=[qk], lane=f"qb{h%2}")
            for m in range(16):
                b, kb_ = colmm(GT0 + m * 128, 128)
                p.act(gs[m % 2][:], b[:], AF.Sigmoid, r=[kb_], w=[f"gs{m%2}"])
                p.dma("sp", g["gT"][m * 128:(m + 1) * 128, T0:T0 + NT], gs[m % 2][:], r=[f"gs{m%2}"], lane=f"gs{m%2}")
        p.emit()


def attn_phase(nc, g, ps, identb, mode):
    NB = 32
    with contextlib.ExitStack() as es:
        sb = lambda n, s, d=F32: es.enter_context(nc.sbuf_tensor("p3" + mode + n, list(s), d))
        dsa = mode == "dsa"
        SD = S if dsa else 2
        SM = 2 if dsa else S
        kiT = sb("kiT", [128, SD], BF16); kaT = sb("kaT", [128, SD], BF16)
        va = sb("va", [128, 64 if dsa else 1, 65], BF16); vb = sb("vb", [128, 1 if dsa else 64, 8 if dsa else 8 * 65], BF16)
        kb = [sb(f"kb{i}", [96, SM], BF16) for i in range(2)]
        Isc2 = [sb(f"Isc{i}", [128, SD]) for i in range(2)]
        nm2 = [sb(f"nm{i}", [128, SD], BF16) for i in range(2)]
        junk = sb("junk", [128, SD], mybir.dt.uint8)
        I4 = sb("I4", [128, 512], BF16); sel = sb("sel", [65, 64], BF16)
        dhi = sb("dhi", [65, 512], BF16); dlo = sb("dlo", [65, 512], BF16)
        qi2 = [sb(f"qi{i}", [128, 8, 128], BF16) for i in range(2)]
        qa2 = [sb(f"qa{i}", [128, 8, 128], BF16) for i in range(2)]
        wq2 = [sb(f"wq{i}", [128, 8]) for i in range(2)]
        Dh2 = [sb(f"Dh{i}", [128, 8, 128], BF16) for i in range(2)]
        rl = [sb(f"rl{i}", [128, 8, 512 if dsa else 2], BF16) for i in range(2)]
        cmq2 = [sb(f"cmq{i}", [128, 256]) for i in range(2)]
        pw = sb("pw", [128, NBIS + 1])
        hk2 = [sb(f"hk{i}", [128, NBIS + 1]) for i in range(2)]
        h22 = [sb(f"h2{i}", [128, NBIS + 1]) for i in range(2)]
        sm2 = [sb(f"sm{i}", [128, 8]) for i in range(2)]
        pt = [sb(f"pt{i}", [128, 1024], BF16) for i in range(3)]
        osb = sb("osb", [65, 1024]); rden = sb("rden", [64, 1024])
        oo = sb("oo", [64, 1024], BF16)
        qb = sb("qb", [96, 8, 2 if dsa else 512], BF16)
        cmk = sb("cmk", [128, 8, 2 if dsa else 512], BF16)
        Bt = sb("Bt", [128, 3, 1024], BF16)
        rb = sb("rb", [128, 32, 8]); dl = sb("dl", [128, len(TH), 8])
        pqi = sb("pqi", [128, 128], I32); pki = sb("pki", [128, 3], I32)
        pqf = sb("pqf", [128, 128]); pkf = sb("pkf", [128, 3])
        rel = sb("rel", [128, 128]); ind = sb("ind", [128, 128]); bacc = sb("bacc", [128, 4, 128])
        p = Phase(nc, "p3" + mode)
        if dsa:
            p.memset("dve", kiT[64:128, :], 0.0, w=["kiT"])
            p.memset("pool", kaT[64:128, :], 0.0, w=["kaT"])
            for sl_ in range(2):
                p.memset("pool", qi2[sl_][64:128, :, :], 0.0, w=[f"qi{sl_}"])
                p.memset("pool", qa2[sl_][64:128, :, :], 0.0, w=[f"qa{sl_}"])
            p.dma("sp", kiT[0:64, :], g["kiT"], w=["kiT"], lane="kiT")
            p.dma("sp", kaT[0:64, :], g["kaT"], w=["kaT"], lane="kaT")
            vav = g["va"].rearrange("(n p) d -> p n d", p=128)
            for q4 in range(16):
                p.dma("sp", va[:, q4 * 4:(q4 + 1) * 4, :], vav[:, q4 * 4:(q4 + 1) * 4, :], w=["va"], lane="va")
        else:
            vbv = g["vb"].rearrange("(n p) h d -> p n (h d)", p=128)
            for q4 in range(16):
                p.dma("sp", vb[:, q4 * 4:(q4 + 1) * 4, :], vbv[:, q4 * 4:(q4 + 1) * 4, :], w=["vb"], lane="vbl")
        for q4 in range(4):
            p.copy("dve", I4[:, q4 * 128:(q4 + 1) * 128], identb, r=["ident"], w=["I4"])
        p.memset("dve", sel[:], 0.0, w=["sel"])
        p.memset("dve", sel[64:65, :], 1.0, w=["sel"])
        for k in range(NBIS + 1):
            p.memset("dve", pw[:, k:k + 1], 2.0 ** -(k + 1), w=["pw"])
        if dsa:
            p.dma("sp", rb[:], g["rb128"], w=["rb"], lane="rb")
            p.dma("sp", pqi[:], g["posq_bc"], w=["pqi"], lane="pqi")
            p.dma("sp", pki[:], g["posk_col"], w=["pki"], lane="pki")
            p.copy("dve", pqf[:], pqi[:], r=["pqi"], w=["pqf"])
            p.copy("dve", pkf[:], pki[:], r=["pki"], w=["pkf"])
            prev = 15
            for j, (th, nb_) in enumerate(TH):
                p.tt("dve", dl[:, j, :], rb[:, nb_, :], rb[:, prev, :], ALU.subtract, r=["rb"], w=["dl"])
                prev = nb_
            for ty in range(3):
                p.ts("dve", rel[:], pqf[:], pkf[:, ty:ty + 1], -1.0, ALU.subtract, ALU.mult, r=["pqf", "pkf"], w=["rel"])
                for hh in range(2):
                    p.memset("pool", bacc[:], 0.0, w=["bacc"] + [f"bacc{h}" for h in range(4)])
                    for j, (th, nb_) in enumerate(TH):
                        p.ts("dve", ind[:], rel[:], float(th), None, ALU.is_ge, r=["rel"], w=["ind"])
                        for h in range(4):
                            p.stt("dve", bacc[:, h, :], ind[:], dl[:, j, hh * 4 + h:hh * 4 + h + 1], bacc[:, h, :], ALU.mult, ALU.add,
                                  r=["ind", "dl", f"bacc{h}"], w=[f"bacc{h}"])
                    p.copy("dve", Bt[:, ty, hh * 512:(hh + 1) * 512], bacc[:].rearrange("p h q -> p (h q)"),
                           r=["bacc"] + [f"bacc{h}" for h in range(4)], w=["Bt", "bacc"])

        bkI = Banks(ps, [0, 1, 2, 3])

        def normalize(accs, width, dst, dkey, dma_fn, part=None):
            if part in (None, 0):
                for hf_, (b, kb_) in enumerate(accs):
                    p.copy("act", osb[:, hf_ * 512:(hf_ + 1) * 512], b[0:65, :], r=[kb_], w=["osb"])
            if part == 0:
                return
            for hf_ in range(len(accs)):
                b, kb_ = ps[6 + hf_ % 2], f"ps{6 + hf_ % 2}"
                p.copy("dve", dhi[:], osb[:, hf_ * 512:(hf_ + 1) * 512], r=["osb"], w=["dhi"])
                p.tt("dve", dlo[:], osb[:, hf_ * 512:(hf_ + 1) * 512], dhi[:], ALU.subtract, r=["osb", "dhi"], w=["dlo"])
                p.mm(b[0:64, :], sel[:], dhi[:], True, False, r=["dhi", "sel"], w=[kb_])
                p.mm(b[0:64, :], sel[:], dlo[:], False, True, r=["dlo", "sel"], w=[kb_])
                p.add("dve", lambda e, b=b, hf_=hf_: e.reciprocal(out=rden[:, hf_ * 512:(hf_ + 1) * 512], in_=b[0:64, :]), r=[kb_], w=["rden"])
            p.tt("dve", oo[:, :width], osb[0:64, :width], rden[:, :width], ALU.mult, r=["osb", "rden"], w=["oo"])
            dma_fn()

        kbcount = [0]

        def dsa_A(i):
            sl = i % 2
            Q0 = i * 128
            p.dma("sp", qi2[sl][0:64, :, :], g["qiT"][:, :, Q0:Q0 + 128], w=[f"qi{sl}"], lane=f"qi{sl}")
            p.dma("sp", qa2[sl][0:64, :, :], g["qaT"][:, :, Q0:Q0 + 128], w=[f"qa{sl}"], lane=f"qa{sl}")
            p.dma("sp", wq2[sl][:], g["widx"][Q0:Q0 + 128, :], w=[f"wq{sl}"], lane=f"wq{sl}")
            p.dma("sp", cmq2[sl][:], g["cmq"][i], w=[f"cmq{sl}"], lane=f"cmq{sl}")
            qi, wq, Dh, Isc = qi2[sl], wq2[sl], Dh2[sl], Isc2[sl]
            for h in range(8):
                p.ts("pool", Dh[:, h, :], identb, wq[:, h:h + 1], 1.0, ALU.mult, ALU.mult, r=["ident", f"wq{sl}"], w=[f"Dh{sl}"])
            groups = []
            for (k0, n) in ((0, (i + 1) * 128), (4096, (i + 1) * 128)):
                o = 0
                while o < n:
                    w_ = min(512, n - o)
                    groups.append((k0 + o, w_))
                    o += w_
            col = 0
            for gi, (k0, w_) in enumerate(groups):
                R_ = rl[gi % 2]
                rk = f"rl{gi%2}"
                for h in range(8):
                    b, kb_ = bkI.get()
                    p.mm(b[:, :w_], qi[:, h, :], kiT[:, k0:k0 + w_], True, True, r=[f"qi{sl}", "kiT"], w=[kb_])
                    p.act(R_[:, h, :w_], b[:, :w_], AF.Relu, r=[kb_], w=[rk])
                b, kb_ = ps[4 + gi % 2], f"ps{4 + gi % 2}"
                for h in range(8):
                    p.mm(b[:, :w_], Dh[:, h, :], R_[:, h, :w_], h == 0, h == 7, r=[f"Dh{sl}", rk], w=[kb_])
                p.copy("act", Isc[:, col:col + w_], b[:, :w_], r=[kb_], w=[f"Isc{sl}"])
                col += w_

        def dsa_B(i):
            sl = i % 2
            W = 2 * (i + 1) * 128
            Isc, nm, sm, hk, h2, cmq = Isc2[sl], nm2[sl], sm2[sl], hk2[sl], h22[sl], cmq2[sl]
            ik, sk, hkk = f"Isc{sl}", f"sm{sl}", f"hk{sl}"
            p.add("dve", lambda e, W=W: e.tensor_reduce(out=sm[:, 0:1], in_=Isc[:, :W], axis=AX.X, op=ALU.max, apply_absolute_value=True),
                  r=[ik], w=[sk])
            c_own = i * 128
            c_oth = (i + 1) * 128 + i * 128
            p.tt("dve", Isc[:, c_own:c_own + 128], Isc[:, c_own:c_own + 128], cmq[:, 0:128], ALU.add, r=[ik, f"cmq{sl}"], w=[ik])
            p.tt("dve", Isc[:, c_oth:c_oth + 128], Isc[:, c_oth:c_oth + 128], cmq[:, 128:256], ALU.add, r=[ik, f"cmq{sl}"], w=[ik])
            p.ts("dve", sm[:, 1:2], sm[:, 0:1], 2.02, 2e-6, ALU.mult, ALU.add, r=[sk], w=[sk])
            p.ts("dve", hk[:], pw[:], sm[:, 1:2], None, ALU.mult, r=["pw", sk], w=[hkk])
            p.ts("dve", h2[:], hk[:], 2.0, None, ALU.mult, r=[hkk], w=[hkk])
            p.ts("dve", sm[:, 2:3], sm[:, 1:2], 0.0, None, ALU.mult, r=[sk], w=[sk])
            for k in range(NBIS):
                p.ts("dve", junk[:, :W], Isc[:, :W], sm[:, 2:3], None, ALU.is_ge, ALU.add, r=[ik, sk], w=["junk", f"cnt{sl}"], accum_out=sm[:, 3:4])
                p.ts("dve", sm[:, 4:5], sm[:, 3:4], float(TOPK), h2[:, k + 1:k + 2], ALU.is_ge, ALU.mult, r=[f"cnt{sl}", hkk], w=[f"tmp{sl}"])
                p.stt("dve", sm[:, 2:3], sm[:, 4:5], hk[:, k + 1:k + 2], sm[:, 2:3], ALU.subtract, ALU.add, r=[f"tmp{sl}", sk, hkk], w=[sk])
            p.ts("dve", nm[:, :W], Isc[:, :W], sm[:, 2:3], NEG, ALU.is_lt, ALU.mult, r=[ik, sk], w=[f"nm{sl}"])

        def dsa_C(i, part):
            sl = i % 2
            Q0 = i * 128
            nk = 2 * (i + 1)
            blocks = list(range(i + 1)) + list(range(32, 32 + i + 1))
            qa, nm = qa2[sl], nm2[sl]
            accs = [(ps[4], "ps4"), (ps[5], "ps5")]

            def s_stage(c, L):
                near = None
                if L == i:
                    near = 0
                elif L == 32 + i - 1:
                    near = 1
                elif L == 32 + i:
                    near = 2
                P_ = pt[c % 3]
                pk = f"pt{c%3}"
                for hf_ in range(2):
                    b, kb_ = ps[(c % 2) * 2 + hf_], f"ps{(c % 2) * 2 + hf_}"
                    p.mm(b[:], kaT[:, L * 128:(L + 1) * 128], qa[:, hf_ * 4:(hf_ + 1) * 4, :].rearrange("d h q -> d (h q)"), True, False,
                         r=["kaT", f"qa{sl}"], w=[kb_])
                    p.mm(b[:], nm[:, c * 128:(c + 1) * 128], I4[:], False, near is None, r=[f"nm{sl}", "I4"], w=[kb_])
                    if near is not None:
                        p.mm(b[:], identb, Bt[:, near, hf_ * 512:(hf_ + 1) * 512], False, True, r=["ident", "Bt"], w=[kb_])
                    p.act(P_[:, hf_ * 512:(hf_ + 1) * 512], b[:], AF.Exp, r=[kb_], w=[pk])

            def pv_stage(c, L):
                P_ = pt[c % 3]
                pk = f"pt{c%3}"
                for hf_ in range(2):
                    b, kb_ = accs[hf_]
                    p.mm(b[0:65, :], va[:, L, :], P_[:, hf_ * 512:(hf_ + 1) * 512], c == 0, c == nk - 1, r=["va", pk], w=[kb_])

            def dma_oa(Q0=Q0):
                p.dma("sp", g["oaT"][:, :, Q0:Q0 + 128], oo[:, :].rearrange("d (h q) -> d h q", h=8), r=["oo"], lane="oa")
            if part == 0:
                s_stage(0, blocks[0])
                for c, L in enumerate(blocks):
                    if c + 1 < nk:
                        s_stage(c + 1, blocks[c + 1])
                    pv_stage(c, L)
                normalize(accs, 1024, oo, "oo", dma_oa, part=0)
            else:
                normalize(accs, 1024, oo, "oo", dma_oa, part=1)

        def kb_load(j, h, slot):
            nb_own = 4 * j + 4
            K_, kk_ = kb[slot], f"kb{slot}"
            p.dma("sp", K_[:, 0:nb_own * 128], g["kbT"][h, :, 0:nb_own * 128], w=[kk_], lane=kk_ + "a")
            p.dma("sp", K_[:, nb_own * 128:2 * nb_own * 128], g["kbT"][h, :, 4096:4096 + nb_own * 128], w=[kk_], lane=kk_ + "b")

        def mla_tile(j):
            T0 = j * NT
            nb_own = 4 * j + 4
            tblocks = list(range(nb_own)) + list(range(32, 32 + nb_own))
            n_ = len(tblocks)
            p.dma("sp", qb[:], g["qbT"][:, :, T0:T0 + NT], w=["qb"], lane="qb")
            p.dma("pool", cmk[:], g["cmk"][j], w=["cmk"], lane="cmk")
            if j == 0:
                kb_load(0, 0, 0)
            for h in range(8):
                slot = (j * 8 + h) % 2
                K_, kk_ = kb[slot], f"kb{slot}"
                if h + 1 < 8:
                    kb_load(j, h + 1, 1 - slot)
                elif j + 1 < 8:
                    kb_load(j + 1, 0, 1 - slot)
                acc = (ps[4 + h % 2], f"ps{4 + h % 2}")

                def s_stage(c, L):
                    b, kb_ = ps[c % 4], f"ps{c % 4}"
                    mi = None
                    if 4 * j <= L < 4 * j + 4:
                        mi = L - 4 * j
                    elif 32 + 4 * j <= L < 32 + 4 * j + 4:
                        mi = 4 + L - 32 - 4 * j
                    p.mm(b[:], K_[:, c * 128:(c + 1) * 128], qb[:, h, :], True, mi is None, r=[kk_, "qb"], w=[kb_])
                    if mi is not None:
                        p.mm(b[:], identb, cmk[:, mi, :], False, True, r=["ident", "cmk"], w=[kb_])
                    p.act(pt[c % 3][:, 0:512], b[:], AF.Exp, r=[kb_], w=[f"pt{c%3}"])

                def pv_stage(c, L):
                    p.mm(acc[0][0:65, :], vb[:, L, h * 65:(h + 1) * 65], pt[c % 3][:, 0:512], c == 0, c == n_ - 1, r=["vb", f"pt{c%3}"], w=[acc[1]])

                s_stage(0, tblocks[0])
                s_stage(1, tblocks[1])
                for c, L in enumerate(tblocks):
                    if c + 2 < n_:
                        s_stage(c + 2, tblocks[c + 2])
                    pv_stage(c, L)

                def dma_ob(h=h, T0=T0):
                    p.dma("sp", g["obT"][:, h, T0:T0 + NT], oo[:, 0:512], r=["oo"], lane="ob")
                normalize([acc], 512, oo, "oo", dma_ob)

        nblk = min(NB, NBLK)
        if dsa:
            for i in range(min(2, nblk)):
                dsa_A(i)
                dsa_B(i)
            for i in range(nblk):
                dsa_C(i, 0)
                if i + 2 < nblk:
                    dsa_A(i + 2)
                dsa_C(i, 1)
                if i + 2 < nblk:
                    dsa_B(i + 2)
        else:
            for i in range(nblk):
                if i % 4 == 3:
                    mla_tile(i // 4)
        p.emit()


def merge_phase(nc, g, x1T, x2T, ps, G2):
    with contextlib.ExitStack() as es:
        sb = lambda n, s, d=F32: es.enter_context(nc.sbuf_tensor("p4" + n, list(s), d))
        woa = sb("woa", [128, 4, D], BF16); wob = sb("wob", [128, 4, D], BF16); wout = sb("wout", [128, 8, D], BF16)
        oa = sb("oa", [128, 4, NT], BF16); ob = sb("ob", [128, 4, NT], BF16)
        gt = sb("gt", [128, 16, NT]); xs = sb("xs", [128, 8, NT])
        y = sb("y", [128, 8, NT], BF16); t1 = sb("t1", [128, NT]); t2 = sb("t2", [128, NT])
        p = Phase(nc, "p4")
        p.dma("pool", woa[:], g["w_o_a"].rearrange("(c p) n -> p c n", p=128), w=["woa"], lane="woa", max_dma_last_dim=4096)
        p.dma("pool", wob[:], g["w_o_b"].rearrange("(c p) n -> p c n", p=128), w=["wob"], lane="wob", max_dma_last_dim=4096)
        p.dma("pool", wout[:], g["w_out"].rearrange("(c p) n -> p c n", p=128), w=["wout"], lane="wout", max_dma_last_dim=4096)
        x1v = x1T.rearrange("(c p) t -> p c t", p=128)
        x2v = x2T.rearrange("(c p) t -> p c t", p=128)
        gv = g["gT"].rearrange("(c p) t -> p c t", p=128)
        for t in range(SOWN // NT):
            T0 = t * NT
            for par in range(2):
                p.dma("sp", oa[par * 64:(par + 1) * 64, :, :], g["oaT"].rearrange("d (c two) t -> d two c t", two=2)[:, par, :, T0:T0 + NT],
                      w=["oa"], lane="oa")
                p.dma("sp", ob[par * 64:(par + 1) * 64, :, :], g["obT"].rearrange("d (c two) t -> d two c t", two=2)[:, par, :, T0:T0 + NT],
                      w=["ob"], lane="ob")
            p.dma("sp", gt[:], gv[:, :, T0:T0 + NT], w=["gt"], lane="gt")
            p.dma("sp", xs[:], x1v[:, :, T0:T0 + NT], w=["xs"], lane="xs")
            for m in range(8):
                ba, ka = ps[m % 2], f"ps{m%2}"
                bb, kb_ = ps[2 + m % 2], f"ps{2 + m%2}"
                for h in range(4):
                    p.mm(ba[:], woa[:, h, m * 128:(m + 1) * 128], oa[:, h, :], h == 0, h == 3, r=["woa", "oa"], w=[ka])
                for h in range(4):
                    p.mm(bb[:], wob[:, h, m * 128:(m + 1) * 128], ob[:, h, :], h == 0, h == 3, r=["wob", "ob"], w=[kb_])
                p.tt("dve", t1[:], ba[:], gt[:, m, :], ALU.mult, r=[ka, "gt"], w=["t1"])
                p.tt("dve", t2[:], bb[:], gt[:, 8 + m, :], ALU.mult, r=[kb_, "gt"], w=["t2"])
                p.tt("dve", y[:, m, :], t1[:], t2[:], ALU.add, r=["t1", "t2"], w=[f"y{m}"])
            for m in range(8):
                b, kb_ = ps[4 + m % 2], f"ps{4 + m%2}"
                for c in range(8):
                    p.mm(b[:], wout[:, c, m * 128:(m + 1) * 128], y[:, c, :], c == 0, c == 7, r=["wout", f"y{c}"], w=[kb_])
                p.stt("dve", xs[:, m, :], b[:], G2[:, m:m + 1], xs[:, m, :], ALU.mult, ALU.add, r=[kb_, "xs"], w=["xs"])
            p.dma("sp", x2v[:, :, T0:T0 + NT], xs[:], r=["xs"], lane="st")
        p.emit()


def build(stage=99, debug=False):
    nc = bass.Bass("TRN2", target_bir_lowering=False)
    dt = lambda n, s, d=F32: nc.dram_tensor(n, list(s), d, kind="ExternalInput").ap()
    xT = dt("xT", [D, S])
    cvec = dt("cvec", [128, 8])
    w_ada = dt("w_ada", [D, 9 * D])
    b_ada = dt("b_ada", [128, 72])
    g_ffn1 = dt("g_ffn1", [128, 8]); g_mix = dt("g_mix", [128, 8]); g_ffn2 = dt("g_ffn2", [128, 8]); g_final = dt("g_final", [128, 8])
    w1i = dt("w_ffn1_in", [D, 2 * DFF]); w1d = dt("w_ffn1_down", [DFF, D])
    w2i = dt("w_ffn2_in", [D, 2 * DFF]); w2d = dt("w_ffn2_down", [DFF, D])
    ident_d = dt("ident", [128, 128])
    g = {}
    g["winP"] = dt("winP", [D, WTOT])
    g["w_uk"] = dt("w_uk", [256, 512]); g["w_uv"] = dt("w_uv", [256, 512])
    g["w_uq"] = dt("w_uq", [384, 768]); g["w_uqs"] = dt("w_uqs", [384, 768])
    g["g_ckv"] = dt("g_ckv", [128, 2]); g["g_cq"] = dt("g_cq", [128, 3])
    g["freqc"] = dt("freqc", [128, 1]); g["sgnc"] = dt("sgnc", [128, 1])
    g["pos32"] = dt("pos32", [32, S], I32)
    g["rb128"] = dt("rb128", [128, 32, 8])
    g["posq_bc"] = dt("posq_bc", [128, 128], I32); g["posk_col"] = dt("posk_col", [128, 3], I32)
    g["cmq"] = dt("cmq", [32, 128, 256]); g["cmk"] = dt("cmk", [8, 128, 8, 512])
    g["w_o_a"] = dt("w_o_a", [512, D]); g["w_o_b"] = dt("w_o_b", [512, D]); g["w_out"] = dt("w_out", [D, D])
    outT = nc.dram_tensor("outT", [D, SOWN], F32, kind="ExternalOutput").ap()
    dbgset = set(debug.split(",")) if debug else set()
    it = lambda n, s, d=F32: nc.dram_tensor(n, list(s), d, kind=("ExternalOutput" if n in dbgset else "Internal")).ap()
    x1T = it("x1T", [D, S]); x2T = it("x2T", [D, SOWN])
    g["kaT"] = it("kaT", [64, S], BF16); g["kiT"] = it("kiT", [64, S], BF16)
    g["kbT"] = it("kbT", [8, 96, S], BF16); g["vb"] = it("vb", [S, 8, 65], BF16); g["va"] = it("va", [S, 65], BF16)
    g["qaT"] = it("qaT", [64, 8, SOWN], BF16); g["qiT"] = it("qiT", [64, 8, SOWN], BF16)
    g["widx"] = it("widx", [SOWN, 8]); g["qbT"] = it("qbT", [96, 8, SOWN], BF16)
    g["gT"] = it("gT", [2048, SOWN])
    g["oaT"] = it("oaT", [64, 8, SOWN], BF16); g["obT"] = it("obT", [64, 8, SOWN], BF16)

    with contextlib.ExitStack() as es:
        ps = [es.enter_context(nc.psum_tensor(f"psb{i}", [128, 512], F32)) for i in range(8)]
        sb = lambda n, s, d=F32: es.enter_context(nc.sbuf_tensor(n, list(s), d))
        ones_t = sb("ones", [128, 128], BF16); ones = ones_t[:]
        epsc_t = sb("epsc", [128, 1]); epsc = epsc_t[:]
        identb_t = sb("identb", [128, 128], BF16); identb = identb_t[:]
        modT_t = sb("modT", [128, 72]); modT = modT_t[:]
        drv_t = sb("drv", [128, 10, 8])
        derived = [drv_t[:, i, :] for i in range(9)]
        gfin = drv_t[:, 9, :]
        A1, S1, G1, A2, S2, G2, A3, S3, G3 = derived

        setup_phase(nc, cvec, w_ada, b_ada, [g_ffn1, g_mix, g_ffn2], modT, None, ones, epsc, ident_d, identb, ps, derived)
        pp = Phase(nc, "gfin")
        pp.dma("sp", gfin, g_final, w=["gf"], lane="gf")
        pp.emit()
        if stage == 0:
            return nc
        if stage == 20:
            proj_phase(nc, x1T, g, ps, ones, epsc, A2, S2)
            return nc
        if stage in (30, 31):
            import os
            global NBLK
            NBLK = int(os.environ.get("NBLK", "32"))
            attn_phase(nc, g, ps, identb, "dsa" if stage == 30 else "mla")
            return nc
        ffn_phase(nc, "f1", xT, x1T, S // NT, w1i, w1d, A1, S1, G1, ps, ones, epsc)
        if stage == 1:
            ffn_phase(nc, "f2", x1T[:, 0:SOWN], outT, SOWN // NT, w2i, w2d, A3, S3, G3, ps, ones, epsc, gfin=gfin)
            return nc
        proj_phase(nc, x1T, g, ps, ones, epsc, A2, S2)
        if stage == 2:
            return nc
        attn_phase(nc, g, ps, identb, "dsa")
        if stage == 3:
            return nc
        attn_phase(nc, g, ps, identb, "mla")
        if stage == 4:
            return nc
        merge_phase(nc, g, x1T, x2T, ps, G2)
        ffn_phase(nc, "f2", x2T, outT, SOWN // NT, w2i, w2d, A3, S3, G3, ps, ones, epsc, gfin=gfin)
    return nc


def local_perm(p):
    own = np.arange(32) * 2 + p
    oth = np.arange(32) * 2 + 1 - p
    blocks = np.concatenate([own, oth])
    return (blocks[:, None] * 128 + np.arange(128)[None, :]).reshape(-1)


def pm(v):
    v = np.asarray(v, np.float32)
    return np.ascontiguousarray(v.reshape(-1, 128).T)


FREQ16 = [1.0, 0.5623413324356079, 0.3162277638912201, 0.17782793939113617, 0.10000000149011612, 0.05623413249850273,
          0.03162277489900589, 0.017782794311642647, 0.009999999776482582, 0.005623413249850273, 0.003162277629598975,
          0.0017782794311642647, 0.0010000000474974513, 0.000562341301701963, 0.0003162277571391314, 0.00017782794020604342]


def host_consts(p):
    perm = local_perm(p)
    lim = (perm // 64 + 1) * 64
    cmq = np.zeros((32, 128, 256), np.float32)
    for i in range(32):
        ql = lim[i * 128:(i + 1) * 128][:, None]
        for half, kbk in ((0, i), (1, 32 + i)):
            kt = perm[kbk * 128:(kbk + 1) * 128][None, :]
            cmq[i, :, half * 128:(half + 1) * 128] = np.where(kt < ql, 0.0, -1e30)
    cmk = np.zeros((8, 128, 8, 512), np.float32)
    for j in range(8):
        ql = lim[j * 512:(j + 1) * 512][None, :]
        for mi in range(8):
            kbk = 4 * j + mi if mi < 4 else 32 + 4 * j + (mi - 4)
            kt = perm[kbk * 128:(kbk + 1) * 128][:, None]
            cmk[j, :, mi, :] = np.where(kt < ql, 0.0, NEG)
    freqc = np.zeros((128, 1), np.float32)
    sgnc = np.zeros((128, 1), np.float32)
    for r in range(32):
        freqc[64 + r, 0] = FREQ16[r % 16]
        sgnc[64 + r, 0] = -1.0 if r < 16 else 1.0
    return perm, cmq, cmk, freqc, sgnc


def kernel(**inputs):
    import os
    stage = int(os.environ.get("KSTAGE", "99"))
    debug = os.environ.get("KDEBUG", "")
    f = lambda a: np.ascontiguousarray(np.asarray(a, np.float32))
    x = np.asarray(inputs["x"], np.float32)
    w_in = np.asarray(inputs["w_in"][0], np.float32)
    q_a, k_a, v_a = w_in[:, 0:512], w_in[:, 512:576], w_in[:, 576:640]
    q_i, k_i, w_i = w_in[:, 640:1152], w_in[:, 1152:1216], w_in[:, 1216:1224]
    c_q, c_kv, k_r, gts = w_in[:, 1224:1608], w_in[:, 1608:1864], w_in[:, 1864:1896], w_in[:, 1896:3944]
    k_rs = np.concatenate([k_r[:, 16:32], k_r[:, 0:16]], axis=1)
    winP = np.ascontiguousarray(np.concatenate([k_a, k_i, c_kv, k_a, k_r, k_a, k_rs, v_a, q_a, q_i, c_q, gts, w_i], axis=1))
    assert winP.shape[1] == WTOT
    w_uq = np.asarray(inputs["w_uq"][0], np.float32)
    w_uqs = w_uq.copy().reshape(384, 8, 96)
    w_uqs[:, :, 64:80], w_uqs[:, :, 80:96] = w_uq.reshape(384, 8, 96)[:, :, 80:96], w_uq.reshape(384, 8, 96)[:, :, 64:80]
    w_uqs = np.ascontiguousarray(w_uqs.reshape(384, 768))
    nc = build(stage, debug)
    in_maps = []
    perms = []
    pos_all = np.asarray(inputs["positions"], np.int32)
    rb128 = np.ascontiguousarray(np.broadcast_to(f(inputs["rel_bias"])[None], (128, 32, 8)))
    consts = [host_consts(0), host_consts(1)]
    for core in range(NCORES):
        b, p = core // 2, core % 2
        perm, cmq, cmk, freqc, sgnc = consts[p]
        perms.append(perm)
        posl = pos_all[b][perm]
        m = {
            "xT": np.ascontiguousarray(x[b][perm].T),
            "cvec": pm(inputs["c"][b]),
            "w_ada": f(inputs["w_ada"][0]),
            "b_ada": pm(inputs["b_ada"][0]),
            "g_ffn1": pm(inputs["g_ffn1"][0]),
            "g_mix": pm(inputs["g_mix"][0]),
            "g_ffn2": pm(inputs["g_ffn2"][0]),
            "g_final": pm(inputs["g_final"]),
            "w_ffn1_in": f(inputs["w_ffn1_in"][0]),
            "w_ffn1_down": f(inputs["w_ffn1_down"][0]),
            "w_ffn2_in": f(inputs["w_ffn2_in"][0]),
            "w_ffn2_down": f(inputs["w_ffn2_down"][0]),
            "ident": np.eye(128, dtype=np.float32),
            "winP": winP, "w_uk": f(inputs["w_uk"][0]), "w_uv": f(inputs["w_uv"][0]), "w_uq": w_uq, "w_uqs": w_uqs,
            "g_ckv": pm(inputs["g_ckv"][0]), "g_cq": pm(inputs["g_cq"][0]),
            "freqc": freqc, "sgnc": sgnc,
            "pos32": np.ascontiguousarray(np.broadcast_to(posl[None], (32, S))),
            "rb128": rb128,
            "posq_bc": np.ascontiguousarray(np.broadcast_to(posl[128:256][None], (128, 128))),
            "posk_col": np.ascontiguousarray(np.stack([posl[128:256], posl[32 * 128:33 * 128], posl[33 * 128:34 * 128]], axis=1)),
            "cmq": cmq, "cmk": cmk,
            "w_o_a": f(inputs["w_o_a"][0]), "w_o_b": f(inputs["w_o_b"][0]), "w_out": f(inputs["w_out"][0]),
        }
        in_maps.append(m)
    res = run_bass_kernel_spmd(nc, in_maps, core_ids=list(range(NCORES)))
    if debug:
        kernel.debug = res.results
        kernel.perms = perms
    out = np.empty((4, S, D), np.float32)
    for core in range(NCORES):
        b = core // 2
        o = res.results[core]["outT"]
        out[b][perms[core][:SOWN]] = o.T
    return out
```
